# Optimizing a Trainium2 kernel written in Bass

```python
import jax, jax.numpy as jnp
from jax import lax
import numpy as np

D_MODEL = 1024
BATCH = 8
SEQ = 8192
DEPTH = 1

MIX_WIDTH = D_MODEL
ATTN_WIDTH = MIX_WIDTH // 2
POOL_WIDTH = MIX_WIDTH - ATTN_WIDTH
N_HEADS = 8
HEAD_DIM = ATTN_WIDTH // N_HEADS
DILATED_BRANCHES = ((128, 1), (512, 4), (2048, 16))
MAX_SPAN = 2048
Q_BLOCK = 128
POOL_WINDOWS = (2, 4, 8, 16)
POOL_GROUP = POOL_WIDTH // len(POOL_WINDOWS)
PEER_HEADS = 8
PEER_NKEYS = 128
PEER_EXPERTS = PEER_NKEYS * PEER_NKEYS
PEER_QDIM = 256
PEER_TOPK = 16
TOKEN_CHUNK = 128
N_MOD = 6
EPS = 1e-6

kernel_name = "hybrid_dilated_attn_pool_peer"


def rmsnorm(x, g):
    xf = x.astype(jnp.float32)
    y = xf * lax.rsqrt(jnp.mean(xf * xf, axis=-1, keepdims=True) + EPS)
    return (y * g.astype(jnp.float32)).astype(x.dtype)


def alibi_slopes(n):
    return jnp.asarray([2.0 ** (-8.0 * (h + 1) / n) for h in range(n)], dtype=jnp.float32)


def dilated_attention(q, k, v):
    B, S, H, hd = q.shape
    nblk = S // Q_BLOCK
    pad = ((0, 0), (MAX_SPAN, 0), (0, 0), (0, 0))
    k_pad = jnp.pad(k, pad)
    v_pad = jnp.pad(v, pad)
    slopes = alibi_slopes(H)
    scale = HEAD_DIM ** -0.5
    qi = jnp.arange(Q_BLOCK)
    branches = []
    for w, d in DILATED_BRANCHES:
        j = jnp.arange(w // d + 1)
        gidx = qi[:, None] + MAX_SPAN - j[None, :] * d
        dist = j * d
        branches.append((gidx, dist))

    def block(idx):
        b = idx // nblk
        start = (idx % nblk) * Q_BLOCK
        qb = lax.dynamic_slice(q, (b, start, 0, 0), (1, Q_BLOCK, H, hd))[0].astype(jnp.float32) * scale
        ks = lax.dynamic_slice(k_pad, (b, start, 0, 0), (1, MAX_SPAN + Q_BLOCK, H, hd))[0]
        vs = lax.dynamic_slice(v_pad, (b, start, 0, 0), (1, MAX_SPAN + Q_BLOCK, H, hd))[0]
        pos = start + qi
        lses, outs = [], []
        for gidx, dist in branches:
            kg = jnp.take(ks, gidx, axis=0).astype(jnp.float32)
            vg = jnp.take(vs, gidx, axis=0).astype(jnp.float32)
            distf = dist.astype(jnp.float32)
            s = jnp.einsum('qhd,qjhd->hqj', qb, kg) - slopes[:, None, None] * distf[None, None, :]
            valid = (pos[:, None] - dist[None, :]) >= 0
            s = jnp.where(valid[None], s, -1e30)
            m = jnp.max(s, axis=-1, keepdims=True)
            p = jnp.exp(s - m)
            z = jnp.sum(p, axis=-1, keepdims=True)
            o = jnp.einsum('hqj,qjhd->qhd', p, vg) / jnp.transpose(z, (1, 0, 2))
            lses.append(jnp.transpose((m + jnp.log(z))[..., 0], (1, 0)))
            outs.append(o)
        wts = jax.nn.softmax(jnp.stack(lses, axis=0), axis=0)
        out = jnp.einsum('gqh,gqhd->qhd', wts, jnp.stack(outs, axis=0))
        return out.astype(q.dtype)

    out = lax.map(block, jnp.arange(B * nblk))
    return out.reshape(B, S, H, hd)


def multiscale_pool(u, w_pool, pool_scale):
    B, S, C = u.shape
    uf = u.astype(jnp.float32)
    cs = jnp.pad(jnp.cumsum(uf, axis=1), ((0, 0), (1, 0), (0, 0)))
    pos1 = jnp.arange(1, S + 1, dtype=jnp.float32)
    outs = []
    for g, w in enumerate(POOL_WINDOWS):
        sl = slice(g * POOL_GROUP, (g + 1) * POOL_GROUP)
        cg = cs[:, :, sl]
        upper = cg[:, 1:]
        lower = jnp.pad(cg, ((0, 0), (w - 1, 0), (0, 0)))[:, :S]
        cnt = jnp.minimum(pos1, float(w))[None, :, None]
        mixed = (upper - lower) / cnt - uf[:, :, sl]
        outs.append(jnp.einsum('bsc,ce->bse', mixed, w_pool[g].astype(jnp.float32)))
    y = jnp.concatenate(outs, axis=-1) * pool_scale.astype(jnp.float32)
    return y.astype(u.dtype)


def peer_ffn(h, w_q, sub_keys, expert_u, expert_v):
    B, S, D = h.shape
    T = B * S
    half = PEER_QDIM // 2
    K = PEER_TOPK
    hc = h.reshape(T // TOKEN_CHUNK, TOKEN_CHUNK, D)

    def chunk(xc):
        C = xc.shape[0]
        q = jnp.einsum('cd,de->ce', xc, w_q).astype(jnp.float32).reshape(C, PEER_HEADS, 2, half)
        s = jnp.einsum('chpk,pnk->chpn', q, sub_keys.astype(jnp.float32))
        sv, si = lax.top_k(s, K)
        cand = sv[:, :, 0, :, None] + sv[:, :, 1, None, :]
        cand_id = si[:, :, 0, :, None] * PEER_NKEYS + si[:, :, 1, None, :]
        cand = cand.reshape(C, PEER_HEADS, K * K)
        cand_id = cand_id.reshape(C, PEER_HEADS, K * K)
        top_v, top_pos = lax.top_k(cand, K)
        eid = jnp.take_along_axis(cand_id, top_pos, axis=-1)
        gate = jax.nn.softmax(top_v, axis=-1)
        u = expert_u[eid]
        act = jax.nn.gelu(jnp.einsum('cd,chkd->chk', xc, u).astype(jnp.float32), approximate=False)
        vv = expert_v[eid]
        y = jnp.einsum('chk,chkd->cd', (gate * act).astype(vv.dtype), vv)
        return y.astype(xc.dtype)

    return lax.map(chunk, hc).reshape(B, S, D)


def setup_inputs(seed: int = 0) -> dict:
    key = jax.random.key(seed)
    ks = jax.random.split(key, 16)
    D = D_MODEL
    proj_out = 3 * ATTN_WIDTH + POOL_WIDTH
    f32 = jnp.float32
    x = jax.random.normal(ks[0], (BATCH, SEQ, D), f32)
    c = jax.random.normal(ks[1], (BATCH, D), f32)
    w_ada = jax.random.normal(ks[2], (DEPTH, D, N_MOD * D), f32) * (0.5 * D ** -0.5)
    b_ada = jax.random.normal(ks[3], (DEPTH, N_MOD * D), f32) * 0.01
    norm1_g = 1.0 + 0.05 * jax.random.normal(ks[4], (DEPTH, D), f32)
    w_in = jax.random.normal(ks[5], (DEPTH, D, proj_out), f32) * D ** -0.5
    pool_w = jax.random.normal(ks[6], (DEPTH, len(POOL_WINDOWS), POOL_GROUP, POOL_GROUP), f32) * POOL_GROUP ** -0.5
    pool_scale = 1.0 + 0.1 * jax.random.normal(ks[7], (DEPTH, POOL_WIDTH), f32)
    w_out = jax.random.normal(ks[8], (DEPTH, MIX_WIDTH, D), f32) * MIX_WIDTH ** -0.5
    norm2_g = 1.0 + 0.05 * jax.random.normal(ks[9], (DEPTH, D), f32)
    peer_wq = jax.random.normal(ks[10], (DEPTH, D, PEER_HEADS * PEER_QDIM), f32) * D ** -0.5
    peer_subkeys = jax.random.normal(ks[11], (DEPTH, 2, PEER_NKEYS, PEER_QDIM // 2), f32) * (PEER_QDIM // 2) ** -0.5
    peer_u = jax.random.normal(ks[12], (DEPTH, PEER_EXPERTS, D), f32) * D ** -0.5
    peer_v = jax.random.normal(ks[13], (DEPTH, PEER_EXPERTS, D), f32) * 0.5
    final_g = 1.0 + 0.05 * jax.random.normal(ks[14], (D,), f32)
    return {"x": x, "c": c, "w_ada": w_ada, "b_ada": b_ada, "norm1_g": norm1_g,
            "w_in": w_in, "pool_w": pool_w, "pool_scale": pool_scale, "w_out": w_out,
            "norm2_g": norm2_g, "peer_wq": peer_wq, "peer_subkeys": peer_subkeys,
            "peer_u": peer_u, "peer_v": peer_v, "final_g": final_g}


def reference(x, c, w_ada, b_ada, norm1_g, w_in, pool_w, pool_scale, w_out,
              norm2_g, peer_wq, peer_subkeys, peer_u, peer_v, final_g):
    B, S, D = x.shape
    A = ATTN_WIDTH
    for l in range(DEPTH):
        mod = jnp.einsum('bd,de->be', jax.nn.silu(c), w_ada[l]) + b_ada[l]
        sh1, sc1, g1, sh2, sc2, g2 = jnp.split(mod[:, None, :], N_MOD, axis=-1)
        h = rmsnorm(x, norm1_g[l]) * (1.0 + sc1) + sh1
        proj = jnp.einsum('bsd,de->bse', h, w_in[l])
        q, k, v, u = jnp.split(proj, [A, 2 * A, 3 * A], axis=-1)
        attn = dilated_attention(q.reshape(B, S, N_HEADS, HEAD_DIM),
                                 k.reshape(B, S, N_HEADS, HEAD_DIM),
                                 v.reshape(B, S, N_HEADS, HEAD_DIM)).reshape(B, S, A)
        pool = multiscale_pool(u, pool_w[l], pool_scale[l])
        mixed = jnp.einsum('bse,ed->bsd', jnp.concatenate([attn, pool], axis=-1), w_out[l])
        x = x + g1 * mixed
        h2 = rmsnorm(x, norm2_g[l]) * (1.0 + sc2) + sh2
        x = x + g2 * peer_ffn(h2, peer_wq[l], peer_subkeys[l], peer_u[l], peer_v[l])
    return rmsnorm(x, final_g)
```

```python
import numpy as np
from contextlib import ExitStack
import concourse.bass as bass
import concourse.mybir as mybir
from concourse.bass_utils import run_bass_kernel_spmd

F32 = mybir.dt.float32
F16 = mybir.dt.float16
F32R = mybir.dt.float32r
BF16 = mybir.dt.bfloat16
U32 = mybir.dt.uint32
AF = mybir.ActivationFunctionType
ALU = mybir.AluOpType
AX = mybir.AxisListType

D = 1024
NH = 8
HD = 64
NEG = -1.0e30


class Sched:
    ENG = ("pe", "act", "dve", "pool", "sp")
    ENGN = {"pe": "tensor", "act": "scalar", "dve": "vector", "pool": "gpsimd", "sp": "sync"}

    def __init__(self, nc, stack):
        self.nc = nc
        self.stack = stack
        self.prog = {e: [] for e in self.ENG}
        self.cnt = {e: 0 for e in self.ENG}
        self.sem = {e: stack.enter_context(nc.semaphore("sem_" + e)) for e in self.ENG}
        self.waited = {e: {} for e in self.ENG}
        self.res = {}
        self.dsem = {}
        self.ninst = 0

    def dma_sem(self, name):
        if name not in self.dsem:
            self.dsem[name] = [self.stack.enter_context(self.nc.semaphore("d_" + name)), 0]
        return self.dsem[name]

    def _need(self, eng, tok, waits):
        if tok is None:
            return
        kind, a, b = tok
        if kind == "e" and a == eng and eng == "pe":
            return
        key = (kind, a)
        if self.waited[eng].get(key, 0) >= b:
            return
        if waits.get(key, 0) < b:
            waits[key] = b

    def _deps(self, eng, reads, writes):
        waits = {}
        for k in reads:
            r = self.res.setdefault(k, {"w": None, "r": {}})
            self._need(eng, r["w"], waits)
        for k in writes:
            r = self.res.setdefault(k, {"w": None, "r": {}})
            self._need(eng, r["w"], waits)
            for (kind, a), b in r["r"].items():
                self._need(eng, (kind, a, b), waits)
        for key, v in waits.items():
            self.waited[eng][key] = v
            self.prog[eng].append(("wait", key, v))

    def _record(self, tok, reads, writes):
        for k in reads:
            rr = self.res[k]["r"]
            key = (tok[0], tok[1])
            if rr.get(key, 0) < tok[2]:
                rr[key] = tok[2]
        for k in writes:
            self.res[k] = {"w": tok, "r": {}}

    def op(self, eng, fn, reads=(), writes=()):
        self._deps(eng, reads, writes)
        self.cnt[eng] += 1
        tok = ("e", eng, self.cnt[eng])
        self.prog[eng].append(("op", fn))
        self._record(tok, reads, writes)
        self.ninst += 1

    def dma(self, q, fn, semname, reads=(), writes=()):
        self._deps(q, reads, writes)
        s = self.dma_sem(semname)
        s[1] += 16
        tok = ("d", semname, s[1])
        self.prog[q].append(("dma", fn, semname))
        self._record(tok, reads, writes)
        self.ninst += 1

    def barrier(self):
        for e in self.ENG:
            for o in self.ENG:
                if o != e and self.cnt[o] > self.waited[e].get(("e", o), 0):
                    self.waited[e][("e", o)] = self.cnt[o]
                    self.prog[e].append(("wait", ("e", o), self.cnt[o]))
            for name, s in self.dsem.items():
                if s[1] > self.waited[e].get(("d", name), 0):
                    self.waited[e][("d", name)] = s[1]
                    self.prog[e].append(("wait", ("d", name), s[1]))

    def emit(self):
        nc = self.nc
        with nc.Block() as block:
            for e in self.ENG:
                prog = self.prog[e]

                def body(engine, prog=prog, e=e):
                    for it in prog:
                        if it[0] == "wait":
                            key, v = it[1], it[2]
                            h = self.sem[key[1]] if key[0] == "e" else self.dsem[key[1]][0]
                            engine.wait_ge(h, v)
                        elif it[0] == "op":
                            it[1](engine).then_inc(self.sem[e], 1)
                        else:
                            it[1](engine).then_inc(self.dsem[it[2]][0], 16)
                getattr(block, self.ENGN[e])(body)
        self.prog = {e: [] for e in self.ENG}

    def mm(self, out, lhsT, rhs, start, stop, reads, writes):
        self.op("pe", lambda e: e.matmul(out, lhsT=lhsT, rhs=rhs, start=start, stop=stop), reads, writes)

    def tr(self, out, in_, ident, reads, writes):
        self.op("pe", lambda e: e.transpose(out, in_, ident), reads, writes)

    def act(self, out, in_, func, reads, writes, scale=None, bias=None, accum_out=None):
        kw = {}
        if scale is not None:
            kw["scale"] = scale
        if bias is not None:
            kw["bias"] = bias
        if accum_out is not None:
            kw["accum_out"] = accum_out
        self.op("act", lambda e: e.activation(out=out, in_=in_, func=func, **kw), reads, writes)

    def tt(self, eng, out, in0, in1, op, reads, writes):
        self.op(eng, lambda e: e.tensor_tensor(out=out, in0=in0, in1=in1, op=op), reads, writes)

    def ts(self, eng, out, in0, s1, s2, op0, op1, reads, writes):
        if op1 is None:
            self.op(eng, lambda e: e.tensor_scalar(out=out, in0=in0, scalar1=s1, scalar2=None, op0=op0), reads, writes)
        else:
            self.op(eng, lambda e: e.tensor_scalar(out=out, in0=in0, scalar1=s1, scalar2=s2, op0=op0, op1=op1), reads, writes)

    def copy(self, eng, out, in_, reads, writes):
        self.op(eng, lambda e: e.tensor_copy(out=out, in_=in_), reads, writes)

    def memset(self, eng, ap, val, writes):
        self.op(eng, lambda e: e.memset(ap, val), (), writes)

    def ld(self, out, in_, sem, writes, q="sp"):
        self.dma(q, lambda e: e.dma_start(out=out, in_=in_), sem, (), writes)

    def st(self, out, in_, sem, reads, q="pool"):
        self.dma(q, lambda e: e.dma_start(out=out, in_=in_), sem, reads, ())


def _consts():
    ident = np.eye(128, dtype=np.float32)
    k = np.arange(128)[:, None, None]
    rel = np.arange(17)[None, :, None]
    q = np.arange(128)[None, None, :]
    dist = rel * 128 + q - k
    cnt = ((dist >= 0) & (dist <= 128)).astype(np.float32)
    cnt += ((dist >= 0) & (dist <= 512) & (dist % 4 == 0)).astype(np.float32)
    cnt += ((dist >= 0) & (dist <= 2048) & (dist % 16 == 0)).astype(np.float32)
    CT = np.ascontiguousarray(cnt.reshape(128, 17 * 128), dtype=np.float32)
    slopes = np.array([2.0 ** (-8.0 * (h + 1) / NH) for h in range(NH)], dtype=np.float64)
    kl = np.arange(128, dtype=np.float64)[:, None, None]
    bk = slopes[None, :, None] * (kl - 64.0) - slopes[None, :, None] * 128.0 * np.arange(17)[None, None, :]
    biasK = np.ascontiguousarray(bk.reshape(128, NH * 17), dtype=np.float32)
    bm = slopes[None, :, None] * (kl - 64.0) - slopes[None, :, None] * 128.0 * (np.arange(-3, 17)[None, None, :] + 1.5)
    biasM = np.ascontiguousarray(bm.reshape(128, NH * 20), dtype=np.float32)
    band = np.zeros((128, 4, 3, 128), dtype=np.float64)
    s = np.arange(128)[:, None]
    t = np.arange(128)[None, :]
    for pg, w in enumerate((2, 4, 8, 16)):
        cur = ((s <= t) & (s > t - w)).astype(np.float64) / w - (s == t)
        prev = ((t + 128 - s) < w).astype(np.float64) / w
        cntf = np.minimum(t + 1, w).astype(np.float64)
        first = ((s <= t) & (s > t - w)).astype(np.float64) / cntf - (s == t)
        band[:, pg, 0, :] = cur
        band[:, pg, 1, :] = prev
        band[:, pg, 2, :] = first
    band = np.ascontiguousarray(band.reshape(128, 12 * 128), dtype=np.float32)
    iota4 = np.ascontiguousarray(np.tile(np.arange(128, dtype=np.float32), (128, 8)))
    return dict(ident=ident, CT=CT, biasK=biasK, biasM=biasM, band=band, iota4=iota4)


def build(S_TOK=8192, debug=False):
    NCHK = S_TOK // 128
    NG = S_TOK // 512
    NB = S_TOK // 256
    nc = bass.Bass("TRN2", target_bir_lowering=False)
    scr_kind = "ExternalOutput" if debug else "Internal"

    def din(name, shape, dt=F32):
        return nc.dram_tensor(name, list(shape), dt, kind="ExternalInput").ap()

    def dscr(name, shape, dt=F32):
        return nc.dram_tensor(name, list(shape), dt, kind=scr_kind).ap()

    x_d = din("x", [S_TOK, D])
    cT_d = din("cT", [128, 8])
    wada_d = din("w_ada", [D, 6 * D])
    badaT_d = din("b_adaT", [128, 48])
    badabc_d = din("b_ada_bc", [128, 2 * D])
    n1g_d = din("n1g", [128, 8])
    n2g_d = din("n2g", [128, 8])
    fgbc_d = din("fg_bc", [128, D])
    win_d = din("w_in", [D, 2048], F32R)
    poolw_d = din("pool_w", [4, 128, 128], F32R)
    poolsc_d = din("pool_scT", [128, 4])
    wout_d = din("w_out", [D, D], F32R)
    wq_d = din("wq", [D, 2048])
    skT_d = din("skT", [2, 128, 128])
    UV_d = din("UV", [128, 128, 2048])
    ident_d = din("ident", [128, 128])
    CT_d = din("CT", [128, 17 * 128])
    biasK_d = din("biasK", [128, NH * 17])
    biasM_d = din("biasM", [128, NH * 20])
    band_d = din("band", [128, 12 * 128], F32R)
    iota4_d = din("iota4", [128, 1024])

    qT_s = dscr("qT_scr", [4, 128, S_TOK], F32R)
    kT_s = dscr("kT_scr", [4, 128, S_TOK], F32R)
    v_s = dscr("v_scr", [S_TOK, NH * 66], F32R)
    u_s = dscr("u_scr", [S_TOK, 512], F32R)
    attn_s = dscr("attn_scr", [S_TOK, 512])
    x1_s = dscr("x1_scr", [S_TOK, D])
    h2T_s = dscr("h2T_scr", [8, 128, S_TOK], BF16)
    UVh_s = dscr("UVh_scr", [128, 128, 2048], BF16)
    q2T_s = dscr("q2T_scr", [8, 128, S_TOK])
    bT_s = dscr("bT_scr", [128, S_TOK])
    nT_s = dscr("nT_scr", [128, S_TOK])
    tT_s = dscr("tT_scr", [128, S_TOK])
    iT_s = dscr("iT_scr", [128, S_TOK])
    y_d = nc.dram_tensor("y", [S_TOK, D], F32, kind="ExternalOutput").ap()

    with ExitStack() as top:
        S = Sched(nc, top)

        def sb(stack, name, shape, dt=F32):
            return stack.enter_context(nc.sbuf_tensor("s_" + name, list(shape), dt))

        psall = top.enter_context(nc.psum_tensor("psall", [128, 8, 512], F32))
        bank = [psall[:, i, :] for i in range(8)]
        BK = ["bank%d" % i for i in range(8)]

        ident = sb(top, "ident", [128, 128])
        scale1 = sb(top, "scale1", [128, 8])
        shift1 = sb(top, "shift1", [128, 8])
        scale2 = sb(top, "scale2", [128, 8])
        shift2 = sb(top, "shift2", [128, 8])
        g1bc = sb(top, "g1bc", [128, D])
        g2bc = sb(top, "g2bc", [128, D])
        S.ld(ident[:], ident_d, "c_ident", ["ident"])

        with ExitStack() as ph:
            cT = sb(ph, "cT", [128, 8])
            cs = sb(ph, "cs", [128, 8])
            csrep = sb(ph, "csrep", [128, 8, 128])
            badaT = sb(ph, "badaT", [128, 48])
            badabc = sb(ph, "badabc", [128, 2 * D])
            n1g = sb(ph, "n1g", [128, 8])
            n2g = sb(ph, "n2g", [128, 8])
            modc = sb(ph, "modc", [128, 48])
            wa = [sb(ph, "wa%d" % i, [128, 8, 512]) for i in range(2)]
            S.ld(cT[:], cT_d, "p1a", ["cT"])
            S.ld(badaT[:], badaT_d, "p1b", ["badaT"])
            S.ld(badabc[:], badabc_d, "p1c", ["badabc"])
            S.ld(n1g[:], n1g_d, "p1d", ["n1g"])
            S.ld(n2g[:], n2g_d, "p1e", ["n2g"])
            S.act(cs[:], cT[:], AF.Silu, ["cT"], ["cs"])
            S.copy("dve", csrep[:], cs[:].unsqueeze(2).to_broadcast([128, 8, 128]), ["cs"], ["csrep"])
            S.memset("dve", modc[:], 0.0, ["modc"])
            for n in range(12):
                w = wa[n % 2]
                wk = "wa%d" % (n % 2)
                S.ld(w[:], wada_d[:, n * 512:(n + 1) * 512].rearrange("(kc p) n -> p kc n", p=128), wk, [wk])
                if n in (4, 5, 10, 11):
                    pb = bank[n % 2]
                    for kc in range(8):
                        S.mm(pb[:, :], csrep[:, kc, :], w[:, kc, :], kc == 0, kc == 7, ["csrep", wk], [BK[n % 2]])
                    dst = g1bc if n < 6 else g2bc
                    dk = "g1bc" if n < 6 else "g2bc"
                    off = (n % 2) * 512
                    boff = off if n < 6 else D + off
                    S.tt("dve", dst[:, off:off + 512], pb[:, :], badabc[:, boff:boff + 512], ALU.add,
                         [BK[n % 2], "badabc"], [dk])
                else:
                    pb = bank[2 + n % 2]
                    for jj in range(4):
                        for kc in range(8):
                            S.mm(pb[:, jj:jj + 1], w[:, kc, jj * 128:(jj + 1) * 128], cs[:, kc:kc + 1],
                                 kc == 0, kc == 7, ["cs", wk], [BK[2 + n % 2]])
                    S.tt("dve", modc[:, 4 * n:4 * n + 4], pb[:, 0:4], badaT[:, 4 * n:4 * n + 4], ALU.add,
                         [BK[2 + n % 2], "badaT"], ["modc"])
            S.op("dve", lambda e: e.scalar_tensor_tensor(out=scale1[:], in0=modc[:, 8:16], scalar=1.0, in1=n1g[:],
                                                         op0=ALU.add, op1=ALU.mult), ["modc", "n1g"], ["scale1"])
            S.op("dve", lambda e: e.scalar_tensor_tensor(out=scale2[:], in0=modc[:, 32:40], scalar=1.0, in1=n2g[:],
                                                         op0=ALU.add, op1=ALU.mult), ["modc", "n2g"], ["scale2"])
            S.copy("dve", shift1[:], modc[:, 0:8], ["modc"], ["shift1"])
            S.copy("dve", shift2[:], modc[:, 24:32], ["modc"], ["shift2"])
            S.barrier()
            S.emit()

        def norm_transpose(xt, xk, xn, xnk, hT, hTk, ss, rstd, nch, scol, scolk, bcol, bcolk, pbanks, hT2=None, hT2k=None):
            for c in range(nch):
                S.act(xn[:, c, :], xt[:, c, :], AF.Square, [xk], [xnk], accum_out=ss[:, c:c + 1])
            S.ts("dve", rstd[:, 0:nch], ss[:, 0:nch], 1.0 / D, 1e-6, ALU.mult, ALU.add, ["ss", xnk], ["rstd"])
            S.act(rstd[:, 0:nch], rstd[:, 0:nch], AF.Sqrt, ["rstd"], ["rstd"])
            S.op("dve", lambda e: e.reciprocal(out=rstd[:, 0:nch], in_=rstd[:, 0:nch]), ["rstd"], ["rstd"])
            for c in range(nch):
                S.act(xn[:, c, :], xt[:, c, :], AF.Identity, [xk, "rstd"], [xnk], scale=rstd[:, c:c + 1])
            for kc in range(8):
                pb = pbanks[kc % 2]
                for c in range(nch):
                    S.tr(bank[pb][:, c * 128:(c + 1) * 128], xn[:, c, kc * 128:(kc + 1) * 128], ident[:],
                         [xnk, "ident"], [BK[pb]])
                S.act(hT[:, kc, 0:nch * 128], bank[pb][:, 0:nch * 128], AF.Identity, [BK[pb], scolk, bcolk], [hTk],
                      scale=scol[:, kc:kc + 1], bias=bcol[:, kc:kc + 1])
                if hT2 is not None:
                    S.act(hT2[:, kc, 0:nch * 128], bank[pb][:, 0:nch * 128], AF.Identity, [BK[pb], scolk, bcolk], [hT2k],
                          scale=scol[:, kc:kc + 1], bias=bcol[:, kc:kc + 1])

        with ExitStack() as ph:
            S.barrier()
            win = sb(ph, "win", [128, 8, 2048], F32R)
            xt = [sb(ph, "p2x%d" % i, [128, 4, D]) for i in range(2)]
            xn = sb(ph, "p2xn", [128, 4, D])
            hT = sb(ph, "p2hT", [128, 8, 512], F32R)
            ss = sb(ph, "p2ss", [128, 4])
            rstd = sb(ph, "p2rstd", [128, 4])
            qst = sb(ph, "p2q", [128, 4, 512], F32R)
            kst = sb(ph, "p2k", [128, 4, 512], F32R)
            vst = sb(ph, "p2v", [128, 4, NH, 66], F32R)
            ones_t = sb(ph, "p2ones", [128, 4 * NH * 66])
            ust = sb(ph, "p2u", [128, 4, 512], F32R)
            for i in range(4):
                S.ld(win[:, :, i * 512:(i + 1) * 512],
                     win_d[:, i * 512:(i + 1) * 512].rearrange("(kc p) n -> p kc n", p=128), "win", ["win"], q="pool")
            S.memset("dve", ones_t[:], 1.0, ["ones_t"])
            S.copy("dve", vst[:].rearrange("p c h f -> p (c h f)"), ones_t[:], ["ones_t"], ["vst"])
            xv = x_d.rearrange("(g c p) d -> g p c d", p=128, c=4)
            S.ld(xt[0][:], xv[0], "p2x0", ["p2x0"])
            for g in range(NG):
                xk = "p2x%d" % (g % 2)
                if g + 1 < NG:
                    S.ld(xt[(g + 1) % 2][:], xv[g + 1], "p2x%d" % ((g + 1) % 2), ["p2x%d" % ((g + 1) % 2)])
                norm_transpose(xt[g % 2], xk, xn, "p2xn", hT, "p2hT", ss, rstd, 4,
                               scale1, "scale1", shift1, "shift1", (0, 1))
                t0 = g * 512
                for hp in range(4):
                    pb = 2 + hp % 2
                    for kc in range(8):
                        S.mm(bank[pb][:, :], win[:, kc, hp * 128:(hp + 1) * 128], hT[:, kc, :], kc == 0, kc == 7,
                             ["win", "p2hT"], [BK[pb]])
                    S.act(qst[:, hp, :], bank[pb][:, :], AF.Copy, [BK[pb]], ["qst"], scale=0.125)
                for hp in range(4):
                    pb = 2 + hp % 2
                    for kc in range(8):
                        S.mm(bank[pb][:, :], win[:, kc, 512 + hp * 128:512 + (hp + 1) * 128], hT[:, kc, :],
                             kc == 0, kc == 7, ["win", "p2hT"], [BK[pb]])
                    S.copy("dve", kst[:, hp, :], bank[pb][:, :], [BK[pb]], ["kst"])
                for c in range(4):
                    pb = 4 + c % 2
                    for kc in range(8):
                        S.mm(bank[pb][:, :], hT[:, kc, c * 128:(c + 1) * 128], win[:, kc, 1024:1536], kc == 0, kc == 7,
                             ["win", "p2hT"], [BK[pb]])
                    S.act(vst[:, c, :, 0:64], bank[pb][:, :].rearrange("p (h f) -> p h f", f=64), AF.Copy,
                          [BK[pb]], ["vst"])
                for c in range(4):
                    pb = 6 + c % 2
                    for kc in range(8):
                        S.mm(bank[pb][:, :], hT[:, kc, c * 128:(c + 1) * 128], win[:, kc, 1536:2048], kc == 0, kc == 7,
                             ["win", "p2hT"], [BK[pb]])
                    S.copy("dve", ust[:, c, :], bank[pb][:, :], [BK[pb]], ["ust"])
                S.st(qT_s[:, :, t0:t0 + 512].rearrange("h p t -> p h t"), qst[:], "p2sq", ["qst"])
                S.st(kT_s[:, :, t0:t0 + 512].rearrange("h p t -> p h t"), kst[:], "p2sk", ["kst"])
                S.st(v_s[t0:t0 + 512, :].rearrange("(c p) f -> p c f", p=128), vst[:].rearrange("p c h f -> p c (h f)"),
                     "p2sv", ["vst"])
                S.st(u_s[t0:t0 + 512, :].rearrange("(c p) f -> p c f", p=128), ust[:], "p2su", ["ust"])
            S.barrier()
            S.emit()

        with ExitStack() as ph:
            S.barrier()
            CT = sb(ph, "CT", [128, 17 * 128])
            biasK = sb(ph, "biasK", [128, NH * 17])
            biasM = sb(ph, "biasM", [128, NH * 20])
            KT = sb(ph, "KT", [128, S_TOK], F32R)
            Vp = sb(ph, "Vp", [128, NCHK, 2, 66], F32R)
            QTg = [sb(ph, "QTg%d" % i, [128, 512], F32R) for i in range(2)]
            PT = [sb(ph, "PT%d" % i, [128, 512]) for i in range(2)]
            PTm = [[sb(ph, "PTm%d_%d" % (p_, i), [128, 512], F32R) for i in range(20)] for p_ in range(2)]
            ao = [sb(ph, "ao%d" % i, [128, 4, 128]) for i in range(2)]
            rden = sb(ph, "rden", [128, 8])
            S.ld(CT[:], CT_d, "p3c", ["CT"])
            S.ld(biasK[:], biasK_d, "p3b", ["biasK"])
            S.ld(biasM[:], biasM_d, "p3b2", ["biasM"])
            cnt3 = dict(nrd=0, nsb=0, nob=0, npt=0)
            units = [(hp, g, hh) for hp in range(4) for g in range(NG) for hh in range(2)]

            def p3_S(n):
                hp, g, hh = units[n]
                par = n % 2
                it = hp * NG + g
                qk = "QTg%d" % (it % 2)
                qt = QTg[it % 2]
                if g == 0 and hh == 0:
                    npc = max(1, S_TOK // 2048)
                    for i in range(npc):
                        w = S_TOK // npc
                        S.ld(KT[:, i * w:(i + 1) * w], kT_s[hp, :, i * w:(i + 1) * w], "p3k", ["KT"], q="pool")
                if hh == 0:
                    S.ld(qt[:], qT_s[hp, :, g * 512:(g + 1) * 512], qk, [qk], q="pool")
                h = 2 * hp + hh
                r0 = 64 * hh
                kbs = list(range(max(0, 4 * g - 16), 4 * g + 4))
                steps = []
                for kb in kbs:
                    steps.append(lambda kb=kb: s_step(kb, hp, g, hh, h, r0, par, qk, qt))
                return steps

            def s_step(kb, hp, g, hh, h, r0, par, qk, qt):
                if True:
                    kbi = kb - (4 * g - 16)
                    sbk = cnt3["nsb"] % 2
                    cnt3["nsb"] += 1
                    S.mm(bank[sbk][:, :], KT[r0:r0 + 64, kb * 128:(kb + 1) * 128], qt[r0:r0 + 64, :], True, True,
                         ["KT", qk], [BK[sbk]])
                    ptk = "PT%d" % (cnt3["npt"] % 2)
                    ptt = PT[cnt3["npt"] % 2]
                    cnt3["npt"] += 1
                    js = [j for j in range(4) if 0 <= 4 * g + j - kb <= 16]
                    j0, j1 = js[0], js[-1]
                    if h >= 2:
                        r_ = 4 * g - kb
                        S.act(ptt[:, j0 * 128:(j1 + 1) * 128], bank[sbk][:, j0 * 128:(j1 + 1) * 128], AF.Exp,
                              [BK[sbk], "biasM"], [ptk], bias=biasM[:, h * 20 + r_ + 3:h * 20 + r_ + 4])
                    else:
                        for j in js:
                            rel = 4 * g + j - kb
                            S.act(ptt[:, j * 128:(j + 1) * 128], bank[sbk][:, j * 128:(j + 1) * 128], AF.Exp,
                                  [BK[sbk], "biasK"], [ptk], bias=biasK[:, h * 17 + rel:h * 17 + rel + 1])
                    rel0 = 4 * g + j0 - kb
                    S.tt("dve", PTm[par][kbi][:, j0 * 128:(j1 + 1) * 128], ptt[:, j0 * 128:(j1 + 1) * 128],
                         CT[:, rel0 * 128:(rel0 + j1 - j0 + 1) * 128], ALU.mult, [ptk, "CT"], ["PTm%d_%d" % (par, kbi)])

            def p3_V(n):
                hp, g, hh = units[n]
                par = n % 2
                it = hp * NG + g
                aok = "ao%d" % (it % 2)
                aot = ao[it % 2]
                if g == 0 and hh == 0:
                    nvp = max(1, NCHK // 8)
                    for i in range(nvp):
                        nb_ = NCHK // nvp
                        S.ld(Vp[:, i * nb_:(i + 1) * nb_, :, :].rearrange("p b h f -> p b (h f)"),
                             v_s[i * nb_ * 128:(i + 1) * nb_ * 128, hp * 132:(hp + 1) * 132].rearrange("(b p) f -> p b f", p=128),
                             "p3v", ["Vp"], q="pool")
                kbs = list(range(max(0, 4 * g - 16), 4 * g + 4))
                steps = []
                for j in range(4):
                    steps.append(lambda j=j: v_step(j, hp, g, hh, par, aok, aot, kbs))
                return steps

            def v_step(j, hp, g, hh, par, aok, aot, kbs):
                if True:
                    obk = 2 + cnt3["nob"] % 2
                    cnt3["nob"] += 1
                    kl = [kb for kb in kbs if 0 <= 4 * g + j - kb <= 16]
                    for n_, kb in enumerate(kl):
                        kbi = kb - (4 * g - 16)
                        S.mm(bank[obk][:, 0:66], PTm[par][kbi][:, j * 128:(j + 1) * 128], Vp[:, kb, hh, :],
                             n_ == 0, n_ == len(kl) - 1, ["PTm%d_%d" % (par, kbi), "Vp"], [BK[obk]])
                    rc = cnt3["nrd"] % 8
                    cnt3["nrd"] += 1
                    S.op("dve", lambda e, obk=obk, rc=rc: e.reciprocal(out=rden[:, rc:rc + 1], in_=bank[obk][:, 64:65]),
                         [BK[obk]], ["rden%d" % rc])
                    S.act(aot[:, j, hh * 64:(hh + 1) * 64], bank[obk][:, 0:64], AF.Identity,
                          [BK[obk], "rden%d" % rc], [aok], scale=rden[:, rc:rc + 1])
                if hh == 1 and j == 3:
                    S.st(attn_s[g * 512:(g + 1) * 512, hp * 128:(hp + 1) * 128].rearrange("(c p) f -> p c f", p=128),
                         aot[:], aok, [aok], q="act")

            for st_ in p3_S(0):
                st_()
            for n in range(len(units)):
                vs = p3_V(n)
                ss_ = p3_S(n + 1) if n + 1 < len(units) else []
                ns = len(ss_)
                vi = 0
                for i_, st_ in enumerate(ss_):
                    st_()
                    while vi < 4 and (i_ + 1) * 4 >= (vi + 1) * ns:
                        vs[vi]()
                        vi += 1
                while vi < 4:
                    vs[vi]()
                    vi += 1
            S.barrier()
            S.emit()

        with ExitStack() as ph:
            S.barrier()
            wout = sb(ph, "wout", [128, 8, D], F32R)
            band = sb(ph, "band", [128, 12 * 128], F32R)
            poolw = sb(ph, "poolw", [128, 4, 128], F32R)
            poolsc = sb(ph, "poolsc", [128, 4])
            xt = [sb(ph, "p4x%d" % i, [128, 4, D]) for i in range(2)]
            at = [sb(ph, "p4a%d" % i, [128, 4, 512]) for i in range(2)]
            ub = [sb(ph, "p4u%d" % i, [128, 5, 512], F32R) for i in range(2)]
            concatT = sb(ph, "concatT", [128, 8, 512], F32R)
            mixT = sb(ph, "mixT", [128, 4, 512], F32R)
            x1t = sb(ph, "x1t", [128, 4, D])
            for i in range(2):
                S.ld(wout[:, :, i * 512:(i + 1) * 512],
                     wout_d[:, i * 512:(i + 1) * 512].rearrange("(kc p) n -> p kc n", p=128), "wout", ["wout"], q="pool")
            S.ld(band[:], band_d, "p4c1", ["band"], q="pool")
            S.ld(poolw[:], poolw_d.rearrange("g c e -> c g e"), "p4c2", ["poolw"], q="pool")
            S.ld(poolsc[:], poolsc_d, "p4c3", ["poolsc"])
            xv = x_d.rearrange("(g c p) d -> g p c d", p=128, c=4)
            av = attn_s.rearrange("(g c p) d -> g p c d", p=128, c=4)
            uv = u_s.rearrange("(g c p) d -> g p c d", p=128, c=4)

            def p4_load(g):
                i = g % 2
                S.ld(xt[i][:], xv[g], "p4x%d" % i, ["p4x%d" % i])
                S.ld(at[i][:], av[g], "p4a%d" % i, ["p4a%d" % i])
                S.ld(ub[i][:, 1:5, :], uv[g], "p4u%d" % i, ["p4u%d" % i], q="pool")
                if g > 0:
                    S.ld(ub[i][:, 0, :], u_s[g * 512 - 128:g * 512, :], "p4u%d" % i, ["p4u%d" % i], q="pool")
            p4_load(0)
            for g in range(NG):
                i = g % 2
                xk, ak, uk = "p4x%d" % i, "p4a%d" % i, "p4u%d" % i
                if g + 1 < NG:
                    p4_load(g + 1)
                for kc in range(4):
                    pb = kc % 2
                    for c in range(4):
                        S.tr(bank[pb][:, c * 128:(c + 1) * 128], at[i][:, c, kc * 128:(kc + 1) * 128], ident[:],
                             [ak, "ident"], [BK[pb]])
                    S.act(concatT[:, kc, :], bank[pb][:, :], AF.Copy, [BK[pb]], ["concatT"])
                for pg in range(4):
                    pb = 2 + pg % 2
                    for c in range(4):
                        o = bank[pb][:, c * 128:(c + 1) * 128]
                        if 4 * g + c == 0:
                            S.mm(o, ub[i][:, 1 + c, pg * 128:(pg + 1) * 128], band[:, (pg * 3 + 2) * 128:(pg * 3 + 3) * 128],
                                 True, True, [uk, "band"], [BK[pb]])
                        else:
                            S.mm(o, ub[i][:, 1 + c, pg * 128:(pg + 1) * 128], band[:, (pg * 3 + 0) * 128:(pg * 3 + 1) * 128],
                                 True, False, [uk, "band"], [BK[pb]])
                            S.mm(o, ub[i][:, c, pg * 128:(pg + 1) * 128], band[:, (pg * 3 + 1) * 128:(pg * 3 + 2) * 128],
                                 False, True, [uk, "band"], [BK[pb]])
                    S.copy("dve", mixT[:, pg, :], bank[pb][:, :], [BK[pb]], ["mixT"])
                for pg in range(4):
                    pb = 4 + pg % 2
                    S.mm(bank[pb][:, :], poolw[:, pg, :], mixT[:, pg, :], True, True, ["poolw", "mixT"], [BK[pb]])
                    S.act(concatT[:, 4 + pg, :], bank[pb][:, :], AF.Identity, [BK[pb], "poolsc"], ["concatT"],
                          scale=poolsc[:, pg:pg + 1])
                for c in range(4):
                    for n in range(2):
                        pb = 6 + n
                        for kc in range(8):
                            S.mm(bank[pb][:, :], concatT[:, kc, c * 128:(c + 1) * 128], wout[:, kc, n * 512:(n + 1) * 512],
                                 kc == 0, kc == 7, ["concatT", "wout"], [BK[pb]])
                        S.tt("dve", x1t[:, c, n * 512:(n + 1) * 512], bank[pb][:, :], g1bc[:, n * 512:(n + 1) * 512], ALU.mult,
                             [BK[pb], "g1bc"], ["x1t"])
                    S.tt("pool", x1t[:, c, :], x1t[:, c, :], xt[i][:, c, :], ALU.add, ["x1t", xk], ["x1t"])
                S.st(x1_s[g * 512:(g + 1) * 512, :].rearrange("(c p) d -> p c d", p=128), x1t[:], "p4s", ["x1t"])
            S.barrier()
            S.emit()

        with ExitStack() as ph:
            S.barrier()
            wq = sb(ph, "wq", [128, 8, 2048])
            skT = sb(ph, "skT", [128, 2, 128])
            xt = [sb(ph, "p5x%d" % i, [128, 2, D]) for i in range(2)]
            xn = sb(ph, "p5xn", [128, 2, D])
            hT = sb(ph, "p5hT", [128, 8, 256])
            hTr = sb(ph, "p5hTr", [128, 8, 256], BF16)
            ss = sb(ph, "p5ss", [128, 4])
            rstd = sb(ph, "p5rstd", [128, 4])
            QT = sb(ph, "p5QT", [128, 16, 256])
            Ssb = [[sb(ph, "Ssb%d_%d" % (a_, c_), [128, 16, 128]) for c_ in range(2)] for a_ in range(2)]
            res4 = [sb(ph, "res4_%d" % c_, [128, 4, 128]) for c_ in range(2)]
            Sm = sb(ph, "Sm", [128, 16, 128])
            sv = sb(ph, "sv", [128, 16, 16])
            si = sb(ph, "si", [128, 8, 16], U32)
            sif = sb(ph, "sif", [128, 8, 16])
            cand = sb(ph, "cand", [128, 8, 256])
            candm = Sm[:].rearrange("p a n -> p (a n)").rearrange("p (h c) -> p h c", c=256)
            ee = sb(ph, "ee", [128, 8, 256])
            c8a = sb(ph, "c8a", [128, 8, 8])
            c8b = sb(ph, "c8b", [128, 8, 8])
            Zt = sb(ph, "Zt", [128, 8])
            mz = sb(ph, "mz", [128, 8])
            b1 = sb(ph, "b1", [128, 8, 16])
            nz = sb(ph, "nz", [128, 8, 16])
            th = sb(ph, "th", [128, 8, 16])
            stg = sb(ph, "stg", [128, 4, 256])
            for i in range(4):
                S.ld(wq[:, :, i * 512:(i + 1) * 512],
                     wq_d[:, i * 512:(i + 1) * 512].rearrange("(kc p) n -> p kc n", p=128), "wq", ["wq"])
            S.ld(skT[:], skT_d.rearrange("h k n -> k h n"), "p5c", ["skT"])
            xv = x1_s.rearrange("(g c p) d -> g p c d", p=128, c=2)
            S.ld(xt[0][:], xv[0], "p5x0", ["p5x0"])
            sv4 = sv[:].rearrange("p (h two) a -> p h two a", two=2)
            sv1 = sv4[:, :, 0, :]
            sv2 = sv4[:, :, 1, :]
            def p5_A1(g):
                xk = "p5x%d" % (g % 2)
                if g + 1 < NB:
                    S.ld(xt[(g + 1) % 2][:], xv[g + 1], "p5x%d" % ((g + 1) % 2), ["p5x%d" % ((g + 1) % 2)])
                norm_transpose(xt[g % 2], xk, xn, "p5xn", hT, "p5hT", ss, rstd, 2,
                               scale2, "scale2", shift2, "shift2", (0, 1), hTr, "p5hTr")
                t0 = g * 256
                S.st(h2T_s[:, :, t0:t0 + 256].rearrange("k p t -> p k t"), hTr[:], "p5sh", ["p5hTr"])

            def p5_A2(g, half):
                for hpq in range(8 * half, 8 * half + 8):
                    pb = 2 + hpq % 2
                    for kc in range(8):
                        S.mm(bank[pb][:, 0:256], wq[:, kc, hpq * 128:(hpq + 1) * 128], hT[:, kc, :], kc == 0, kc == 7,
                             ["wq", "p5hT"], [BK[pb]])
                    S.act(QT[:, hpq, :], bank[pb][:, 0:256], AF.Copy, [BK[pb]], ["p5QT"])

            def p5_A3(g):
                t0 = g * 256
                S.st(q2T_s[:, :, t0:t0 + 256].rearrange("h p t -> p h t"),
                     QT[:].rearrange("p (h two) t -> p h two t", two=2)[:, :, 1, :], "p5sq", ["p5QT"])
                for c in range(2):
                    sk_ = "Ssb%d_%d" % (g % 2, c)
                    for b4 in range(4):
                        pb = 4 + b4
                        for k4 in range(4):
                            hpq = b4 * 4 + k4
                            S.mm(bank[pb][:, k4 * 128:(k4 + 1) * 128], QT[:, hpq, c * 128:(c + 1) * 128], skT[:, hpq % 2, :],
                                 True, True, ["p5QT", "skT"], [BK[pb]])
                        S.act(Ssb[g % 2][c][:, b4 * 4:(b4 + 1) * 4, :], bank[pb][:, :].rearrange("p (a n) -> p a n", n=128), AF.Copy,
                              [BK[pb]], [sk_])

            def p5_B1(g, c):
                sk_ = "Ssb%d_%d" % (g % 2, c)
                Sc = Ssb[g % 2][c]
                for hpq in range(16):
                    S.op("dve", lambda e, hpq=hpq: e.max(out=sv[:, hpq, 0:8], in_=Sc[:, hpq, :]), [sk_], ["sv"])
                    S.op("dve", lambda e, hpq=hpq: e.match_replace(out=Sm[:, hpq, :], in_to_replace=sv[:, hpq, 0:8],
                                                                 in_values=Sc[:, hpq, :], imm_value=NEG),
                         [sk_, "sv"], ["Sm"])
                    S.op("dve", lambda e, hpq=hpq: e.max(out=sv[:, hpq, 8:16], in_=Sm[:, hpq, :]), ["Sm"], ["sv"])
                for h in range(8):
                    S.op("dve", lambda e, h=h: e.max_index(out=si[:, h, 0:8], in_max=sv[:, 2 * h, 0:8],
                                                          in_values=Sc[:, 2 * h, :]), [sk_, "sv"], ["si"])
                    S.op("dve", lambda e, h=h: e.max_index(out=si[:, h, 8:16], in_max=sv[:, 2 * h, 8:16],
                                                          in_values=Sm[:, 2 * h, :]), ["Sm", "sv"], ["si"])
                S.copy("dve", res4[c][:, 3, :].rearrange("p (h a) -> p h a", a=16), si[:], ["si"], ["res4_%d" % c])
                S.tt("dve", cand[:].rearrange("p h (a b) -> p h a b", b=16),
                     sv1.unsqueeze(3).to_broadcast([128, 8, 16, 16]),
                     sv2.unsqueeze(2).to_broadcast([128, 8, 16, 16]), ALU.add, ["sv"], ["cand"])
                for h in range(8):
                    S.op("dve", lambda e, h=h: e.max(out=c8a[:, h, :], in_=cand[:, h, :]), ["cand"], ["c8a"])
                    S.op("dve", lambda e, h=h: e.match_replace(out=candm[:, h, :], in_to_replace=c8a[:, h, :],
                                                             in_values=cand[:, h, :], imm_value=NEG),
                         ["cand", "c8a"], ["Sm"])
                    S.op("dve", lambda e, h=h: e.max(out=c8b[:, h, :], in_=candm[:, h, :]), ["Sm"], ["c8b"])
                S.tt("dve", ee[:], cand[:], c8a[:, :, 0:1].to_broadcast([128, 8, 256]), ALU.subtract,
                     ["cand", "c8a"], ["ee"])

            def p5_B2(g, c):
                S.act(ee[:], ee[:], AF.Exp, ["ee"], ["ee"])
                S.tt("dve", candm[:], cand[:], c8b[:, :, 7:8].to_broadcast([128, 8, 256]), ALU.is_ge,
                     ["cand", "c8b"], ["Sm"])
                S.tt("dve", ee[:], ee[:], candm[:], ALU.mult, ["ee", "Sm"], ["ee"])
                S.op("dve", lambda e: e.tensor_reduce(out=Zt[:], in_=ee[:], axis=AX.X, op=ALU.add), ["ee"], ["Zt"])
                S.act(Zt[:], Zt[:], AF.Ln, ["Zt"], ["Zt"])
                S.tt("dve", mz[:], Zt[:], c8a[:, :, 0], ALU.add, ["Zt", "c8a"], ["mz"])
                rk = "res4_%d" % c
                S.copy("dve", res4[c][:, 0, :].rearrange("p (h a) -> p h a", a=16), sv1, ["sv"], [rk])
                S.ts("dve", res4[c][:, 1, :].rearrange("p (h a) -> p h a", a=16),
                     mz[:].unsqueeze(2).to_broadcast([128, 8, 16]), -1.0, None, ALU.mult, None, ["mz"], [rk])
                S.copy("dve", res4[c][:, 2, :].rearrange("p (h a) -> p h a", a=16),
                       c8b[:, :, 7:8].to_broadcast([128, 8, 16]), ["c8b"], [rk])

            def p5_C(g):
                t0 = g * 256
                for c in range(2):
                    for n_ in range(4):
                        pb = n_ % 2
                        S.tr(bank[pb][:, 0:128], res4[c][:, n_, :], ident[:], ["res4_%d" % c, "ident"], [BK[pb]])
                        S.act(stg[:, n_, c * 128:(c + 1) * 128], bank[pb][:, 0:128], AF.Copy, [BK[pb]], ["stg"])
                S.st(bT_s[:, t0:t0 + 256], stg[:, 0, :], "p5s1", ["stg"])
                S.st(nT_s[:, t0:t0 + 256], stg[:, 1, :], "p5s4", ["stg"])
                S.st(tT_s[:, t0:t0 + 256], stg[:, 2, :], "p5s2", ["stg"])
                S.st(iT_s[:, t0:t0 + 256], stg[:, 3, :], "p5s3", ["stg"])

            cin = [sb(ph, "cin%d" % i, [128, 1024]) for i in range(2)]
            cout = [sb(ph, "cout%d" % i, [128, 1024], BF16) for i in range(2)]
            conv_state = [0]

            def conv_some(n_items):
                for _ in range(n_items):
                    i = conv_state[0]
                    if i >= 256:
                        return
                    conv_state[0] += 1
                    k2 = i % 2
                    hf = (i % 2) * 1024
                    S.ld(cin[k2][:], UV_d[i // 2][:, hf:hf + 1024], "cin%d" % k2, ["cin%d" % k2])
                    S.act(cout[k2][:], cin[k2][:], AF.Copy, ["cin%d" % k2], ["cout%d" % k2])
                    S.st(UVh_s[i // 2][:, hf:hf + 1024], cout[k2][:], "cout%d" % k2, ["cout%d" % k2], q="act")

            per_blk = (256 + NB - 1) // NB
            p5_A1(0)
            p5_A2(0, 0)
            p5_A2(0, 1)
            p5_A3(0)
            for g in range(NB):
                nx = g + 1 < NB
                if nx:
                    p5_A1(g + 1)
                p5_B1(g, 0)
                if nx:
                    p5_A2(g + 1, 0)
                conv_some(per_blk)
                p5_B2(g, 0)
                p5_B1(g, 1)
                if nx:
                    p5_A2(g + 1, 1)
                    p5_A3(g + 1)
                p5_B2(g, 1)
                p5_C(g)
            conv_some(256)
            S.barrier()
            S.emit()

        with ExitStack() as ph:
            S.barrier()
            skT2 = sb(ph, "skT2", [128, 128])
            iota8 = sb(ph, "iota8", [128, 8, 128])
            fgbc = sb(ph, "fgbc", [128, D])
            hT = [sb(ph, "bhT%d" % i, [128, 8, 256], BF16) for i in range(2)]
            q2 = [sb(ph, "bq2%d" % i, [128, 8, 256]) for i in range(2)]
            bti = [sb(ph, "bbti%d" % i, [128, 4, 256]) for i in range(2)]
            x1b = [sb(ph, "bx1%d" % i, [128, 2, D]) for i in range(2)]
            Q2rep = [sb(ph, "Q2rep%d" % i, [128, 16, 128]) for i in range(2)]
            Xt = [sb(ph, "Xt%d" % i, [128, 8, 128]) for i in range(2)]
            Et = [sb(ph, "Et%d" % i, [128, 8, 128]) for i in range(2)]
            Lp = [sb(ph, "Lp%d" % i, [128, 8, 128]) for i in range(2)]
            WT = sb(ph, "WT", [128, 256, 128], F16)
            NUV = 6
            UVb = [sb(ph, "UVb%d" % i, [128, 2048], BF16) for i in range(NUV)]
            Gt = [sb(ph, "Gt%d" % i, [128, 256]) for i in range(2)]
            At = [sb(ph, "At%d" % i, [128, 256], BF16) for i in range(2)]
            x2 = sb(ph, "x2", [128, 2, D])
            ss = sb(ph, "bss", [128, 2])
            rstd = sb(ph, "brstd", [128, 2])
            junk = sb(ph, "bjunk", [128, D], BF16)
            S.ld(skT2[:], skT_d[1], "b_c1", ["skT2"])
            S.ld(iota8[:].rearrange("p a n -> p (a n)"), iota4_d, "b_c2", ["iota8"])
            S.ld(fgbc[:], fgbc_d, "b_c3", ["fgbc"])

            def b_load(bb):
                i = bb % 2
                t0 = bb * 256
                S.ld(hT[i][:], h2T_s[:, :, t0:t0 + 256].rearrange("k p t -> p k t"), "bhT%d" % i, ["bhT%d" % i])
                S.ld(q2[i][:], q2T_s[:, :, t0:t0 + 256].rearrange("h p t -> p h t"), "bq2%d" % i, ["bq2%d" % i])
                S.ld(bti[i][:, 0, :], bT_s[:, t0:t0 + 256], "bbti%d" % i, ["bbti%d" % i])
                S.ld(bti[i][:, 1, :], nT_s[:, t0:t0 + 256], "bbti%d" % i, ["bbti%d" % i])
                S.ld(bti[i][:, 2, :], tT_s[:, t0:t0 + 256], "bbti%d" % i, ["bbti%d" % i])
                S.ld(bti[i][:, 3, :], iT_s[:, t0:t0 + 256], "bbti%d" % i, ["bbti%d" % i])
                S.ld(x1b[i][:], x1_s[t0:t0 + 256, :].rearrange("(c p) d -> p c d", p=128), "bx1%d" % i, ["bx1%d" % i])

            nuv_ctr = [0]

            def uv_load(i):
                s_ = nuv_ctr[0] % NUV
                nuv_ctr[0] += 1
                S.ld(UVb[s_][:], UVh_s[i], "UVb%d" % s_, ["UVb%d" % s_])
                return s_

            b_load(0)
            ngrp = 0
            for bb in range(NB):
                bi = bb % 2
                hk, qk, btk, x1k = "bhT%d" % bi, "bq2%d" % bi, "bbti%d" % bi, "bx1%d" % bi
                if bb + 1 < NB:
                    b_load(bb + 1)
                pend = [uv_load(i_) for i_ in range(NUV)]
                def emit_q2rep(pr):
                    tq = pr * 16
                    qr = Q2rep[pr % 2]
                    qrk = "Q2rep%d" % (pr % 2)
                    S.act(qr[:].rearrange("k t (h a) -> k t h a", a=16),
                          q2[bi][:, :, tq:tq + 16].rearrange("k h t -> k t h").unsqueeze(3).to_broadcast([128, 16, 8, 16]),
                          AF.Copy, [qk], [qrk])

                def emit_s2rep(grp):
                    qr = Q2rep[(grp // 2) % 2]
                    qrk = "Q2rep%d" % ((grp // 2) % 2)
                    rb = 2 * (grp % 2)
                    for tt_ in range(8):
                        S.mm(bank[rb + tt_ // 4][:, (tt_ % 4) * 128:(tt_ % 4 + 1) * 128], qr[:, (grp % 2) * 8 + tt_, :], skT2[:],
                             True, True, [qrk, "skT2"], [BK[rb + tt_ // 4]])

                def emit_gate(grp):
                    tq = grp * 8
                    u = grp % 2
                    rb = 2 * u
                    wb = 4 + 2 * u
                    xk, ek, lk = "Xt%d" % u, "Et%d" % u, "Lp%d" % u
                    r3 = psall[:, rb:rb + 2, :].rearrange("p b (a n) -> p (b a) n", n=128)
                    rks = [BK[rb], BK[rb + 1]]

                    def bc(row):
                        return bti[bi][:, row, tq:tq + 8].unsqueeze(2).to_broadcast([128, 8, 128])
                    S.tt("dve", Xt[u][:], r3, bc(0), ALU.add, rks + [btk], [xk])
                    S.tt("dve", Et[u][:], Xt[u][:], bc(2), ALU.is_lt, [xk, btk], [ek])
                    S.op("dve", lambda e: e.scalar_tensor_tensor(out=Et[u][:], in0=Et[u][:], scalar=-1.0e4, in1=Xt[u][:],
                                                                 op0=ALU.mult, op1=ALU.add), [ek, xk], [ek])
                    for tt_ in range(8):
                        S.act(Et[u][:, tt_, :], Et[u][:, tt_, :], AF.Exp, [ek, btk], [ek],
                              bias=bti[bi][:, 1, tq + tt_:tq + tt_ + 1])
                    S.tt("dve", Lp[u][:], iota8[:], bc(3), ALU.is_equal, ["iota8", btk], [lk])
                    for tt_ in range(8):
                        S.mm(bank[wb + tt_ // 4][:, (tt_ % 4) * 128:(tt_ % 4 + 1) * 128], Et[u][:, tt_, :], Lp[u][:, tt_, :],
                             True, True, [ek, lk], [BK[wb + tt_ // 4]])

                def emit_gate_back(grp):
                    tq = grp * 8
                    wb = 4 + 2 * (grp % 2)
                    S.act(WT[:, tq:tq + 8, :], psall[:, wb:wb + 2, :].rearrange("p b (a n) -> p (b a) n", n=128), AF.Copy,
                          [BK[wb], BK[wb + 1]], ["WT"])

                emit_q2rep(0)
                emit_s2rep(0)
                emit_q2rep(1)
                for grp in range(32):
                    if grp + 1 < 32:
                        emit_s2rep(grp + 1)
                    if grp % 2 == 1 and grp // 2 + 2 < 16:
                        emit_q2rep(grp // 2 + 2)
                    emit_gate(grp)
                    if grp >= 1:
                        emit_gate_back(grp - 1)
                emit_gate_back(31)
                slot = {}

                def emit_A(i):
                    slot[i] = pend.pop(0)
                    s_ = slot[i]
                    ab = i % 2
                    for kc in range(8):
                        S.mm(bank[ab][:, 0:256], UVb[s_][:, kc * 128:(kc + 1) * 128], hT[bi][:, kc, :], kc == 0, kc == 7,
                             ["UVb%d" % s_, hk], [BK[ab]])

                def emit_V(i):
                    s_ = slot[i]
                    ab = i % 2
                    S.act(Gt[ab][:], bank[ab][:, 0:256], AF.Gelu, [BK[ab]], ["Gt%d" % ab])
                    S.tt("dve", At[ab][:], Gt[ab][:], WT[:, :, i], ALU.mult, ["Gt%d" % ab, "WT"], ["At%d" % ab])
                    for c in range(2):
                        for n in range(2):
                            yb = 4 + c * 2 + n
                            S.mm(bank[yb][:, :], At[ab][:, c * 128:(c + 1) * 128], UVb[s_][:, D + n * 512:D + (n + 1) * 512],
                                 i == 0, i == 127, ["At%d" % ab, "UVb%d" % s_], [BK[yb]])
                    if i + NUV < 128:
                        pend.append(uv_load(i + NUV))

                emit_A(0)
                for i in range(128):
                    if i + 1 < 128:
                        emit_A(i + 1)
                    emit_V(i)
                for c in range(2):
                    for n in range(2):
                        yb = 4 + c * 2 + n
                        S.tt("dve", x2[:, c, n * 512:(n + 1) * 512], bank[yb][:, :], g2bc[:, n * 512:(n + 1) * 512], ALU.mult,
                             [BK[yb], "g2bc"], ["x2"])
                    S.tt("pool", x2[:, c, :], x2[:, c, :], x1b[bi][:, c, :], ALU.add, ["x2", x1k], ["x2"])
                    S.act(junk[:], x2[:, c, :], AF.Square, ["x2"], ["bjunk"], accum_out=ss[:, c:c + 1])
                S.ts("dve", rstd[:], ss[:], 1.0 / D, 1e-6, ALU.mult, ALU.add, ["bss", "bjunk"], ["brstd"])
                S.act(rstd[:], rstd[:], AF.Sqrt, ["brstd"], ["brstd"])
                S.op("dve", lambda e: e.reciprocal(out=rstd[:], in_=rstd[:]), ["brstd"], ["brstd"])
                for c in range(2):
                    S.act(x2[:, c, :], x2[:, c, :], AF.Identity, ["x2", "brstd"], ["x2"], scale=rstd[:, c:c + 1])
                    S.tt("pool", x2[:, c, :], x2[:, c, :], fgbc[:], ALU.mult, ["x2", "fgbc"], ["x2"])
                S.st(y_d[bb * 256:(bb + 1) * 256, :].rearrange("(c p) d -> p c d", p=128), x2[:], "yout", ["x2"])
            S.barrier()
            S.emit()
        print("bass instructions:", S.ninst)
    return nc


def make_in_maps(inputs, S_TOK=8192, n_cores=8):
    f = lambda a: np.ascontiguousarray(a, dtype=np.float32)
    cst = _consts()
    w_ada = f(inputs["w_ada"][0])
    b_ada = inputs["b_ada"][0]
    shared = dict(
        w_ada=w_ada,
        b_adaT=f(b_ada.reshape(48, 128).T),
        b_ada_bc=f(np.broadcast_to(np.concatenate([b_ada[2048:3072], b_ada[5120:6144]])[None, :], (128, 2048))),
        n1g=f(inputs["norm1_g"][0].reshape(8, 128).T),
        n2g=f(inputs["norm2_g"][0].reshape(8, 128).T),
        fg_bc=f(np.broadcast_to(inputs["final_g"][None, :], (128, D))),
        w_in=f(inputs["w_in"][0]),
        pool_w=f(inputs["pool_w"][0]),
        pool_scT=f(inputs["pool_scale"][0].reshape(4, 128).T),
        w_out=f(inputs["w_out"][0]),
        wq=f(inputs["peer_wq"][0]),
        skT=f(np.transpose(inputs["peer_subkeys"][0], (0, 2, 1))),
        UV=f(np.concatenate([np.transpose(inputs["peer_u"][0].reshape(128, 128, 8, 128), (0, 3, 2, 1)).reshape(128, 128, 1024),
                             inputs["peer_v"][0].reshape(128, 128, 1024)], axis=2)),
        **cst,
    )
    maps = []
    for b in range(n_cores):
        m = dict(shared)
        m["x"] = f(inputs["x"][b, :S_TOK])
        m["cT"] = f(inputs["c"][b].reshape(8, 128).T)
        maps.append(m)
    return maps


def kernel(**inputs):
    nc = build(8192)
    in_maps = make_in_maps(inputs, 8192, 8)
    res = run_bass_kernel_spmd(nc, in_maps, core_ids=list(range(8)))
    return np.stack([np.asarray(r["y"], dtype=np.float32) for r in res.results], axis=0)
```

```python
import numpy as np
from contextlib import ExitStack
import concourse.bass as bass
import concourse.mybir as mybir
from concourse.bass_utils import run_bass_kernel_spmd

F32 = mybir.dt.float32
F16 = mybir.dt.float16
F32R = mybir.dt.float32r
BF16 = mybir.dt.bfloat16
U32 = mybir.dt.uint32
AF = mybir.ActivationFunctionType
ALU = mybir.AluOpType
AX = mybir.AxisListType

D = 1024
NH = 8
HD = 64
NEG = -1.0e30


class Sched:
    ENG = ("pe", "act", "dve", "pool", "sp")
    ENGN = {"pe": "tensor", "act": "scalar", "dve": "vector", "pool": "gpsimd", "sp": "sync"}

    def __init__(self, nc, stack):
        self.nc = nc
        self.stack = stack
        self.prog = {e: [] for e in self.ENG}
        self.cnt = {e: 0 for e in self.ENG}
        self.sem = {e: stack.enter_context(nc.semaphore("sem_" + e)) for e in self.ENG}
        self.waited = {e: {} for e in self.ENG}
        self.res = {}
        self.dsem = {}
        self.ninst = 0

    def dma_sem(self, name):
        if name not in self.dsem:
            self.dsem[name] = [self.stack.enter_context(self.nc.semaphore("d_" + name)), 0]
        return self.dsem[name]

    def _need(self, eng, tok, waits):
        if tok is None:
            return
        kind, a, b = tok
        if kind == "e" and a == eng and eng == "pe":
            return
        key = (kind, a)
        if self.waited[eng].get(key, 0) >= b:
            return
        if waits.get(key, 0) < b:
            waits[key] = b

    def _deps(self, eng, reads, writes):
        waits = {}
        for k in reads:
            r = self.res.setdefault(k, {"w": None, "r": {}})
            self._need(eng, r["w"], waits)
        for k in writes:
            r = self.res.setdefault(k, {"w": None, "r": {}})
            self._need(eng, r["w"], waits)
            for (kind, a), b in r["r"].items():
                self._need(eng, (kind, a, b), waits)
        for key, v in waits.items():
            self.waited[eng][key] = v
            self.prog[eng].append(("wait", key, v))

    def _record(self, tok, reads, writes):
        for k in reads:
            rr = self.res[k]["r"]
            key = (tok[0], tok[1])
            if rr.get(key, 0) < tok[2]:
                rr[key] = tok[2]
        for k in writes:
            self.res[k] = {"w": tok, "r": {}}

    def op(self, eng, fn, reads=(), writes=()):
        self._deps(eng, reads, writes)
        self.cnt[eng] += 1
        tok = ("e", eng, self.cnt[eng])
        self.prog[eng].append(("op", fn))
        self._record(tok, reads, writes)
        self.ninst += 1

    def dma(self, q, fn, semname, reads=(), writes=()):
        self._deps(q, reads, writes)
        s = self.dma_sem(semname)
        s[1] += 16
        tok = ("d", semname, s[1])
        self.prog[q].append(("dma", fn, semname))
        self._record(tok, reads, writes)
        self.ninst += 1

    def barrier(self):
        for e in self.ENG:
            for o in self.ENG:
                if o != e and self.cnt[o] > self.waited[e].get(("e", o), 0):
                    self.waited[e][("e", o)] = self.cnt[o]
                    self.prog[e].append(("wait", ("e", o), self.cnt[o]))
            for name, s in self.dsem.items():
                if s[1] > self.waited[e].get(("d", name), 0):
                    self.waited[e][("d", name)] = s[1]
                    self.prog[e].append(("wait", ("d", name), s[1]))

    def emit(self):
        nc = self.nc
        with nc.Block() as block:
            for e in self.ENG:
                prog = self.prog[e]

                def body(engine, prog=prog, e=e):
                    for it in prog:
                        if it[0] == "wait":
                            key, v = it[1], it[2]
                            h = self.sem[key[1]] if key[0] == "e" else self.dsem[key[1]][0]
                            engine.wait_ge(h, v)
                        elif it[0] == "op":
                            it[1](engine).then_inc(self.sem[e], 1)
                        else:
                            it[1](engine).then_inc(self.dsem[it[2]][0], 16)
                getattr(block, self.ENGN[e])(body)
        self.prog = {e: [] for e in self.ENG}

    def mm(self, out, lhsT, rhs, start, stop, reads, writes):
        self.op("pe", lambda e: e.matmul(out, lhsT=lhsT, rhs=rhs, start=start, stop=stop), reads, writes)

    def tr(self, out, in_, ident, reads, writes):
        self.op("pe", lambda e: e.transpose(out, in_, ident), reads, writes)

    def act(self, out, in_, func, reads, writes, scale=None, bias=None, accum_out=None):
        kw = {}
        if scale is not None:
            kw["scale"] = scale
        if bias is not None:
            kw["bias"] = bias
        if accum_out is not None:
            kw["accum_out"] = accum_out
        self.op("act", lambda e: e.activation(out=out, in_=in_, func=func, **kw), reads, writes)

    def tt(self, eng, out, in0, in1, op, reads, writes):
        self.op(eng, lambda e: e.tensor_tensor(out=out, in0=in0, in1=in1, op=op), reads, writes)

    def ts(self, eng, out, in0, s1, s2, op0, op1, reads, writes):
        if op1 is None:
            self.op(eng, lambda e: e.tensor_scalar(out=out, in0=in0, scalar1=s1, scalar2=None, op0=op0), reads, writes)
        else:
            self.op(eng, lambda e: e.tensor_scalar(out=out, in0=in0, scalar1=s1, scalar2=s2, op0=op0, op1=op1), reads, writes)

    def copy(self, eng, out, in_, reads, writes):
        self.op(eng, lambda e: e.tensor_copy(out=out, in_=in_), reads, writes)

    def memset(self, eng, ap, val, writes):
        self.op(eng, lambda e: e.memset(ap, val), (), writes)

    def ld(self, out, in_, sem, writes, q="sp"):
        self.dma(q, lambda e: e.dma_start(out=out, in_=in_), sem, (), writes)

    def st(self, out, in_, sem, reads, q="pool"):
        self.dma(q, lambda e: e.dma_start(out=out, in_=in_), sem, reads, ())


def _consts():
    ident = np.eye(128, dtype=np.float32)
    k = np.arange(128)[:, None, None]
    rel = np.arange(17)[None, :, None]
    q = np.arange(128)[None, None, :]
    dist = rel * 128 + q - k
    cnt = ((dist >= 0) & (dist <= 128)).astype(np.float32)
    cnt += ((dist >= 0) & (dist <= 512) & (dist % 4 == 0)).astype(np.float32)
    cnt += ((dist >= 0) & (dist <= 2048) & (dist % 16 == 0)).astype(np.float32)
    CT = np.ascontiguousarray(cnt.reshape(128, 17 * 128), dtype=np.float32)
    slopes = np.array([2.0 ** (-8.0 * (h + 1) / NH) for h in range(NH)], dtype=np.float64)
    kl = np.arange(128, dtype=np.float64)[:, None, None]
    bk = slopes[None, :, None] * (kl - 64.0) - slopes[None, :, None] * 128.0 * np.arange(17)[None, None, :]
    biasK = np.ascontiguousarray(bk.reshape(128, NH * 17), dtype=np.float32)
    bm = slopes[None, :, None] * (kl - 64.0) - slopes[None, :, None] * 128.0 * (np.arange(-3, 17)[None, None, :] + 1.5)
    biasM = np.ascontiguousarray(bm.reshape(128, NH * 20), dtype=np.float32)
    band = np.zeros((128, 4, 3, 128), dtype=np.float64)
    s = np.arange(128)[:, None]
    t = np.arange(128)[None, :]
    for pg, w in enumerate((2, 4, 8, 16)):
        cur = ((s <= t) & (s > t - w)).astype(np.float64) / w - (s == t)
        prev = ((t + 128 - s) < w).astype(np.float64) / w
        cntf = np.minimum(t + 1, w).astype(np.float64)
        first = ((s <= t) & (s > t - w)).astype(np.float64) / cntf - (s == t)
        band[:, pg, 0, :] = cur
        band[:, pg, 1, :] = prev
        band[:, pg, 2, :] = first
    band = np.ascontiguousarray(band.reshape(128, 12 * 128), dtype=np.float32)
    iota4 = np.ascontiguousarray(np.tile(np.arange(128, dtype=np.float32), (128, 8)))
    return dict(ident=ident, CT=CT, biasK=biasK, biasM=biasM, band=band, iota4=iota4)


def build(S_TOK=8192, debug=False):
    NCHK = S_TOK // 128
    NG = S_TOK // 512
    NB = S_TOK // 256
    nc = bass.Bass("TRN2", target_bir_lowering=False)
    scr_kind = "ExternalOutput" if debug else "Internal"

    def din(name, shape, dt=F32):
        return nc.dram_tensor(name, list(shape), dt, kind="ExternalInput").ap()

    def dscr(name, shape, dt=F32):
        return nc.dram_tensor(name, list(shape), dt, kind=scr_kind).ap()

    x_d = din("x", [S_TOK, D])
    cT_d = din("cT", [128, 8])
    wada_d = din("w_ada", [D, 6 * D])
    badaT_d = din("b_adaT", [128, 48])
    badabc_d = din("b_ada_bc", [128, 2 * D])
    n1g_d = din("n1g", [128, 8])
    n2g_d = din("n2g", [128, 8])
    fgbc_d = din("fg_bc", [128, D])
    win_d = din("w_in", [D, 2048], F32R)
    poolw_d = din("pool_w", [4, 128, 128], F32R)
    poolsc_d = din("pool_scT", [128, 4])
    wout_d = din("w_out", [D, D], F32R)
    wq_d = din("wq", [D, 2048])
    skT_d = din("skT", [2, 128, 128])
    UV_d = din("UV", [128, 128, 2048])
    ident_d = din("ident", [128, 128])
    CT_d = din("CT", [128, 17 * 128])
    biasK_d = din("biasK", [128, NH * 17])
    biasM_d = din("biasM", [128, NH * 20])
    band_d = din("band", [128, 12 * 128], F32R)
    iota4_d = din("iota4", [128, 1024])

    qT_s = dscr("qT_scr", [4, 128, S_TOK], F32R)
    kT_s = dscr("kT_scr", [4, 128, S_TOK], F32R)
    v_s = dscr("v_scr", [S_TOK, NH * 66], BF16)
    u_s = dscr("u_scr", [S_TOK, 512], F32R)
    attn_s = dscr("attn_scr", [S_TOK, 512])
    x1_s = dscr("x1_scr", [S_TOK, D])
    h2T_s = dscr("h2T_scr", [8, 128, S_TOK], BF16)
    UVh_s = dscr("UVh_scr", [128, 128, 2048], BF16)
    q2T_s = dscr("q2T_scr", [8, 128, S_TOK])
    bT_s = dscr("bT_scr", [128, S_TOK])
    nT_s = dscr("nT_scr", [128, S_TOK])
    tT_s = dscr("tT_scr", [128, S_TOK])
    iT_s = dscr("iT_scr", [128, S_TOK])
    y_d = nc.dram_tensor("y", [S_TOK, D], F32, kind="ExternalOutput").ap()

    with ExitStack() as top:
        S = Sched(nc, top)

        def sb(stack, name, shape, dt=F32):
            return stack.enter_context(nc.sbuf_tensor("s_" + name, list(shape), dt))

        psall = top.enter_context(nc.psum_tensor("psall", [128, 8, 512], F32))
        bank = [psall[:, i, :] for i in range(8)]
        BK = ["bank%d" % i for i in range(8)]

        ident = sb(top, "ident", [128, 128])
        scale1 = sb(top, "scale1", [128, 8])
        shift1 = sb(top, "shift1", [128, 8])
        scale2 = sb(top, "scale2", [128, 8])
        shift2 = sb(top, "shift2", [128, 8])
        g1bc = sb(top, "g1bc", [128, D])
        g2bc = sb(top, "g2bc", [128, D])
        S.ld(ident[:], ident_d, "c_ident", ["ident"])

        with ExitStack() as ph:
            cT = sb(ph, "cT", [128, 8])
            cs = sb(ph, "cs", [128, 8])
            csrep = sb(ph, "csrep", [128, 8, 128])
            badaT = sb(ph, "badaT", [128, 48])
            badabc = sb(ph, "badabc", [128, 2 * D])
            n1g = sb(ph, "n1g", [128, 8])
            n2g = sb(ph, "n2g", [128, 8])
            modc = sb(ph, "modc", [128, 48])
            wa = [sb(ph, "wa%d" % i, [128, 8, 512]) for i in range(2)]
            S.ld(cT[:], cT_d, "p1a", ["cT"])
            S.ld(badaT[:], badaT_d, "p1b", ["badaT"])
            S.ld(badabc[:], badabc_d, "p1c", ["badabc"])
            S.ld(n1g[:], n1g_d, "p1d", ["n1g"])
            S.ld(n2g[:], n2g_d, "p1e", ["n2g"])
            S.act(cs[:], cT[:], AF.Silu, ["cT"], ["cs"])
            S.copy("dve", csrep[:], cs[:].unsqueeze(2).to_broadcast([128, 8, 128]), ["cs"], ["csrep"])
            S.memset("dve", modc[:], 0.0, ["modc"])
            for n in range(12):
                w = wa[n % 2]
                wk = "wa%d" % (n % 2)
                S.ld(w[:], wada_d[:, n * 512:(n + 1) * 512].rearrange("(kc p) n -> p kc n", p=128), wk, [wk])
                if n in (4, 5, 10, 11):
                    pb = bank[n % 2]
                    for kc in range(8):
                        S.mm(pb[:, :], csrep[:, kc, :], w[:, kc, :], kc == 0, kc == 7, ["csrep", wk], [BK[n % 2]])
                    dst = g1bc if n < 6 else g2bc
                    dk = "g1bc" if n < 6 else "g2bc"
                    off = (n % 2) * 512
                    boff = off if n < 6 else D + off
                    S.tt("dve", dst[:, off:off + 512], pb[:, :], badabc[:, boff:boff + 512], ALU.add,
                         [BK[n % 2], "badabc"], [dk])
                else:
                    pb = bank[2 + n % 2]
                    for jj in range(4):
                        for kc in range(8):
                            S.mm(pb[:, jj:jj + 1], w[:, kc, jj * 128:(jj + 1) * 128], cs[:, kc:kc + 1],
                                 kc == 0, kc == 7, ["cs", wk], [BK[2 + n % 2]])
                    S.tt("dve", modc[:, 4 * n:4 * n + 4], pb[:, 0:4], badaT[:, 4 * n:4 * n + 4], ALU.add,
                         [BK[2 + n % 2], "badaT"], ["modc"])
            S.op("dve", lambda e: e.scalar_tensor_tensor(out=scale1[:], in0=modc[:, 8:16], scalar=1.0, in1=n1g[:],
                                                         op0=ALU.add, op1=ALU.mult), ["modc", "n1g"], ["scale1"])
            S.op("dve", lambda e: e.scalar_tensor_tensor(out=scale2[:], in0=modc[:, 32:40], scalar=1.0, in1=n2g[:],
                                                         op0=ALU.add, op1=ALU.mult), ["modc", "n2g"], ["scale2"])
            S.copy("dve", shift1[:], modc[:, 0:8], ["modc"], ["shift1"])
            S.copy("dve", shift2[:], modc[:, 24:32], ["modc"], ["shift2"])
            S.barrier()
            S.emit()

        def norm_transpose(xt, xk, xn, xnk, hT, hTk, ss, rstd, nch, scol, scolk, bcol, bcolk, pbanks, hT2=None, hT2k=None):
            for c in range(nch):
                S.act(xn[:, c, :], xt[:, c, :], AF.Square, [xk], [xnk], accum_out=ss[:, c:c + 1])
            S.ts("dve", rstd[:, 0:nch], ss[:, 0:nch], 1.0 / D, 1e-6, ALU.mult, ALU.add, ["ss", xnk], ["rstd"])
            S.act(rstd[:, 0:nch], rstd[:, 0:nch], AF.Sqrt, ["rstd"], ["rstd"])
            S.op("dve", lambda e: e.reciprocal(out=rstd[:, 0:nch], in_=rstd[:, 0:nch]), ["rstd"], ["rstd"])
            for c in range(nch):
                S.act(xn[:, c, :], xt[:, c, :], AF.Identity, [xk, "rstd"], [xnk], scale=rstd[:, c:c + 1])
            for kc in range(8):
                pb = pbanks[kc % 2]
                for c in range(nch):
                    S.tr(bank[pb][:, c * 128:(c + 1) * 128], xn[:, c, kc * 128:(kc + 1) * 128], ident[:],
                         [xnk, "ident"], [BK[pb]])
                S.act(hT[:, kc, 0:nch * 128], bank[pb][:, 0:nch * 128], AF.Identity, [BK[pb], scolk, bcolk], [hTk],
                      scale=scol[:, kc:kc + 1], bias=bcol[:, kc:kc + 1])
                if hT2 is not None:
                    S.act(hT2[:, kc, 0:nch * 128], bank[pb][:, 0:nch * 128], AF.Identity, [BK[pb], scolk, bcolk], [hT2k],
                          scale=scol[:, kc:kc + 1], bias=bcol[:, kc:kc + 1])

        with ExitStack() as ph:
            S.barrier()
            win = sb(ph, "win", [128, 8, 2048], F32R)
            xt = [sb(ph, "p2x%d" % i, [128, 4, D]) for i in range(2)]
            xn = sb(ph, "p2xn", [128, 4, D])
            hT = sb(ph, "p2hT", [128, 8, 512], F32R)
            ss = sb(ph, "p2ss", [128, 4])
            rstd = sb(ph, "p2rstd", [128, 4])
            qst = sb(ph, "p2q", [128, 4, 512], F32R)
            kst = sb(ph, "p2k", [128, 4, 512], F32R)
            vst = sb(ph, "p2v", [128, 4, NH, 66], BF16)
            ones_t = sb(ph, "p2ones", [128, 4 * NH * 66])
            ust = sb(ph, "p2u", [128, 4, 512], F32R)
            for i in range(4):
                S.ld(win[:, :, i * 512:(i + 1) * 512],
                     win_d[:, i * 512:(i + 1) * 512].rearrange("(kc p) n -> p kc n", p=128), "win", ["win"], q="pool")
            S.memset("dve", ones_t[:], 1.0, ["ones_t"])
            S.copy("dve", vst[:].rearrange("p c h f -> p (c h f)"), ones_t[:], ["ones_t"], ["vst"])
            xv = x_d.rearrange("(g c p) d -> g p c d", p=128, c=4)
            S.ld(xt[0][:], xv[0], "p2x0", ["p2x0"])
            for g in range(NG):
                xk = "p2x%d" % (g % 2)
                if g + 1 < NG:
                    S.ld(xt[(g + 1) % 2][:], xv[g + 1], "p2x%d" % ((g + 1) % 2), ["p2x%d" % ((g + 1) % 2)])
                norm_transpose(xt[g % 2], xk, xn, "p2xn", hT, "p2hT", ss, rstd, 4,
                               scale1, "scale1", shift1, "shift1", (0, 1))
                t0 = g * 512
                for hp in range(4):
                    pb = 2 + hp % 2
                    for kc in range(8):
                        S.mm(bank[pb][:, :], win[:, kc, hp * 128:(hp + 1) * 128], hT[:, kc, :], kc == 0, kc == 7,
                             ["win", "p2hT"], [BK[pb]])
                    S.act(qst[:, hp, :], bank[pb][:, :], AF.Copy, [BK[pb]], ["qst"], scale=0.125)
                for hp in range(4):
                    pb = 2 + hp % 2
                    for kc in range(8):
                        S.mm(bank[pb][:, :], win[:, kc, 512 + hp * 128:512 + (hp + 1) * 128], hT[:, kc, :],
                             kc == 0, kc == 7, ["win", "p2hT"], [BK[pb]])
                    S.copy("dve", kst[:, hp, :], bank[pb][:, :], [BK[pb]], ["kst"])
                for c in range(4):
                    pb = 4 + c % 2
                    for kc in range(8):
                        S.mm(bank[pb][:, :], hT[:, kc, c * 128:(c + 1) * 128], win[:, kc, 1024:1536], kc == 0, kc == 7,
                             ["win", "p2hT"], [BK[pb]])
                    S.act(vst[:, c, :, 0:64], bank[pb][:, :].rearrange("p (h f) -> p h f", f=64), AF.Copy,
                          [BK[pb]], ["vst"])
                for c in range(4):
                    pb = 6 + c % 2
                    for kc in range(8):
                        S.mm(bank[pb][:, :], hT[:, kc, c * 128:(c + 1) * 128], win[:, kc, 1536:2048], kc == 0, kc == 7,
                             ["win", "p2hT"], [BK[pb]])
                    S.copy("dve", ust[:, c, :], bank[pb][:, :], [BK[pb]], ["ust"])
                S.st(qT_s[:, :, t0:t0 + 512].rearrange("h p t -> p h t"), qst[:], "p2sq", ["qst"])
                S.st(kT_s[:, :, t0:t0 + 512].rearrange("h p t -> p h t"), kst[:], "p2sk", ["kst"])
                S.st(v_s[t0:t0 + 512, :].rearrange("(c p) f -> p c f", p=128), vst[:].rearrange("p c h f -> p c (h f)"),
                     "p2sv", ["vst"])
                S.st(u_s[t0:t0 + 512, :].rearrange("(c p) f -> p c f", p=128), ust[:], "p2su", ["ust"])
            S.barrier()
            S.emit()

        with ExitStack() as ph:
            S.barrier()
            CT = sb(ph, "CT", [128, 17 * 128])
            biasK = sb(ph, "biasK", [128, NH * 17])
            biasM = sb(ph, "biasM", [128, NH * 20])
            KT = sb(ph, "KT", [128, S_TOK], F32R)
            Vp = sb(ph, "Vp", [128, NCHK, 2, 66], BF16)
            QTg = [sb(ph, "QTg%d" % i, [128, 512], F32R) for i in range(2)]
            PT = [sb(ph, "PT%d" % i, [128, 512]) for i in range(2)]
            PTm = [[sb(ph, "PTm%d_%d" % (p_, i), [128, 512], BF16) for i in range(20)] for p_ in range(2)]
            ao = [sb(ph, "ao%d" % i, [128, 4, 128]) for i in range(2)]
            rden = sb(ph, "rden", [128, 8])
            S.ld(CT[:], CT_d, "p3c", ["CT"])
            S.ld(biasK[:], biasK_d, "p3b", ["biasK"])
            S.ld(biasM[:], biasM_d, "p3b2", ["biasM"])
            cnt3 = dict(nrd=0, nsb=0, nob=0, npt=0)
            units = [(hp, g, hh) for hp in range(4) for g in range(NG) for hh in range(2)]

            def p3_S(n):
                hp, g, hh = units[n]
                par = n % 2
                it = hp * NG + g
                qk = "QTg%d" % (it % 2)
                qt = QTg[it % 2]
                if g == 0 and hh == 0:
                    npc = max(1, S_TOK // 2048)
                    for i in range(npc):
                        w = S_TOK // npc
                        S.ld(KT[:, i * w:(i + 1) * w], kT_s[hp, :, i * w:(i + 1) * w], "p3k", ["KT"], q="pool")
                if hh == 0:
                    S.ld(qt[:], qT_s[hp, :, g * 512:(g + 1) * 512], qk, [qk], q="pool")
                h = 2 * hp + hh
                r0 = 64 * hh
                kbs = list(range(max(0, 4 * g - 16), 4 * g + 4))
                steps = []
                for kb in kbs:
                    steps.append(lambda kb=kb: s_step(kb, hp, g, hh, h, r0, par, qk, qt))
                return steps

            def s_step(kb, hp, g, hh, h, r0, par, qk, qt):
                if True:
                    kbi = kb - (4 * g - 16)
                    sbk = cnt3["nsb"] % 2
                    cnt3["nsb"] += 1
                    S.mm(bank[sbk][:, :], KT[r0:r0 + 64, kb * 128:(kb + 1) * 128], qt[r0:r0 + 64, :], True, True,
                         ["KT", qk], [BK[sbk]])
                    ptk = "PT%d" % (cnt3["npt"] % 2)
                    ptt = PT[cnt3["npt"] % 2]
                    cnt3["npt"] += 1
                    js = [j for j in range(4) if 0 <= 4 * g + j - kb <= 16]
                    j0, j1 = js[0], js[-1]
                    if h >= 2:
                        r_ = 4 * g - kb
                        S.act(ptt[:, j0 * 128:(j1 + 1) * 128], bank[sbk][:, j0 * 128:(j1 + 1) * 128], AF.Exp,
                              [BK[sbk], "biasM"], [ptk], bias=biasM[:, h * 20 + r_ + 3:h * 20 + r_ + 4])
                    else:
                        for j in js:
                            rel = 4 * g + j - kb
                            S.act(ptt[:, j * 128:(j + 1) * 128], bank[sbk][:, j * 128:(j + 1) * 128], AF.Exp,
                                  [BK[sbk], "biasK"], [ptk], bias=biasK[:, h * 17 + rel:h * 17 + rel + 1])
                    rel0 = 4 * g + j0 - kb
                    S.tt("dve", PTm[par][kbi][:, j0 * 128:(j1 + 1) * 128], ptt[:, j0 * 128:(j1 + 1) * 128],
                         CT[:, rel0 * 128:(rel0 + j1 - j0 + 1) * 128], ALU.mult, [ptk, "CT"], ["PTm%d_%d" % (par, kbi)])

            def p3_V(n):
                hp, g, hh = units[n]
                par = n % 2
                it = hp * NG + g
                aok = "ao%d" % (it % 2)
                aot = ao[it % 2]
                if g == 0 and hh == 0:
                    nvp = max(1, NCHK // 8)
                    for i in range(nvp):
                        nb_ = NCHK // nvp
                        S.ld(Vp[:, i * nb_:(i + 1) * nb_, :, :].rearrange("p b h f -> p b (h f)"),
                             v_s[i * nb_ * 128:(i + 1) * nb_ * 128, hp * 132:(hp + 1) * 132].rearrange("(b p) f -> p b f", p=128),
                             "p3v", ["Vp"], q="sp")
                kbs = list(range(max(0, 4 * g - 16), 4 * g + 4))
                steps = []
                for j in range(4):
                    steps.append(lambda j=j: v_step(j, hp, g, hh, par, aok, aot, kbs))
                return steps

            def v_step(j, hp, g, hh, par, aok, aot, kbs):
                if True:
                    obk = 2 + cnt3["nob"] % 2
                    cnt3["nob"] += 1
                    kl = [kb for kb in kbs if 0 <= 4 * g + j - kb <= 16]
                    for n_, kb in enumerate(kl):
                        kbi = kb - (4 * g - 16)
                        S.mm(bank[obk][:, 0:66], PTm[par][kbi][:, j * 128:(j + 1) * 128], Vp[:, kb, hh, :],
                             n_ == 0, n_ == len(kl) - 1, ["PTm%d_%d" % (par, kbi), "Vp"], [BK[obk]])
                    rc = cnt3["nrd"] % 8
                    cnt3["nrd"] += 1
                    S.op("dve", lambda e, obk=obk, rc=rc: e.reciprocal(out=rden[:, rc:rc + 1], in_=bank[obk][:, 64:65]),
                         [BK[obk]], ["rden%d" % rc])
                    S.act(aot[:, j, hh * 64:(hh + 1) * 64], bank[obk][:, 0:64], AF.Identity,
                          [BK[obk], "rden%d" % rc], [aok], scale=rden[:, rc:rc + 1])
                if hh == 1 and j == 3:
                    S.st(attn_s[g * 512:(g + 1) * 512, hp * 128:(hp + 1) * 128].rearrange("(c p) f -> p c f", p=128),
                         aot[:], aok, [aok], q="act")

            for st_ in p3_S(0):
                st_()
            for n in range(len(units)):
                vs = p3_V(n)
                ss_ = p3_S(n + 1) if n + 1 < len(units) else []
                ns = len(ss_)
                vi = 0
                for i_, st_ in enumerate(ss_):
                    st_()
                    while vi < 4 and (i_ + 1) * 4 >= (vi + 1) * ns:
                        vs[vi]()
                        vi += 1
                while vi < 4:
                    vs[vi]()
                    vi += 1
            S.barrier()
            S.emit()

        with ExitStack() as ph:
            S.barrier()
            wout = sb(ph, "wout", [128, 8, D], F32R)
            band = sb(ph, "band", [128, 12 * 128], F32R)
            poolw = sb(ph, "poolw", [128, 4, 128], F32R)
            poolsc = sb(ph, "poolsc", [128, 4])
            xt = [sb(ph, "p4x%d" % i, [128, 4, D]) for i in range(2)]
            at = [sb(ph, "p4a%d" % i, [128, 4, 512]) for i in range(2)]
            ub = [sb(ph, "p4u%d" % i, [128, 5, 512], F32R) for i in range(2)]
            concatT = sb(ph, "concatT", [128, 8, 512], F32R)
            mixT = sb(ph, "mixT", [128, 4, 512], F32R)
            x1t = sb(ph, "x1t", [128, 4, D])
            for i in range(2):
                S.ld(wout[:, :, i * 512:(i + 1) * 512],
                     wout_d[:, i * 512:(i + 1) * 512].rearrange("(kc p) n -> p kc n", p=128), "wout", ["wout"], q="pool")
            S.ld(band[:], band_d, "p4c1", ["band"], q="pool")
            S.ld(poolw[:], poolw_d.rearrange("g c e -> c g e"), "p4c2", ["poolw"], q="pool")
            S.ld(poolsc[:], poolsc_d, "p4c3", ["poolsc"])
            xv = x_d.rearrange("(g c p) d -> g p c d", p=128, c=4)
            av = attn_s.rearrange("(g c p) d -> g p c d", p=128, c=4)
            uv = u_s.rearrange("(g c p) d -> g p c d", p=128, c=4)

            def p4_load(g):
                i = g % 2
                S.ld(xt[i][:], xv[g], "p4x%d" % i, ["p4x%d" % i])
                S.ld(at[i][:], av[g], "p4a%d" % i, ["p4a%d" % i])
                S.ld(ub[i][:, 1:5, :], uv[g], "p4u%d" % i, ["p4u%d" % i], q="pool")
                if g > 0:
                    S.ld(ub[i][:, 0, :], u_s[g * 512 - 128:g * 512, :], "p4u%d" % i, ["p4u%d" % i], q="pool")
            p4_load(0)
            for g in range(NG):
                i = g % 2
                xk, ak, uk = "p4x%d" % i, "p4a%d" % i, "p4u%d" % i
                if g + 1 < NG:
                    p4_load(g + 1)
                for kc in range(4):
                    pb = kc % 2
                    for c in range(4):
                        S.tr(bank[pb][:, c * 128:(c + 1) * 128], at[i][:, c, kc * 128:(kc + 1) * 128], ident[:],
                             [ak, "ident"], [BK[pb]])
                    S.act(concatT[:, kc, :], bank[pb][:, :], AF.Copy, [BK[pb]], ["concatT"])
                for pg in range(4):
                    pb = 2 + pg % 2
                    for c in range(4):
                        o = bank[pb][:, c * 128:(c + 1) * 128]
                        if 4 * g + c == 0:
                            S.mm(o, ub[i][:, 1 + c, pg * 128:(pg + 1) * 128], band[:, (pg * 3 + 2) * 128:(pg * 3 + 3) * 128],
                                 True, True, [uk, "band"], [BK[pb]])
                        else:
                            S.mm(o, ub[i][:, 1 + c, pg * 128:(pg + 1) * 128], band[:, (pg * 3 + 0) * 128:(pg * 3 + 1) * 128],
                                 True, False, [uk, "band"], [BK[pb]])
                            S.mm(o, ub[i][:, c, pg * 128:(pg + 1) * 128], band[:, (pg * 3 + 1) * 128:(pg * 3 + 2) * 128],
                                 False, True, [uk, "band"], [BK[pb]])
                    S.copy("dve", mixT[:, pg, :], bank[pb][:, :], [BK[pb]], ["mixT"])
                for pg in range(4):
                    pb = 4 + pg % 2
                    S.mm(bank[pb][:, :], poolw[:, pg, :], mixT[:, pg, :], True, True, ["poolw", "mixT"], [BK[pb]])
                    S.act(concatT[:, 4 + pg, :], bank[pb][:, :], AF.Identity, [BK[pb], "poolsc"], ["concatT"],
                          scale=poolsc[:, pg:pg + 1])
                for c in range(4):
                    for n in range(2):
                        pb = 6 + n
                        for kc in range(8):
                            S.mm(bank[pb][:, :], concatT[:, kc, c * 128:(c + 1) * 128], wout[:, kc, n * 512:(n + 1) * 512],
                                 kc == 0, kc == 7, ["concatT", "wout"], [BK[pb]])
                        S.tt("dve", x1t[:, c, n * 512:(n + 1) * 512], bank[pb][:, :], g1bc[:, n * 512:(n + 1) * 512], ALU.mult,
                             [BK[pb], "g1bc"], ["x1t"])
                    S.tt("pool", x1t[:, c, :], x1t[:, c, :], xt[i][:, c, :], ALU.add, ["x1t", xk], ["x1t"])
                S.st(x1_s[g * 512:(g + 1) * 512, :].rearrange("(c p) d -> p c d", p=128), x1t[:], "p4s", ["x1t"])
            S.barrier()
            S.emit()

        with ExitStack() as ph:
            S.barrier()
            wq = sb(ph, "wq", [128, 8, 2048])
            skT = sb(ph, "skT", [128, 2, 128])
            xt = [sb(ph, "p5x%d" % i, [128, 2, D]) for i in range(2)]
            xn = sb(ph, "p5xn", [128, 2, D])
            hT = sb(ph, "p5hT", [128, 8, 256])
            hTr = sb(ph, "p5hTr", [128, 8, 256], BF16)
            ss = sb(ph, "p5ss", [128, 4])
            rstd = sb(ph, "p5rstd", [128, 4])
            QT = sb(ph, "p5QT", [128, 16, 256])
            Ssb = [[sb(ph, "Ssb%d_%d" % (a_, c_), [128, 16, 128]) for c_ in range(2)] for a_ in range(2)]
            res4 = [sb(ph, "res4_%d" % c_, [128, 4, 128]) for c_ in range(2)]
            Sm = sb(ph, "Sm", [128, 16, 128])
            sv = sb(ph, "sv", [128, 16, 16])
            si = sb(ph, "si", [128, 8, 16], U32)
            sif = sb(ph, "sif", [128, 8, 16])
            cand = sb(ph, "cand", [128, 8, 256])
            candm = Sm[:].rearrange("p a n -> p (a n)").rearrange("p (h c) -> p h c", c=256)
            ee = sb(ph, "ee", [128, 8, 256])
            c8a = sb(ph, "c8a", [128, 8, 8])
            c8b = sb(ph, "c8b", [128, 8, 8])
            Zt = sb(ph, "Zt", [128, 8])
            mz = sb(ph, "mz", [128, 8])
            b1 = sb(ph, "b1", [128, 8, 16])
            nz = sb(ph, "nz", [128, 8, 16])
            th = sb(ph, "th", [128, 8, 16])
            stg = sb(ph, "stg", [128, 4, 256])
            for i in range(4):
                S.ld(wq[:, :, i * 512:(i + 1) * 512],
                     wq_d[:, i * 512:(i + 1) * 512].rearrange("(kc p) n -> p kc n", p=128), "wq", ["wq"])
            S.ld(skT[:], skT_d.rearrange("h k n -> k h n"), "p5c", ["skT"])
            xv = x1_s.rearrange("(g c p) d -> g p c d", p=128, c=2)
            S.ld(xt[0][:], xv[0], "p5x0", ["p5x0"])
            sv4 = sv[:].rearrange("p (h two) a -> p h two a", two=2)
            sv1 = sv4[:, :, 0, :]
            sv2 = sv4[:, :, 1, :]
            def p5_A1(g):
                xk = "p5x%d" % (g % 2)
                if g + 1 < NB:
                    S.ld(xt[(g + 1) % 2][:], xv[g + 1], "p5x%d" % ((g + 1) % 2), ["p5x%d" % ((g + 1) % 2)])
                norm_transpose(xt[g % 2], xk, xn, "p5xn", hT, "p5hT", ss, rstd, 2,
                               scale2, "scale2", shift2, "shift2", (0, 1), hTr, "p5hTr")
                t0 = g * 256
                S.st(h2T_s[:, :, t0:t0 + 256].rearrange("k p t -> p k t"), hTr[:], "p5sh", ["p5hTr"])

            def p5_A2(g, half):
                for hpq in range(8 * half, 8 * half + 8):
                    pb = 2 + hpq % 2
                    for kc in range(8):
                        S.mm(bank[pb][:, 0:256], wq[:, kc, hpq * 128:(hpq + 1) * 128], hT[:, kc, :], kc == 0, kc == 7,
                             ["wq", "p5hT"], [BK[pb]])
                    S.act(QT[:, hpq, :], bank[pb][:, 0:256], AF.Copy, [BK[pb]], ["p5QT"])

            def p5_A3(g):
                t0 = g * 256
                S.st(q2T_s[:, :, t0:t0 + 256].rearrange("h p t -> p h t"),
                     QT[:].rearrange("p (h two) t -> p h two t", two=2)[:, :, 1, :], "p5sq", ["p5QT"])
                for c in range(2):
                    sk_ = "Ssb%d_%d" % (g % 2, c)
                    for b4 in range(4):
                        pb = 4 + b4
                        for k4 in range(4):
                            hpq = b4 * 4 + k4
                            S.mm(bank[pb][:, k4 * 128:(k4 + 1) * 128], QT[:, hpq, c * 128:(c + 1) * 128], skT[:, hpq % 2, :],
                                 True, True, ["p5QT", "skT"], [BK[pb]])
                        S.act(Ssb[g % 2][c][:, b4 * 4:(b4 + 1) * 4, :], bank[pb][:, :].rearrange("p (a n) -> p a n", n=128), AF.Copy,
                              [BK[pb]], [sk_])

            def p5_B1(g, c):
                sk_ = "Ssb%d_%d" % (g % 2, c)
                Sc = Ssb[g % 2][c]
                for hpq in range(16):
                    S.op("dve", lambda e, hpq=hpq: e.max(out=sv[:, hpq, 0:8], in_=Sc[:, hpq, :]), [sk_], ["sv"])
                    S.op("dve", lambda e, hpq=hpq: e.match_replace(out=Sm[:, hpq, :], in_to_replace=sv[:, hpq, 0:8],
                                                                 in_values=Sc[:, hpq, :], imm_value=NEG),
                         [sk_, "sv"], ["Sm"])
                    S.op("dve", lambda e, hpq=hpq: e.max(out=sv[:, hpq, 8:16], in_=Sm[:, hpq, :]), ["Sm"], ["sv"])
                for h in range(8):
                    S.op("dve", lambda e, h=h: e.max_index(out=si[:, h, 0:8], in_max=sv[:, 2 * h, 0:8],
                                                          in_values=Sc[:, 2 * h, :]), [sk_, "sv"], ["si"])
                    S.op("dve", lambda e, h=h: e.max_index(out=si[:, h, 8:16], in_max=sv[:, 2 * h, 8:16],
                                                          in_values=Sm[:, 2 * h, :]), ["Sm", "sv"], ["si"])
                S.copy("dve", res4[c][:, 3, :].rearrange("p (h a) -> p h a", a=16), si[:], ["si"], ["res4_%d" % c])
                S.tt("dve", cand[:].rearrange("p h (a b) -> p h a b", b=16),
                     sv1.unsqueeze(3).to_broadcast([128, 8, 16, 16]),
                     sv2.unsqueeze(2).to_broadcast([128, 8, 16, 16]), ALU.add, ["sv"], ["cand"])
                for h in range(8):
                    S.op("dve", lambda e, h=h: e.max(out=c8a[:, h, :], in_=cand[:, h, :]), ["cand"], ["c8a"])
                    S.op("dve", lambda e, h=h: e.match_replace(out=candm[:, h, :], in_to_replace=c8a[:, h, :],
                                                             in_values=cand[:, h, :], imm_value=NEG),
                         ["cand", "c8a"], ["Sm"])
                    S.op("dve", lambda e, h=h: e.max(out=c8b[:, h, :], in_=candm[:, h, :]), ["Sm"], ["c8b"])
                S.tt("dve", ee[:], cand[:], c8a[:, :, 0:1].to_broadcast([128, 8, 256]), ALU.subtract,
                     ["cand", "c8a"], ["ee"])

            def p5_B2(g, c):
                S.act(ee[:], ee[:], AF.Exp, ["ee"], ["ee"])
                S.tt("dve", candm[:], cand[:], c8b[:, :, 7:8].to_broadcast([128, 8, 256]), ALU.is_ge,
                     ["cand", "c8b"], ["Sm"])
                S.tt("dve", ee[:], ee[:], candm[:], ALU.mult, ["ee", "Sm"], ["ee"])
                S.op("dve", lambda e: e.tensor_reduce(out=Zt[:], in_=ee[:], axis=AX.X, op=ALU.add), ["ee"], ["Zt"])
                S.act(Zt[:], Zt[:], AF.Ln, ["Zt"], ["Zt"])
                S.tt("dve", mz[:], Zt[:], c8a[:, :, 0], ALU.add, ["Zt", "c8a"], ["mz"])
                rk = "res4_%d" % c
                S.copy("dve", res4[c][:, 0, :].rearrange("p (h a) -> p h a", a=16), sv1, ["sv"], [rk])
                S.ts("dve", res4[c][:, 1, :].rearrange("p (h a) -> p h a", a=16),
                     mz[:].unsqueeze(2).to_broadcast([128, 8, 16]), -1.0, None, ALU.mult, None, ["mz"], [rk])
                S.copy("dve", res4[c][:, 2, :].rearrange("p (h a) -> p h a", a=16),
                       c8b[:, :, 7:8].to_broadcast([128, 8, 16]), ["c8b"], [rk])

            def p5_C(g):
                t0 = g * 256
                for c in range(2):
                    for n_ in range(4):
                        pb = n_ % 2
                        S.tr(bank[pb][:, 0:128], res4[c][:, n_, :], ident[:], ["res4_%d" % c, "ident"], [BK[pb]])
                        S.act(stg[:, n_, c * 128:(c + 1) * 128], bank[pb][:, 0:128], AF.Copy, [BK[pb]], ["stg"])
                S.st(bT_s[:, t0:t0 + 256], stg[:, 0, :], "p5s1", ["stg"])
                S.st(nT_s[:, t0:t0 + 256], stg[:, 1, :], "p5s4", ["stg"])
                S.st(tT_s[:, t0:t0 + 256], stg[:, 2, :], "p5s2", ["stg"])
                S.st(iT_s[:, t0:t0 + 256], stg[:, 3, :], "p5s3", ["stg"])

            cin = [sb(ph, "cin%d" % i, [128, 1024]) for i in range(2)]
            cout = [sb(ph, "cout%d" % i, [128, 1024], BF16) for i in range(2)]
            conv_state = [0]

            def conv_some(n_items):
                for _ in range(n_items):
                    i = conv_state[0]
                    if i >= 256:
                        return
                    conv_state[0] += 1
                    k2 = i % 2
                    hf = (i % 2) * 1024
                    S.ld(cin[k2][:], UV_d[i // 2][:, hf:hf + 1024], "cin%d" % k2, ["cin%d" % k2])
                    S.act(cout[k2][:], cin[k2][:], AF.Copy, ["cin%d" % k2], ["cout%d" % k2])
                    S.st(UVh_s[i // 2][:, hf:hf + 1024], cout[k2][:], "cout%d" % k2, ["cout%d" % k2], q="act")

            per_blk = (256 + NB - 1) // NB
            p5_A1(0)
            p5_A2(0, 0)
            p5_A2(0, 1)
            p5_A3(0)
            for g in range(NB):
                nx = g + 1 < NB
                if nx:
                    p5_A1(g + 1)
                p5_B1(g, 0)
                if nx:
                    p5_A2(g + 1, 0)
                conv_some(per_blk)
                p5_B2(g, 0)
                p5_B1(g, 1)
                if nx:
                    p5_A2(g + 1, 1)
                    p5_A3(g + 1)
                p5_B2(g, 1)
                p5_C(g)
            conv_some(256)
            S.barrier()
            S.emit()

        with ExitStack() as ph:
            S.barrier()
            skT2 = sb(ph, "skT2", [128, 128])
            iota8 = sb(ph, "iota8", [128, 8, 128])
            fgbc = sb(ph, "fgbc", [128, D])
            hT = [sb(ph, "bhT%d" % i, [128, 8, 256], BF16) for i in range(2)]
            q2 = [sb(ph, "bq2%d" % i, [128, 8, 256]) for i in range(2)]
            bti = [sb(ph, "bbti%d" % i, [128, 4, 256]) for i in range(2)]
            x1b = [sb(ph, "bx1%d" % i, [128, 2, D]) for i in range(2)]
            Q2rep = [sb(ph, "Q2rep%d" % i, [128, 16, 128]) for i in range(2)]
            Xt = [sb(ph, "Xt%d" % i, [128, 8, 128]) for i in range(2)]
            Et = [sb(ph, "Et%d" % i, [128, 8, 128]) for i in range(2)]
            Lp = [sb(ph, "Lp%d" % i, [128, 8, 128]) for i in range(2)]
            WT = sb(ph, "WT", [128, 256, 128], F16)
            NUV = 6
            UVb = [sb(ph, "UVb%d" % i, [128, 2048], BF16) for i in range(NUV)]
            Gt = [sb(ph, "Gt%d" % i, [128, 256]) for i in range(2)]
            At = [sb(ph, "At%d" % i, [128, 256], BF16) for i in range(2)]
            x2 = sb(ph, "x2", [128, 2, D])
            ss = sb(ph, "bss", [128, 2])
            rstd = sb(ph, "brstd", [128, 2])
            junk = sb(ph, "bjunk", [128, D], BF16)
            S.ld(skT2[:], skT_d[1], "b_c1", ["skT2"])
            S.ld(iota8[:].rearrange("p a n -> p (a n)"), iota4_d, "b_c2", ["iota8"])
            S.ld(fgbc[:], fgbc_d, "b_c3", ["fgbc"])

            def b_load(bb):
                i = bb % 2
                t0 = bb * 256
                S.ld(hT[i][:], h2T_s[:, :, t0:t0 + 256].rearrange("k p t -> p k t"), "bhT%d" % i, ["bhT%d" % i])
                S.ld(q2[i][:], q2T_s[:, :, t0:t0 + 256].rearrange("h p t -> p h t"), "bq2%d" % i, ["bq2%d" % i])
                S.ld(bti[i][:, 0, :], bT_s[:, t0:t0 + 256], "bbti%d" % i, ["bbti%d" % i])
                S.ld(bti[i][:, 1, :], nT_s[:, t0:t0 + 256], "bbti%d" % i, ["bbti%d" % i])
                S.ld(bti[i][:, 2, :], tT_s[:, t0:t0 + 256], "bbti%d" % i, ["bbti%d" % i])
                S.ld(bti[i][:, 3, :], iT_s[:, t0:t0 + 256], "bbti%d" % i, ["bbti%d" % i])
                S.ld(x1b[i][:], x1_s[t0:t0 + 256, :].rearrange("(c p) d -> p c d", p=128), "bx1%d" % i, ["bx1%d" % i])

            nuv_ctr = [0]

            def uv_load(i):
                s_ = nuv_ctr[0] % NUV
                nuv_ctr[0] += 1
                S.ld(UVb[s_][:], UVh_s[i], "UVb%d" % s_, ["UVb%d" % s_])
                return s_

            b_load(0)
            ngrp = 0
            for bb in range(NB):
                bi = bb % 2
                hk, qk, btk, x1k = "bhT%d" % bi, "bq2%d" % bi, "bbti%d" % bi, "bx1%d" % bi
                if bb + 1 < NB:
                    b_load(bb + 1)
                pend = [uv_load(i_) for i_ in range(NUV)]
                def emit_q2rep(pr):
                    tq = pr * 16
                    qr = Q2rep[pr % 2]
                    qrk = "Q2rep%d" % (pr % 2)
                    S.act(qr[:].rearrange("k t (h a) -> k t h a", a=16),
                          q2[bi][:, :, tq:tq + 16].rearrange("k h t -> k t h").unsqueeze(3).to_broadcast([128, 16, 8, 16]),
                          AF.Copy, [qk], [qrk])

                def emit_s2rep(grp):
                    qr = Q2rep[(grp // 2) % 2]
                    qrk = "Q2rep%d" % ((grp // 2) % 2)
                    rb = 2 * (grp % 2)
                    for tt_ in range(8):
                        S.mm(bank[rb + tt_ // 4][:, (tt_ % 4) * 128:(tt_ % 4 + 1) * 128], qr[:, (grp % 2) * 8 + tt_, :], skT2[:],
                             True, True, [qrk, "skT2"], [BK[rb + tt_ // 4]])

                def emit_gate(grp):
                    tq = grp * 8
                    u = grp % 2
                    rb = 2 * u
                    wb = 4 + 2 * u
                    xk, ek, lk = "Xt%d" % u, "Et%d" % u, "Lp%d" % u
                    r3 = psall[:, rb:rb + 2, :].rearrange("p b (a n) -> p (b a) n", n=128)
                    rks = [BK[rb], BK[rb + 1]]

                    def bc(row):
                        return bti[bi][:, row, tq:tq + 8].unsqueeze(2).to_broadcast([128, 8, 128])
                    S.tt("dve", Xt[u][:], r3, bc(0), ALU.add, rks + [btk], [xk])
                    S.tt("dve", Et[u][:], Xt[u][:], bc(2), ALU.is_lt, [xk, btk], [ek])
                    S.op("dve", lambda e: e.scalar_tensor_tensor(out=Et[u][:], in0=Et[u][:], scalar=-1.0e4, in1=Xt[u][:],
                                                                 op0=ALU.mult, op1=ALU.add), [ek, xk], [ek])
                    for tt_ in range(8):
                        S.act(Et[u][:, tt_, :], Et[u][:, tt_, :], AF.Exp, [ek, btk], [ek],
                              bias=bti[bi][:, 1, tq + tt_:tq + tt_ + 1])
                    S.tt("dve", Lp[u][:], iota8[:], bc(3), ALU.is_equal, ["iota8", btk], [lk])
                    for tt_ in range(8):
                        S.mm(bank[wb + tt_ // 4][:, (tt_ % 4) * 128:(tt_ % 4 + 1) * 128], Et[u][:, tt_, :], Lp[u][:, tt_, :],
                             True, True, [ek, lk], [BK[wb + tt_ // 4]])

                def emit_gate_back(grp):
                    tq = grp * 8
                    wb = 4 + 2 * (grp % 2)
                    S.act(WT[:, tq:tq + 8, :], psall[:, wb:wb + 2, :].rearrange("p b (a n) -> p (b a) n", n=128), AF.Copy,
                          [BK[wb], BK[wb + 1]], ["WT"])

                emit_q2rep(0)
                emit_s2rep(0)
                emit_q2rep(1)
                for grp in range(32):
                    if grp + 1 < 32:
                        emit_s2rep(grp + 1)
                    if grp % 2 == 1 and grp // 2 + 2 < 16:
                        emit_q2rep(grp // 2 + 2)
                    emit_gate(grp)
                    if grp >= 1:
                        emit_gate_back(grp - 1)
                emit_gate_back(31)
                slot = {}

                def emit_A(i):
                    slot[i] = pend.pop(0)
                    s_ = slot[i]
                    ab = i % 2
                    for kc in range(8):
                        S.mm(bank[ab][:, 0:256], UVb[s_][:, kc * 128:(kc + 1) * 128], hT[bi][:, kc, :], kc == 0, kc == 7,
                             ["UVb%d" % s_, hk], [BK[ab]])

                def emit_V(i):
                    s_ = slot[i]
                    ab = i % 2
                    S.act(Gt[ab][:], bank[ab][:, 0:256], AF.Gelu, [BK[ab]], ["Gt%d" % ab])
                    S.tt("dve", At[ab][:], Gt[ab][:], WT[:, :, i], ALU.mult, ["Gt%d" % ab, "WT"], ["At%d" % ab])
                    for c in range(2):
                        for n in range(2):
                            yb = 4 + c * 2 + n
                            S.mm(bank[yb][:, :], At[ab][:, c * 128:(c + 1) * 128], UVb[s_][:, D + n * 512:D + (n + 1) * 512],
                                 i == 0, i == 127, ["At%d" % ab, "UVb%d" % s_], [BK[yb]])
                    if i + NUV < 128:
                        pend.append(uv_load(i + NUV))

                emit_A(0)
                for i in range(128):
                    if i + 1 < 128:
                        emit_A(i + 1)
                    emit_V(i)
                for c in range(2):
                    for n in range(2):
                        yb = 4 + c * 2 + n
                        S.tt("dve", x2[:, c, n * 512:(n + 1) * 512], bank[yb][:, :], g2bc[:, n * 512:(n + 1) * 512], ALU.mult,
                             [BK[yb], "g2bc"], ["x2"])
                    S.tt("pool", x2[:, c, :], x2[:, c, :], x1b[bi][:, c, :], ALU.add, ["x2", x1k], ["x2"])
                    S.act(junk[:], x2[:, c, :], AF.Square, ["x2"], ["bjunk"], accum_out=ss[:, c:c + 1])
                S.ts("dve", rstd[:], ss[:], 1.0 / D, 1e-6, ALU.mult, ALU.add, ["bss", "bjunk"], ["brstd"])
                S.act(rstd[:], rstd[:], AF.Sqrt, ["brstd"], ["brstd"])
                S.op("dve", lambda e: e.reciprocal(out=rstd[:], in_=rstd[:]), ["brstd"], ["brstd"])
                for c in range(2):
                    S.act(x2[:, c, :], x2[:, c, :], AF.Identity, ["x2", "brstd"], ["x2"], scale=rstd[:, c:c + 1])
                    S.tt("pool", x2[:, c, :], x2[:, c, :], fgbc[:], ALU.mult, ["x2", "fgbc"], ["x2"])
                S.st(y_d[bb * 256:(bb + 1) * 256, :].rearrange("(c p) d -> p c d", p=128), x2[:], "yout", ["x2"])
            S.barrier()
            S.emit()
        print("bass instructions:", S.ninst)
    return nc


def make_in_maps(inputs, S_TOK=8192, n_cores=8):
    f = lambda a: np.ascontiguousarray(a, dtype=np.float32)
    cst = _consts()
    w_ada = f(inputs["w_ada"][0])
    b_ada = inputs["b_ada"][0]
    shared = dict(
        w_ada=w_ada,
        b_adaT=f(b_ada.reshape(48, 128).T),
        b_ada_bc=f(np.broadcast_to(np.concatenate([b_ada[2048:3072], b_ada[5120:6144]])[None, :], (128, 2048))),
        n1g=f(inputs["norm1_g"][0].reshape(8, 128).T),
        n2g=f(inputs["norm2_g"][0].reshape(8, 128).T),
        fg_bc=f(np.broadcast_to(inputs["final_g"][None, :], (128, D))),
        w_in=f(inputs["w_in"][0]),
        pool_w=f(inputs["pool_w"][0]),
        pool_scT=f(inputs["pool_scale"][0].reshape(4, 128).T),
        w_out=f(inputs["w_out"][0]),
        wq=f(inputs["peer_wq"][0]),
        skT=f(np.transpose(inputs["peer_subkeys"][0], (0, 2, 1))),
        UV=f(np.concatenate([np.transpose(inputs["peer_u"][0].reshape(128, 128, 8, 128), (0, 3, 2, 1)).reshape(128, 128, 1024),
                             inputs["peer_v"][0].reshape(128, 128, 1024)], axis=2)),
        **cst,
    )
    maps = []
    for b in range(n_cores):
        m = dict(shared)
        m["x"] = f(inputs["x"][b, :S_TOK])
        m["cT"] = f(inputs["c"][b].reshape(8, 128).T)
        maps.append(m)
    return maps


def kernel(**inputs):
    nc = build(8192)
    in_maps = make_in_maps(inputs, 8192, 8)
    res = run_bass_kernel_spmd(nc, in_maps, core_ids=list(range(8)))
    return np.stack([np.asarray(r["y"], dtype=np.float32) for r in res.results], axis=0)
```

```python
import numpy as np
from contextlib import ExitStack
import concourse.bass as bass
import concourse.mybir as mybir
from concourse.bass_utils import run_bass_kernel_spmd

F32 = mybir.dt.float32
F16 = mybir.dt.float16
F32R = mybir.dt.float32r
BF16 = mybir.dt.bfloat16
U32 = mybir.dt.uint32
AF = mybir.ActivationFunctionType
ALU = mybir.AluOpType
AX = mybir.AxisListType

D = 1024
NH = 8
HD = 64
NEG = -1.0e30


class Sched:
    ENG = ("pe", "act", "dve", "pool", "sp")
    ENGN = {"pe": "tensor", "act": "scalar", "dve": "vector", "pool": "gpsimd", "sp": "sync"}

    def __init__(self, nc, stack):
        self.nc = nc
        self.stack = stack
        self.prog = {e: [] for e in self.ENG}
        self.cnt = {e: 0 for e in self.ENG}
        self.sem = {e: stack.enter_context(nc.semaphore("sem_" + e)) for e in self.ENG}
        self.waited = {e: {} for e in self.ENG}
        self.res = {}
        self.dsem = {}
        self.ninst = 0

    def dma_sem(self, name):
        if name not in self.dsem:
            self.dsem[name] = [self.stack.enter_context(self.nc.semaphore("d_" + name)), 0]
        return self.dsem[name]

    def _need(self, eng, tok, waits):
        if tok is None:
            return
        kind, a, b = tok
        if kind == "e" and a == eng and eng == "pe":
            return
        key = (kind, a)
        if self.waited[eng].get(key, 0) >= b:
            return
        if waits.get(key, 0) < b:
            waits[key] = b

    def _deps(self, eng, reads, writes):
        waits = {}
        for k in reads:
            r = self.res.setdefault(k, {"w": None, "r": {}})
            self._need(eng, r["w"], waits)
        for k in writes:
            r = self.res.setdefault(k, {"w": None, "r": {}})
            self._need(eng, r["w"], waits)
            for (kind, a), b in r["r"].items():
                self._need(eng, (kind, a, b), waits)
        for key, v in waits.items():
            self.waited[eng][key] = v
            self.prog[eng].append(("wait", key, v))

    def _record(self, tok, reads, writes):
        for k in reads:
            rr = self.res[k]["r"]
            key = (tok[0], tok[1])
            if rr.get(key, 0) < tok[2]:
                rr[key] = tok[2]
        for k in writes:
            self.res[k] = {"w": tok, "r": {}}

    def op(self, eng, fn, reads=(), writes=()):
        self._deps(eng, reads, writes)
        self.cnt[eng] += 1
        tok = ("e", eng, self.cnt[eng])
        self.prog[eng].append(("op", fn))
        self._record(tok, reads, writes)
        self.ninst += 1

    def dma(self, q, fn, semname, reads=(), writes=()):
        self._deps(q, reads, writes)
        s = self.dma_sem(semname)
        s[1] += 16
        tok = ("d", semname, s[1])
        self.prog[q].append(("dma", fn, semname))
        self._record(tok, reads, writes)
        self.ninst += 1

    def barrier(self):
        for e in self.ENG:
            for o in self.ENG:
                if o != e and self.cnt[o] > self.waited[e].get(("e", o), 0):
                    self.waited[e][("e", o)] = self.cnt[o]
                    self.prog[e].append(("wait", ("e", o), self.cnt[o]))
            for name, s in self.dsem.items():
                if s[1] > self.waited[e].get(("d", name), 0):
                    self.waited[e][("d", name)] = s[1]
                    self.prog[e].append(("wait", ("d", name), s[1]))

    def emit(self):
        nc = self.nc
        with nc.Block() as block:
            for e in self.ENG:
                prog = self.prog[e]

                def body(engine, prog=prog, e=e):
                    for it in prog:
                        if it[0] == "wait":
                            key, v = it[1], it[2]
                            h = self.sem[key[1]] if key[0] == "e" else self.dsem[key[1]][0]
                            engine.wait_ge(h, v)
                        elif it[0] == "op":
                            it[1](engine).then_inc(self.sem[e], 1)
                        else:
                            it[1](engine).then_inc(self.dsem[it[2]][0], 16)
                getattr(block, self.ENGN[e])(body)
        self.prog = {e: [] for e in self.ENG}

    def mm(self, out, lhsT, rhs, start, stop, reads, writes):
        self.op("pe", lambda e: e.matmul(out, lhsT=lhsT, rhs=rhs, start=start, stop=stop), reads, writes)

    def tr(self, out, in_, ident, reads, writes):
        self.op("pe", lambda e: e.transpose(out, in_, ident), reads, writes)

    def act(self, out, in_, func, reads, writes, scale=None, bias=None, accum_out=None):
        kw = {}
        if scale is not None:
            kw["scale"] = scale
        if bias is not None:
            kw["bias"] = bias
        if accum_out is not None:
            kw["accum_out"] = accum_out
        self.op("act", lambda e: e.activation(out=out, in_=in_, func=func, **kw), reads, writes)

    def tt(self, eng, out, in0, in1, op, reads, writes):
        self.op(eng, lambda e: e.tensor_tensor(out=out, in0=in0, in1=in1, op=op), reads, writes)

    def ts(self, eng, out, in0, s1, s2, op0, op1, reads, writes):
        if op1 is None:
            self.op(eng, lambda e: e.tensor_scalar(out=out, in0=in0, scalar1=s1, scalar2=None, op0=op0), reads, writes)
        else:
            self.op(eng, lambda e: e.tensor_scalar(out=out, in0=in0, scalar1=s1, scalar2=s2, op0=op0, op1=op1), reads, writes)

    def copy(self, eng, out, in_, reads, writes):
        self.op(eng, lambda e: e.tensor_copy(out=out, in_=in_), reads, writes)

    def memset(self, eng, ap, val, writes):
        self.op(eng, lambda e: e.memset(ap, val), (), writes)

    def ld(self, out, in_, sem, writes, q="sp"):
        self.dma(q, lambda e: e.dma_start(out=out, in_=in_), sem, (), writes)

    def st(self, out, in_, sem, reads, q="pool"):
        self.dma(q, lambda e: e.dma_start(out=out, in_=in_), sem, reads, ())


def _consts():
    ident = np.eye(128, dtype=np.float32)
    k = np.arange(128)[:, None, None]
    rel = np.arange(17)[None, :, None]
    q = np.arange(128)[None, None, :]
    dist = rel * 128 + q - k
    cnt = ((dist >= 0) & (dist <= 128)).astype(np.float32)
    cnt += ((dist >= 0) & (dist <= 512) & (dist % 4 == 0)).astype(np.float32)
    cnt += ((dist >= 0) & (dist <= 2048) & (dist % 16 == 0)).astype(np.float32)
    CT = np.ascontiguousarray(cnt.reshape(128, 17 * 128), dtype=np.float32)
    slopes = np.array([2.0 ** (-8.0 * (h + 1) / NH) for h in range(NH)], dtype=np.float64)
    kl = np.arange(128, dtype=np.float64)[:, None, None]
    bk = slopes[None, :, None] * (kl - 64.0) - slopes[None, :, None] * 128.0 * np.arange(17)[None, None, :]
    biasK = np.ascontiguousarray(bk.reshape(128, NH * 17), dtype=np.float32)
    bm = slopes[None, :, None] * (kl - 64.0) - slopes[None, :, None] * 128.0 * (np.arange(-3, 17)[None, None, :] + 1.5)
    biasM = np.ascontiguousarray(bm.reshape(128, NH * 20), dtype=np.float32)
    band = np.zeros((128, 4, 3, 128), dtype=np.float64)
    s = np.arange(128)[:, None]
    t = np.arange(128)[None, :]
    for pg, w in enumerate((2, 4, 8, 16)):
        cur = ((s <= t) & (s > t - w)).astype(np.float64) / w - (s == t)
        prev = ((t + 128 - s) < w).astype(np.float64) / w
        cntf = np.minimum(t + 1, w).astype(np.float64)
        first = ((s <= t) & (s > t - w)).astype(np.float64) / cntf - (s == t)
        band[:, pg, 0, :] = cur
        band[:, pg, 1, :] = prev
        band[:, pg, 2, :] = first
    band = np.ascontiguousarray(band.reshape(128, 12 * 128), dtype=np.float32)
    iota4 = np.ascontiguousarray(np.tile(np.arange(128, dtype=np.float32), (128, 8)))
    return dict(ident=ident, CT=CT, biasK=biasK, biasM=biasM, band=band, iota4=iota4)


def build(S_TOK=8192, debug=False):
    NCHK = S_TOK // 128
    NG = S_TOK // 512
    NB = S_TOK // 256
    nc = bass.Bass("TRN2", target_bir_lowering=False)
    scr_kind = "ExternalOutput" if debug else "Internal"

    def din(name, shape, dt=F32):
        return nc.dram_tensor(name, list(shape), dt, kind="ExternalInput").ap()

    def dscr(name, shape, dt=F32):
        return nc.dram_tensor(name, list(shape), dt, kind=scr_kind).ap()

    x_d = din("x", [S_TOK, D])
    cT_d = din("cT", [128, 8])
    wada_d = din("w_ada", [D, 6 * D])
    badaT_d = din("b_adaT", [128, 48])
    badabc_d = din("b_ada_bc", [128, 2 * D])
    n1g_d = din("n1g", [128, 8])
    n2g_d = din("n2g", [128, 8])
    fgbc_d = din("fg_bc", [128, D])
    win_d = din("w_in", [D, 2048], F32R)
    poolw_d = din("pool_w", [4, 128, 128], F32R)
    poolsc_d = din("pool_scT", [128, 4])
    wout_d = din("w_out", [D, D], F32R)
    wq_d = din("wq", [D, 2048])
    skT_d = din("skT", [2, 128, 128])
    UV_d = din("UV", [128, 128, 2048])
    ident_d = din("ident", [128, 128])
    CT_d = din("CT", [128, 17 * 128])
    biasK_d = din("biasK", [128, NH * 17])
    biasM_d = din("biasM", [128, NH * 20])
    band_d = din("band", [128, 12 * 128], F32R)
    iota4_d = din("iota4", [128, 1024])

    qT_s = dscr("qT_scr", [4, 128, S_TOK], F32R)
    kT_s = dscr("kT_scr", [4, 128, S_TOK], F32R)
    v_s = dscr("v_scr", [S_TOK, NH * 66], BF16)
    u_s = dscr("u_scr", [S_TOK, 512], F32R)
    attn_s = dscr("attn_scr", [S_TOK, 512])
    x1_s = dscr("x1_scr", [S_TOK, D])
    h2T_s = dscr("h2T_scr", [8, 128, S_TOK], BF16)
    UVh_s = dscr("UVh_scr", [128, 128, 2048], BF16)
    q2T_s = dscr("q2T_scr", [8, 128, S_TOK])
    bT_s = dscr("bT_scr", [128, S_TOK])
    nT_s = dscr("nT_scr", [128, S_TOK])
    tT_s = dscr("tT_scr", [128, S_TOK])
    iT_s = dscr("iT_scr", [128, S_TOK])
    y_d = nc.dram_tensor("y", [S_TOK, D], F32, kind="ExternalOutput").ap()

    with ExitStack() as top:
        S = Sched(nc, top)

        def sb(stack, name, shape, dt=F32):
            return stack.enter_context(nc.sbuf_tensor("s_" + name, list(shape), dt))

        psall = top.enter_context(nc.psum_tensor("psall", [128, 8, 512], F32))
        bank = [psall[:, i, :] for i in range(8)]
        BK = ["bank%d" % i for i in range(8)]

        ident = sb(top, "ident", [128, 128])
        scale1 = sb(top, "scale1", [128, 8])
        shift1 = sb(top, "shift1", [128, 8])
        scale2 = sb(top, "scale2", [128, 8])
        shift2 = sb(top, "shift2", [128, 8])
        g1bc = sb(top, "g1bc", [128, D])
        g2bc = sb(top, "g2bc", [128, D])
        S.ld(ident[:], ident_d, "c_ident", ["ident"])

        with ExitStack() as ph:
            cT = sb(ph, "cT", [128, 8])
            cs = sb(ph, "cs", [128, 8])
            csrep = sb(ph, "csrep", [128, 8, 128])
            badaT = sb(ph, "badaT", [128, 48])
            badabc = sb(ph, "badabc", [128, 2 * D])
            n1g = sb(ph, "n1g", [128, 8])
            n2g = sb(ph, "n2g", [128, 8])
            modc = sb(ph, "modc", [128, 48])
            wa = [sb(ph, "wa%d" % i, [128, 8, 512]) for i in range(2)]
            S.ld(cT[:], cT_d, "p1a", ["cT"])
            S.ld(badaT[:], badaT_d, "p1b", ["badaT"])
            S.ld(badabc[:], badabc_d, "p1c", ["badabc"])
            S.ld(n1g[:], n1g_d, "p1d", ["n1g"])
            S.ld(n2g[:], n2g_d, "p1e", ["n2g"])
            S.act(cs[:], cT[:], AF.Silu, ["cT"], ["cs"])
            S.copy("dve", csrep[:], cs[:].unsqueeze(2).to_broadcast([128, 8, 128]), ["cs"], ["csrep"])
            S.memset("dve", modc[:], 0.0, ["modc"])
            for n in range(12):
                w = wa[n % 2]
                wk = "wa%d" % (n % 2)
                S.ld(w[:], wada_d[:, n * 512:(n + 1) * 512].rearrange("(kc p) n -> p kc n", p=128), wk, [wk])
                if n in (4, 5, 10, 11):
                    pb = bank[n % 2]
                    for kc in range(8):
                        S.mm(pb[:, :], csrep[:, kc, :], w[:, kc, :], kc == 0, kc == 7, ["csrep", wk], [BK[n % 2]])
                    dst = g1bc if n < 6 else g2bc
                    dk = "g1bc" if n < 6 else "g2bc"
                    off = (n % 2) * 512
                    boff = off if n < 6 else D + off
                    S.tt("dve", dst[:, off:off + 512], pb[:, :], badabc[:, boff:boff + 512], ALU.add,
                         [BK[n % 2], "badabc"], [dk])
                else:
                    pb = bank[2 + n % 2]
                    for jj in range(4):
                        for kc in range(8):
                            S.mm(pb[:, jj:jj + 1], w[:, kc, jj * 128:(jj + 1) * 128], cs[:, kc:kc + 1],
                                 kc == 0, kc == 7, ["cs", wk], [BK[2 + n % 2]])
                    S.tt("dve", modc[:, 4 * n:4 * n + 4], pb[:, 0:4], badaT[:, 4 * n:4 * n + 4], ALU.add,
                         [BK[2 + n % 2], "badaT"], ["modc"])
            S.op("dve", lambda e: e.scalar_tensor_tensor(out=scale1[:], in0=modc[:, 8:16], scalar=1.0, in1=n1g[:],
                                                         op0=ALU.add, op1=ALU.mult), ["modc", "n1g"], ["scale1"])
            S.op("dve", lambda e: e.scalar_tensor_tensor(out=scale2[:], in0=modc[:, 32:40], scalar=1.0, in1=n2g[:],
                                                         op0=ALU.add, op1=ALU.mult), ["modc", "n2g"], ["scale2"])
            S.copy("dve", shift1[:], modc[:, 0:8], ["modc"], ["shift1"])
            S.copy("dve", shift2[:], modc[:, 24:32], ["modc"], ["shift2"])
            S.barrier()
            S.emit()

        def norm_transpose(xt, xk, xn, xnk, hT, hTk, ss, rstd, nch, scol, scolk, bcol, bcolk, pbanks, hT2=None, hT2k=None):
            for c in range(nch):
                S.act(xn[:, c, :], xt[:, c, :], AF.Square, [xk], [xnk], accum_out=ss[:, c:c + 1])
            S.ts("dve", rstd[:, 0:nch], ss[:, 0:nch], 1.0 / D, 1e-6, ALU.mult, ALU.add, ["ss", xnk], ["rstd"])
            S.act(rstd[:, 0:nch], rstd[:, 0:nch], AF.Sqrt, ["rstd"], ["rstd"])
            S.op("dve", lambda e: e.reciprocal(out=rstd[:, 0:nch], in_=rstd[:, 0:nch]), ["rstd"], ["rstd"])
            for c in range(nch):
                S.act(xn[:, c, :], xt[:, c, :], AF.Identity, [xk, "rstd"], [xnk], scale=rstd[:, c:c + 1])
            for kc in range(8):
                pb = pbanks[kc % 2]
                for c in range(nch):
                    S.tr(bank[pb][:, c * 128:(c + 1) * 128], xn[:, c, kc * 128:(kc + 1) * 128], ident[:],
                         [xnk, "ident"], [BK[pb]])
                S.act(hT[:, kc, 0:nch * 128], bank[pb][:, 0:nch * 128], AF.Identity, [BK[pb], scolk, bcolk], [hTk],
                      scale=scol[:, kc:kc + 1], bias=bcol[:, kc:kc + 1])
                if hT2 is not None:
                    S.act(hT2[:, kc, 0:nch * 128], bank[pb][:, 0:nch * 128], AF.Identity, [BK[pb], scolk, bcolk], [hT2k],
                          scale=scol[:, kc:kc + 1], bias=bcol[:, kc:kc + 1])

        with ExitStack() as ph:
            S.barrier()
            win = sb(ph, "win", [128, 8, 2048], F32R)
            xt = [sb(ph, "p2x%d" % i, [128, 4, D]) for i in range(2)]
            xn = sb(ph, "p2xn", [128, 4, D])
            hT = sb(ph, "p2hT", [128, 8, 512], F32R)
            ss = sb(ph, "p2ss", [128, 4])
            rstd = sb(ph, "p2rstd", [128, 4])
            qst = sb(ph, "p2q", [128, 4, 512], F32R)
            kst = sb(ph, "p2k", [128, 4, 512], F32R)
            vst = sb(ph, "p2v", [128, 4, NH, 66], BF16)
            ones_t = sb(ph, "p2ones", [128, 4 * NH * 66])
            ust = sb(ph, "p2u", [128, 4, 512], F32R)
            for i in range(4):
                S.ld(win[:, :, i * 512:(i + 1) * 512],
                     win_d[:, i * 512:(i + 1) * 512].rearrange("(kc p) n -> p kc n", p=128), "win", ["win"], q="pool")
            S.memset("dve", ones_t[:], 1.0, ["ones_t"])
            S.copy("dve", vst[:].rearrange("p c h f -> p (c h f)"), ones_t[:], ["ones_t"], ["vst"])
            xv = x_d.rearrange("(g c p) d -> g p c d", p=128, c=4)
            S.ld(xt[0][:], xv[0], "p2x0", ["p2x0"])
            for g in range(NG):
                xk = "p2x%d" % (g % 2)
                if g + 1 < NG:
                    S.ld(xt[(g + 1) % 2][:], xv[g + 1], "p2x%d" % ((g + 1) % 2), ["p2x%d" % ((g + 1) % 2)])
                norm_transpose(xt[g % 2], xk, xn, "p2xn", hT, "p2hT", ss, rstd, 4,
                               scale1, "scale1", shift1, "shift1", (0, 1))
                t0 = g * 512
                for hp in range(4):
                    pb = 2 + hp % 2
                    for kc in range(8):
                        S.mm(bank[pb][:, :], win[:, kc, hp * 128:(hp + 1) * 128], hT[:, kc, :], kc == 0, kc == 7,
                             ["win", "p2hT"], [BK[pb]])
                    S.act(qst[:, hp, :], bank[pb][:, :], AF.Copy, [BK[pb]], ["qst"], scale=0.125)
                for hp in range(4):
                    pb = 2 + hp % 2
                    for kc in range(8):
                        S.mm(bank[pb][:, :], win[:, kc, 512 + hp * 128:512 + (hp + 1) * 128], hT[:, kc, :],
                             kc == 0, kc == 7, ["win", "p2hT"], [BK[pb]])
                    S.copy("dve", kst[:, hp, :], bank[pb][:, :], [BK[pb]], ["kst"])
                for c in range(4):
                    pb = 4 + c % 2
                    for kc in range(8):
                        S.mm(bank[pb][:, :], hT[:, kc, c * 128:(c + 1) * 128], win[:, kc, 1024:1536], kc == 0, kc == 7,
                             ["win", "p2hT"], [BK[pb]])
                    S.act(vst[:, c, :, 0:64], bank[pb][:, :].rearrange("p (h f) -> p h f", f=64), AF.Copy,
                          [BK[pb]], ["vst"])
                for c in range(4):
                    pb = 6 + c % 2
                    for kc in range(8):
                        S.mm(bank[pb][:, :], hT[:, kc, c * 128:(c + 1) * 128], win[:, kc, 1536:2048], kc == 0, kc == 7,
                             ["win", "p2hT"], [BK[pb]])
                    S.copy("dve", ust[:, c, :], bank[pb][:, :], [BK[pb]], ["ust"])
                S.st(qT_s[:, :, t0:t0 + 512].rearrange("h p t -> p h t"), qst[:], "p2sq", ["qst"])
                S.st(kT_s[:, :, t0:t0 + 512].rearrange("h p t -> p h t"), kst[:], "p2sk", ["kst"])
                S.st(v_s[t0:t0 + 512, :].rearrange("(c p) f -> p c f", p=128), vst[:].rearrange("p c h f -> p c (h f)"),
                     "p2sv", ["vst"])
                S.st(u_s[t0:t0 + 512, :].rearrange("(c p) f -> p c f", p=128), ust[:], "p2su", ["ust"])
            S.barrier()
            S.emit()

        with ExitStack() as ph:
            S.barrier()
            CT = sb(ph, "CT", [128, 17 * 128])
            biasK = sb(ph, "biasK", [128, NH * 17])
            biasM = sb(ph, "biasM", [128, NH * 20])
            KT = sb(ph, "KT", [128, S_TOK], F32R)
            Vp = sb(ph, "Vp", [128, NCHK, 2, 66], BF16)
            QTg = [sb(ph, "QTg%d" % i, [128, 512], F32R) for i in range(2)]
            PT = [sb(ph, "PT%d" % i, [128, 512]) for i in range(2)]
            PTm = [[sb(ph, "PTm%d_%d" % (p_, i), [128, 512], BF16) for i in range(20)] for p_ in range(2)]
            ao = [sb(ph, "ao%d" % i, [128, 4, 128]) for i in range(2)]
            rden = sb(ph, "rden", [128, 8])
            S.ld(CT[:], CT_d, "p3c", ["CT"])
            S.ld(biasK[:], biasK_d, "p3b", ["biasK"])
            S.ld(biasM[:], biasM_d, "p3b2", ["biasM"])
            cnt3 = dict(nrd=0, nsb=0, nob=0, npt=0)
            units = [(hp, g, hh) for hp in range(4) for g in range(NG) for hh in range(2)]

            def p3_S(n):
                hp, g, hh = units[n]
                par = n % 2
                it = hp * NG + g
                qk = "QTg%d" % (it % 2)
                qt = QTg[it % 2]
                if g == 0 and hh == 0:
                    npc = max(1, S_TOK // 2048)
                    for i in range(npc):
                        w = S_TOK // npc
                        S.ld(KT[:, i * w:(i + 1) * w], kT_s[hp, :, i * w:(i + 1) * w], "p3k", ["KT"], q="pool")
                if hh == 0:
                    S.ld(qt[:], qT_s[hp, :, g * 512:(g + 1) * 512], qk, [qk], q="pool")
                h = 2 * hp + hh
                r0 = 64 * hh
                kbs = list(range(max(0, 4 * g - 16), 4 * g + 4))
                steps = []
                for kb in kbs:
                    steps.append(lambda kb=kb: s_step(kb, hp, g, hh, h, r0, par, qk, qt))
                return steps

            def s_step(kb, hp, g, hh, h, r0, par, qk, qt):
                if True:
                    kbi = kb - (4 * g - 16)
                    sbk = cnt3["nsb"] % 2
                    cnt3["nsb"] += 1
                    S.mm(bank[sbk][:, :], KT[r0:r0 + 64, kb * 128:(kb + 1) * 128], qt[r0:r0 + 64, :], True, True,
                         ["KT", qk], [BK[sbk]])
                    ptk = "PT%d" % (cnt3["npt"] % 2)
                    ptt = PT[cnt3["npt"] % 2]
                    cnt3["npt"] += 1
                    js = [j for j in range(4) if 0 <= 4 * g + j - kb <= 16]
                    j0, j1 = js[0], js[-1]
                    if h >= 2:
                        r_ = 4 * g - kb
                        S.act(ptt[:, j0 * 128:(j1 + 1) * 128], bank[sbk][:, j0 * 128:(j1 + 1) * 128], AF.Exp,
                              [BK[sbk], "biasM"], [ptk], bias=biasM[:, h * 20 + r_ + 3:h * 20 + r_ + 4])
                    else:
                        for j in js:
                            rel = 4 * g + j - kb
                            S.act(ptt[:, j * 128:(j + 1) * 128], bank[sbk][:, j * 128:(j + 1) * 128], AF.Exp,
                                  [BK[sbk], "biasK"], [ptk], bias=biasK[:, h * 17 + rel:h * 17 + rel + 1])
                    rel0 = 4 * g + j0 - kb
                    S.tt("dve", PTm[par][kbi][:, j0 * 128:(j1 + 1) * 128], ptt[:, j0 * 128:(j1 + 1) * 128],
                         CT[:, rel0 * 128:(rel0 + j1 - j0 + 1) * 128], ALU.mult, [ptk, "CT"], ["PTm%d_%d" % (par, kbi)])

            def p3_V(n):
                hp, g, hh = units[n]
                par = n % 2
                it = hp * NG + g
                aok = "ao%d" % (it % 2)
                aot = ao[it % 2]
                if g == 0 and hh == 0:
                    nvp = max(1, NCHK // 8)
                    for i in range(nvp):
                        nb_ = NCHK // nvp
                        S.ld(Vp[:, i * nb_:(i + 1) * nb_, :, :].rearrange("p b h f -> p b (h f)"),
                             v_s[i * nb_ * 128:(i + 1) * nb_ * 128, hp * 132:(hp + 1) * 132].rearrange("(b p) f -> p b f", p=128),
                             "p3v", ["Vp"], q="sp")
                kbs = list(range(max(0, 4 * g - 16), 4 * g + 4))
                steps = []
                for j in range(4):
                    steps.append(lambda j=j: v_step(j, hp, g, hh, par, aok, aot, kbs))
                return steps

            def v_step(j, hp, g, hh, par, aok, aot, kbs):
                if True:
                    obk = 2 + cnt3["nob"] % 2
                    cnt3["nob"] += 1
                    kl = [kb for kb in kbs if 0 <= 4 * g + j - kb <= 16]
                    for n_, kb in enumerate(kl):
                        kbi = kb - (4 * g - 16)
                        S.mm(bank[obk][:, 0:66], PTm[par][kbi][:, j * 128:(j + 1) * 128], Vp[:, kb, hh, :],
                             n_ == 0, n_ == len(kl) - 1, ["PTm%d_%d" % (par, kbi), "Vp"], [BK[obk]])
                    rc = cnt3["nrd"] % 8
                    cnt3["nrd"] += 1
                    S.op("dve", lambda e, obk=obk, rc=rc: e.reciprocal(out=rden[:, rc:rc + 1], in_=bank[obk][:, 64:65]),
                         [BK[obk]], ["rden%d" % rc])
                    S.act(aot[:, j, hh * 64:(hh + 1) * 64], bank[obk][:, 0:64], AF.Identity,
                          [BK[obk], "rden%d" % rc], [aok], scale=rden[:, rc:rc + 1])
                if hh == 1 and j == 3:
                    S.st(attn_s[g * 512:(g + 1) * 512, hp * 128:(hp + 1) * 128].rearrange("(c p) f -> p c f", p=128),
                         aot[:], aok, [aok], q="act")

            for st_ in p3_S(0):
                st_()
            for n in range(len(units)):
                vs = p3_V(n)
                ss_ = p3_S(n + 1) if n + 1 < len(units) else []
                ns = len(ss_)
                vi = 0
                for i_, st_ in enumerate(ss_):
                    st_()
                    while vi < 4 and (i_ + 1) * 4 >= (vi + 1) * ns:
                        vs[vi]()
                        vi += 1
                while vi < 4:
                    vs[vi]()
                    vi += 1
            S.barrier()
            S.emit()

        with ExitStack() as ph:
            S.barrier()
            wout = sb(ph, "wout", [128, 8, D], F32R)
            band = sb(ph, "band", [128, 12 * 128], F32R)
            poolw = sb(ph, "poolw", [128, 4, 128], F32R)
            poolsc = sb(ph, "poolsc", [128, 4])
            xt = [sb(ph, "p4x%d" % i, [128, 4, D]) for i in range(2)]
            at = [sb(ph, "p4a%d" % i, [128, 4, 512]) for i in range(2)]
            ub = [sb(ph, "p4u%d" % i, [128, 5, 512], F32R) for i in range(2)]
            concatT = sb(ph, "concatT", [128, 8, 512], F32R)
            mixT = sb(ph, "mixT", [128, 4, 512], F32R)
            x1t = sb(ph, "x1t", [128, 4, D])
            for i in range(2):
                S.ld(wout[:, :, i * 512:(i + 1) * 512],
                     wout_d[:, i * 512:(i + 1) * 512].rearrange("(kc p) n -> p kc n", p=128), "wout", ["wout"], q="pool")
            S.ld(band[:], band_d, "p4c1", ["band"], q="pool")
            S.ld(poolw[:], poolw_d.rearrange("g c e -> c g e"), "p4c2", ["poolw"], q="pool")
            S.ld(poolsc[:], poolsc_d, "p4c3", ["poolsc"])
            xv = x_d.rearrange("(g c p) d -> g p c d", p=128, c=4)
            av = attn_s.rearrange("(g c p) d -> g p c d", p=128, c=4)
            uv = u_s.rearrange("(g c p) d -> g p c d", p=128, c=4)

            def p4_load(g):
                i = g % 2
                S.ld(xt[i][:], xv[g], "p4x%d" % i, ["p4x%d" % i])
                S.ld(at[i][:], av[g], "p4a%d" % i, ["p4a%d" % i])
                S.ld(ub[i][:, 1:5, :], uv[g], "p4u%d" % i, ["p4u%d" % i], q="pool")
                if g > 0:
                    S.ld(ub[i][:, 0, :], u_s[g * 512 - 128:g * 512, :], "p4u%d" % i, ["p4u%d" % i], q="pool")
            p4_load(0)
            for g in range(NG):
                i = g % 2
                xk, ak, uk = "p4x%d" % i, "p4a%d" % i, "p4u%d" % i
                if g + 1 < NG:
                    p4_load(g + 1)
                for kc in range(4):
                    pb = kc % 2
                    for c in range(4):
                        S.tr(bank[pb][:, c * 128:(c + 1) * 128], at[i][:, c, kc * 128:(kc + 1) * 128], ident[:],
                             [ak, "ident"], [BK[pb]])
                    S.act(concatT[:, kc, :], bank[pb][:, :], AF.Copy, [BK[pb]], ["concatT"])
                for pg in range(4):
                    pb = 2 + pg % 2
                    for c in range(4):
                        o = bank[pb][:, c * 128:(c + 1) * 128]
                        if 4 * g + c == 0:
                            S.mm(o, ub[i][:, 1 + c, pg * 128:(pg + 1) * 128], band[:, (pg * 3 + 2) * 128:(pg * 3 + 3) * 128],
                                 True, True, [uk, "band"], [BK[pb]])
                        else:
                            S.mm(o, ub[i][:, 1 + c, pg * 128:(pg + 1) * 128], band[:, (pg * 3 + 0) * 128:(pg * 3 + 1) * 128],
                                 True, False, [uk, "band"], [BK[pb]])
                            S.mm(o, ub[i][:, c, pg * 128:(pg + 1) * 128], band[:, (pg * 3 + 1) * 128:(pg * 3 + 2) * 128],
                                 False, True, [uk, "band"], [BK[pb]])
                    S.copy("dve", mixT[:, pg, :], bank[pb][:, :], [BK[pb]], ["mixT"])
                for pg in range(4):
                    pb = 4 + pg % 2
                    S.mm(bank[pb][:, :], poolw[:, pg, :], mixT[:, pg, :], True, True, ["poolw", "mixT"], [BK[pb]])
                    S.act(concatT[:, 4 + pg, :], bank[pb][:, :], AF.Identity, [BK[pb], "poolsc"], ["concatT"],
                          scale=poolsc[:, pg:pg + 1])
                for c in range(4):
                    for n in range(2):
                        pb = 6 + n
                        for kc in range(8):
                            S.mm(bank[pb][:, :], concatT[:, kc, c * 128:(c + 1) * 128], wout[:, kc, n * 512:(n + 1) * 512],
                                 kc == 0, kc == 7, ["concatT", "wout"], [BK[pb]])
                        S.tt("dve", x1t[:, c, n * 512:(n + 1) * 512], bank[pb][:, :], g1bc[:, n * 512:(n + 1) * 512], ALU.mult,
                             [BK[pb], "g1bc"], ["x1t"])
                    S.tt("pool", x1t[:, c, :], x1t[:, c, :], xt[i][:, c, :], ALU.add, ["x1t", xk], ["x1t"])
                S.st(x1_s[g * 512:(g + 1) * 512, :].rearrange("(c p) d -> p c d", p=128), x1t[:], "p4s", ["x1t"])
            S.barrier()
            S.emit()

        with ExitStack() as ph:
            S.barrier()
            wq = sb(ph, "wq", [128, 8, 2048])
            skT = sb(ph, "skT", [128, 2, 128])
            xt = [sb(ph, "p5x%d" % i, [128, 2, D]) for i in range(2)]
            xn = sb(ph, "p5xn", [128, 2, D])
            hT = sb(ph, "p5hT", [128, 8, 256])
            hTr = sb(ph, "p5hTr", [128, 8, 256], BF16)
            ss = sb(ph, "p5ss", [128, 4])
            rstd = sb(ph, "p5rstd", [128, 4])
            QT = sb(ph, "p5QT", [128, 16, 256])
            Ssb = [[sb(ph, "Ssb%d_%d" % (a_, c_), [128, 16, 128]) for c_ in range(2)] for a_ in range(2)]
            res4 = [sb(ph, "res4_%d" % c_, [128, 4, 128]) for c_ in range(2)]
            Sm = sb(ph, "Sm", [128, 16, 128])
            sv = sb(ph, "sv", [128, 16, 16])
            si = sb(ph, "si", [128, 8, 16], U32)
            sif = sb(ph, "sif", [128, 8, 16])
            cand = sb(ph, "cand", [128, 8, 256])
            candm = Sm[:].rearrange("p a n -> p (a n)").rearrange("p (h c) -> p h c", c=256)
            ee = sb(ph, "ee", [128, 8, 256])
            c8a = sb(ph, "c8a", [128, 8, 8])
            c8b = sb(ph, "c8b", [128, 8, 8])
            Zt = sb(ph, "Zt", [128, 8])
            mz = sb(ph, "mz", [128, 8])
            b1 = sb(ph, "b1", [128, 8, 16])
            nz = sb(ph, "nz", [128, 8, 16])
            th = sb(ph, "th", [128, 8, 16])
            stg = sb(ph, "stg", [128, 4, 256])
            for i in range(4):
                S.ld(wq[:, :, i * 512:(i + 1) * 512],
                     wq_d[:, i * 512:(i + 1) * 512].rearrange("(kc p) n -> p kc n", p=128), "wq", ["wq"])
            S.ld(skT[:], skT_d.rearrange("h k n -> k h n"), "p5c", ["skT"])
            xv = x1_s.rearrange("(g c p) d -> g p c d", p=128, c=2)
            S.ld(xt[0][:], xv[0], "p5x0", ["p5x0"])
            sv4 = sv[:].rearrange("p (h two) a -> p h two a", two=2)
            sv1 = sv4[:, :, 0, :]
            sv2 = sv4[:, :, 1, :]
            def p5_A1(g):
                xk = "p5x%d" % (g % 2)
                if g + 1 < NB:
                    S.ld(xt[(g + 1) % 2][:], xv[g + 1], "p5x%d" % ((g + 1) % 2), ["p5x%d" % ((g + 1) % 2)])
                norm_transpose(xt[g % 2], xk, xn, "p5xn", hT, "p5hT", ss, rstd, 2,
                               scale2, "scale2", shift2, "shift2", (0, 1), hTr, "p5hTr")
                t0 = g * 256
                S.st(h2T_s[:, :, t0:t0 + 256].rearrange("k p t -> p k t"), hTr[:], "p5sh", ["p5hTr"])

            def p5_A2(g, half):
                for hpq in range(8 * half, 8 * half + 8):
                    pb = 2 + hpq % 2
                    for kc in range(8):
                        S.mm(bank[pb][:, 0:256], wq[:, kc, hpq * 128:(hpq + 1) * 128], hT[:, kc, :], kc == 0, kc == 7,
                             ["wq", "p5hT"], [BK[pb]])
                    S.act(QT[:, hpq, :], bank[pb][:, 0:256], AF.Copy, [BK[pb]], ["p5QT"])
                    if hpq % 2 == 1:
                        conv_some(1)

            def p5_A3(g):
                t0 = g * 256
                S.st(q2T_s[:, :, t0:t0 + 256].rearrange("h p t -> p h t"),
                     QT[:].rearrange("p (h two) t -> p h two t", two=2)[:, :, 1, :], "p5sq", ["p5QT"])
                for c in range(2):
                    sk_ = "Ssb%d_%d" % (g % 2, c)
                    for b4 in range(4):
                        pb = 4 + b4
                        for k4 in range(4):
                            hpq = b4 * 4 + k4
                            S.mm(bank[pb][:, k4 * 128:(k4 + 1) * 128], QT[:, hpq, c * 128:(c + 1) * 128], skT[:, hpq % 2, :],
                                 True, True, ["p5QT", "skT"], [BK[pb]])
                        S.act(Ssb[g % 2][c][:, b4 * 4:(b4 + 1) * 4, :], bank[pb][:, :].rearrange("p (a n) -> p a n", n=128), AF.Copy,
                              [BK[pb]], [sk_])

            def p5_B1(g, c):
                sk_ = "Ssb%d_%d" % (g % 2, c)
                Sc = Ssb[g % 2][c]
                for hpq in range(16):
                    S.op("dve", lambda e, hpq=hpq: e.max(out=sv[:, hpq, 0:8], in_=Sc[:, hpq, :]), [sk_], ["sv"])
                    S.op("dve", lambda e, hpq=hpq: e.match_replace(out=Sm[:, hpq, :], in_to_replace=sv[:, hpq, 0:8],
                                                                 in_values=Sc[:, hpq, :], imm_value=NEG),
                         [sk_, "sv"], ["Sm"])
                    S.op("dve", lambda e, hpq=hpq: e.max(out=sv[:, hpq, 8:16], in_=Sm[:, hpq, :]), ["Sm"], ["sv"])
                for h in range(8):
                    S.op("dve", lambda e, h=h: e.max_index(out=si[:, h, 0:8], in_max=sv[:, 2 * h, 0:8],
                                                          in_values=Sc[:, 2 * h, :]), [sk_, "sv"], ["si"])
                    S.op("dve", lambda e, h=h: e.max_index(out=si[:, h, 8:16], in_max=sv[:, 2 * h, 8:16],
                                                          in_values=Sm[:, 2 * h, :]), ["Sm", "sv"], ["si"])
                S.copy("dve", res4[c][:, 3, :].rearrange("p (h a) -> p h a", a=16), si[:], ["si"], ["res4_%d" % c])
                S.tt("dve", cand[:].rearrange("p h (a b) -> p h a b", b=16),
                     sv1.unsqueeze(3).to_broadcast([128, 8, 16, 16]),
                     sv2.unsqueeze(2).to_broadcast([128, 8, 16, 16]), ALU.add, ["sv"], ["cand"])
                for h in range(8):
                    S.op("dve", lambda e, h=h: e.max(out=c8a[:, h, :], in_=cand[:, h, :]), ["cand"], ["c8a"])
                    S.op("dve", lambda e, h=h: e.match_replace(out=candm[:, h, :], in_to_replace=c8a[:, h, :],
                                                             in_values=cand[:, h, :], imm_value=NEG),
                         ["cand", "c8a"], ["Sm"])
                    S.op("dve", lambda e, h=h: e.max(out=c8b[:, h, :], in_=candm[:, h, :]), ["Sm"], ["c8b"])
                S.tt("dve", ee[:], cand[:], c8a[:, :, 0:1].to_broadcast([128, 8, 256]), ALU.subtract,
                     ["cand", "c8a"], ["ee"])

            def p5_B2(g, c):
                S.act(ee[:], ee[:], AF.Exp, ["ee"], ["ee"])
                S.tt("dve", candm[:], cand[:], c8b[:, :, 7:8].to_broadcast([128, 8, 256]), ALU.is_ge,
                     ["cand", "c8b"], ["Sm"])
                S.tt("dve", ee[:], ee[:], candm[:], ALU.mult, ["ee", "Sm"], ["ee"])
                S.op("dve", lambda e: e.tensor_reduce(out=Zt[:], in_=ee[:], axis=AX.X, op=ALU.add), ["ee"], ["Zt"])
                S.act(Zt[:], Zt[:], AF.Ln, ["Zt"], ["Zt"])
                S.tt("dve", mz[:], Zt[:], c8a[:, :, 0], ALU.add, ["Zt", "c8a"], ["mz"])
                rk = "res4_%d" % c
                S.copy("dve", res4[c][:, 0, :].rearrange("p (h a) -> p h a", a=16), sv1, ["sv"], [rk])
                S.ts("dve", res4[c][:, 1, :].rearrange("p (h a) -> p h a", a=16),
                     mz[:].unsqueeze(2).to_broadcast([128, 8, 16]), -1.0, None, ALU.mult, None, ["mz"], [rk])
                S.copy("dve", res4[c][:, 2, :].rearrange("p (h a) -> p h a", a=16),
                       c8b[:, :, 7:8].to_broadcast([128, 8, 16]), ["c8b"], [rk])

            def p5_C(g):
                t0 = g * 256
                for c in range(2):
                    for n_ in range(4):
                        pb = n_ % 2
                        S.tr(bank[pb][:, 0:128], res4[c][:, n_, :], ident[:], ["res4_%d" % c, "ident"], [BK[pb]])
                        S.act(stg[:, n_, c * 128:(c + 1) * 128], bank[pb][:, 0:128], AF.Copy, [BK[pb]], ["stg"])
                S.st(bT_s[:, t0:t0 + 256], stg[:, 0, :], "p5s1", ["stg"])
                S.st(nT_s[:, t0:t0 + 256], stg[:, 1, :], "p5s4", ["stg"])
                S.st(tT_s[:, t0:t0 + 256], stg[:, 2, :], "p5s2", ["stg"])
                S.st(iT_s[:, t0:t0 + 256], stg[:, 3, :], "p5s3", ["stg"])

            cin = [sb(ph, "cin%d" % i, [128, 1024]) for i in range(2)]
            cout = [sb(ph, "cout%d" % i, [128, 1024], BF16) for i in range(2)]
            conv_state = [0]

            def conv_some(n_items):
                for _ in range(n_items):
                    i = conv_state[0]
                    if i >= 256:
                        return
                    conv_state[0] += 1
                    k2 = i % 2
                    hf = (i % 2) * 1024
                    S.ld(cin[k2][:], UV_d[i // 2][:, hf:hf + 1024], "cin%d" % k2, ["cin%d" % k2])
                    S.act(cout[k2][:], cin[k2][:], AF.Copy, ["cin%d" % k2], ["cout%d" % k2])
                    S.st(UVh_s[i // 2][:, hf:hf + 1024], cout[k2][:], "cout%d" % k2, ["cout%d" % k2], q="act")

            per_blk = (256 + NB - 1) // NB
            p5_A1(0)
            p5_A2(0, 0)
            p5_A2(0, 1)
            p5_A3(0)
            for g in range(NB):
                nx = g + 1 < NB
                if nx:
                    p5_A1(g + 1)
                p5_B1(g, 0)
                if nx:
                    p5_A2(g + 1, 0)
                p5_B2(g, 0)
                p5_B1(g, 1)
                if nx:
                    p5_A2(g + 1, 1)
                    p5_A3(g + 1)
                p5_B2(g, 1)
                p5_C(g)
            conv_some(256)
            S.barrier()
            S.emit()

        with ExitStack() as ph:
            S.barrier()
            skT2 = sb(ph, "skT2", [128, 128])
            iota8 = sb(ph, "iota8", [128, 8, 128])
            fgbc = sb(ph, "fgbc", [128, D])
            hT = [sb(ph, "bhT%d" % i, [128, 8, 256], BF16) for i in range(2)]
            q2 = [sb(ph, "bq2%d" % i, [128, 8, 256]) for i in range(2)]
            bti = [sb(ph, "bbti%d" % i, [128, 4, 256]) for i in range(2)]
            x1b = [sb(ph, "bx1%d" % i, [128, 2, D]) for i in range(2)]
            Q2rep = [sb(ph, "Q2rep%d" % i, [128, 16, 128]) for i in range(2)]
            Xt = [sb(ph, "Xt%d" % i, [128, 8, 128]) for i in range(2)]
            Et = [sb(ph, "Et%d" % i, [128, 8, 128]) for i in range(2)]
            Lp = [sb(ph, "Lp%d" % i, [128, 8, 128]) for i in range(2)]
            WT = sb(ph, "WT", [128, 256, 128], F16)
            NUV = 6
            UVb = [sb(ph, "UVb%d" % i, [128, 2048], BF16) for i in range(NUV)]
            Gt = [sb(ph, "Gt%d" % i, [128, 256]) for i in range(2)]
            At = [sb(ph, "At%d" % i, [128, 256], BF16) for i in range(2)]
            x2 = sb(ph, "x2", [128, 2, D])
            ss = sb(ph, "bss", [128, 2])
            rstd = sb(ph, "brstd", [128, 2])
            junk = sb(ph, "bjunk", [128, D], BF16)
            S.ld(skT2[:], skT_d[1], "b_c1", ["skT2"])
            S.ld(iota8[:].rearrange("p a n -> p (a n)"), iota4_d, "b_c2", ["iota8"])
            S.ld(fgbc[:], fgbc_d, "b_c3", ["fgbc"])

            def b_load(bb):
                i = bb % 2
                t0 = bb * 256
                S.ld(hT[i][:], h2T_s[:, :, t0:t0 + 256].rearrange("k p t -> p k t"), "bhT%d" % i, ["bhT%d" % i])
                S.ld(q2[i][:], q2T_s[:, :, t0:t0 + 256].rearrange("h p t -> p h t"), "bq2%d" % i, ["bq2%d" % i])
                S.ld(bti[i][:, 0, :], bT_s[:, t0:t0 + 256], "bbti%d" % i, ["bbti%d" % i])
                S.ld(bti[i][:, 1, :], nT_s[:, t0:t0 + 256], "bbti%d" % i, ["bbti%d" % i])
                S.ld(bti[i][:, 2, :], tT_s[:, t0:t0 + 256], "bbti%d" % i, ["bbti%d" % i])
                S.ld(bti[i][:, 3, :], iT_s[:, t0:t0 + 256], "bbti%d" % i, ["bbti%d" % i])
                S.ld(x1b[i][:], x1_s[t0:t0 + 256, :].rearrange("(c p) d -> p c d", p=128), "bx1%d" % i, ["bx1%d" % i])

            nuv_ctr = [0]

            def uv_load(i):
                s_ = nuv_ctr[0] % NUV
                nuv_ctr[0] += 1
                S.ld(UVb[s_][:], UVh_s[i], "UVb%d" % s_, ["UVb%d" % s_])
                return s_

            b_load(0)
            ngrp = 0
            for bb in range(NB):
                bi = bb % 2
                hk, qk, btk, x1k = "bhT%d" % bi, "bq2%d" % bi, "bbti%d" % bi, "bx1%d" % bi
                if bb + 1 < NB:
                    b_load(bb + 1)
                pend = [uv_load(i_) for i_ in range(NUV)]
                def emit_q2rep(pr):
                    tq = pr * 16
                    qr = Q2rep[pr % 2]
                    qrk = "Q2rep%d" % (pr % 2)
                    S.act(qr[:].rearrange("k t (h a) -> k t h a", a=16),
                          q2[bi][:, :, tq:tq + 16].rearrange("k h t -> k t h").unsqueeze(3).to_broadcast([128, 16, 8, 16]),
                          AF.Copy, [qk], [qrk])

                def emit_s2rep(grp):
                    qr = Q2rep[(grp // 2) % 2]
                    qrk = "Q2rep%d" % ((grp // 2) % 2)
                    rb = 2 * (grp % 2)
                    for tt_ in range(8):
                        S.mm(bank[rb + tt_ // 4][:, (tt_ % 4) * 128:(tt_ % 4 + 1) * 128], qr[:, (grp % 2) * 8 + tt_, :], skT2[:],
                             True, True, [qrk, "skT2"], [BK[rb + tt_ // 4]])

                def emit_gate(grp):
                    tq = grp * 8
                    u = grp % 2
                    rb = 2 * u
                    wb = 4 + 2 * u
                    xk, ek, lk = "Xt%d" % u, "Et%d" % u, "Lp%d" % u
                    r3 = psall[:, rb:rb + 2, :].rearrange("p b (a n) -> p (b a) n", n=128)
                    rks = [BK[rb], BK[rb + 1]]

                    def bc(row):
                        return bti[bi][:, row, tq:tq + 8].unsqueeze(2).to_broadcast([128, 8, 128])
                    S.tt("dve", Xt[u][:], r3, bc(0), ALU.add, rks + [btk], [xk])
                    S.tt("dve", Et[u][:], Xt[u][:], bc(2), ALU.is_lt, [xk, btk], [ek])
                    S.op("dve", lambda e: e.scalar_tensor_tensor(out=Et[u][:], in0=Et[u][:], scalar=-1.0e4, in1=Xt[u][:],
                                                                 op0=ALU.mult, op1=ALU.add), [ek, xk], [ek])
                    for tt_ in range(8):
                        S.act(Et[u][:, tt_, :], Et[u][:, tt_, :], AF.Exp, [ek, btk], [ek],
                              bias=bti[bi][:, 1, tq + tt_:tq + tt_ + 1])
                    S.tt("dve", Lp[u][:], iota8[:], bc(3), ALU.is_equal, ["iota8", btk], [lk])
                    for tt_ in range(8):
                        S.mm(bank[wb + tt_ // 4][:, (tt_ % 4) * 128:(tt_ % 4 + 1) * 128], Et[u][:, tt_, :], Lp[u][:, tt_, :],
                             True, True, [ek, lk], [BK[wb + tt_ // 4]])

                def emit_gate_back(grp):
                    tq = grp * 8
                    wb = 4 + 2 * (grp % 2)
                    S.act(WT[:, tq:tq + 8, :], psall[:, wb:wb + 2, :].rearrange("p b (a n) -> p (b a) n", n=128), AF.Copy,
                          [BK[wb], BK[wb + 1]], ["WT"])

                emit_q2rep(0)
                emit_s2rep(0)
                emit_q2rep(1)
                for grp in range(32):
                    if grp + 1 < 32:
                        emit_s2rep(grp + 1)
                    if grp % 2 == 1 and grp // 2 + 2 < 16:
                        emit_q2rep(grp // 2 + 2)
                    emit_gate(grp)
                    if grp >= 1:
                        emit_gate_back(grp - 1)
                emit_gate_back(31)
                slot = {}

                def emit_A(i):
                    slot[i] = pend.pop(0)
                    s_ = slot[i]
                    ab = i % 2
                    for kc in range(8):
                        S.mm(bank[ab][:, 0:256], UVb[s_][:, kc * 128:(kc + 1) * 128], hT[bi][:, kc, :], kc == 0, kc == 7,
                             ["UVb%d" % s_, hk], [BK[ab]])

                def emit_V(i):
                    s_ = slot[i]
                    ab = i % 2
                    S.act(Gt[ab][:], bank[ab][:, 0:256], AF.Gelu, [BK[ab]], ["Gt%d" % ab])
                    S.tt("dve", At[ab][:], Gt[ab][:], WT[:, :, i], ALU.mult, ["Gt%d" % ab, "WT"], ["At%d" % ab])
                    for c in range(2):
                        for n in range(2):
                            yb = 4 + c * 2 + n
                            S.mm(bank[yb][:, :], At[ab][:, c * 128:(c + 1) * 128], UVb[s_][:, D + n * 512:D + (n + 1) * 512],
                                 i == 0, i == 127, ["At%d" % ab, "UVb%d" % s_], [BK[yb]])
                    if i + NUV < 128:
                        pend.append(uv_load(i + NUV))

                emit_A(0)
                for i in range(128):
                    if i + 1 < 128:
                        emit_A(i + 1)
                    emit_V(i)
                for c in range(2):
                    for n in range(2):
                        yb = 4 + c * 2 + n
                        S.tt("dve", x2[:, c, n * 512:(n + 1) * 512], bank[yb][:, :], g2bc[:, n * 512:(n + 1) * 512], ALU.mult,
                             [BK[yb], "g2bc"], ["x2"])
                    S.tt("pool", x2[:, c, :], x2[:, c, :], x1b[bi][:, c, :], ALU.add, ["x2", x1k], ["x2"])
                    S.act(junk[:], x2[:, c, :], AF.Square, ["x2"], ["bjunk"], accum_out=ss[:, c:c + 1])
                S.ts("dve", rstd[:], ss[:], 1.0 / D, 1e-6, ALU.mult, ALU.add, ["bss", "bjunk"], ["brstd"])
                S.act(rstd[:], rstd[:], AF.Sqrt, ["brstd"], ["brstd"])
                S.op("dve", lambda e: e.reciprocal(out=rstd[:], in_=rstd[:]), ["brstd"], ["brstd"])
                for c in range(2):
                    S.act(x2[:, c, :], x2[:, c, :], AF.Identity, ["x2", "brstd"], ["x2"], scale=rstd[:, c:c + 1])
                    S.tt("pool", x2[:, c, :], x2[:, c, :], fgbc[:], ALU.mult, ["x2", "fgbc"], ["x2"])
                S.st(y_d[bb * 256:(bb + 1) * 256, :].rearrange("(c p) d -> p c d", p=128), x2[:], "yout", ["x2"])
            S.barrier()
            S.emit()
        print("bass instructions:", S.ninst)
    return nc


def make_in_maps(inputs, S_TOK=8192, n_cores=8):
    f = lambda a: np.ascontiguousarray(a, dtype=np.float32)
    cst = _consts()
    w_ada = f(inputs["w_ada"][0])
    b_ada = inputs["b_ada"][0]
    shared = dict(
        w_ada=w_ada,
        b_adaT=f(b_ada.reshape(48, 128).T),
        b_ada_bc=f(np.broadcast_to(np.concatenate([b_ada[2048:3072], b_ada[5120:6144]])[None, :], (128, 2048))),
        n1g=f(inputs["norm1_g"][0].reshape(8, 128).T),
        n2g=f(inputs["norm2_g"][0].reshape(8, 128).T),
        fg_bc=f(np.broadcast_to(inputs["final_g"][None, :], (128, D))),
        w_in=f(inputs["w_in"][0]),
        pool_w=f(inputs["pool_w"][0]),
        pool_scT=f(inputs["pool_scale"][0].reshape(4, 128).T),
        w_out=f(inputs["w_out"][0]),
        wq=f(inputs["peer_wq"][0]),
        skT=f(np.transpose(inputs["peer_subkeys"][0], (0, 2, 1))),
        UV=f(np.concatenate([np.transpose(inputs["peer_u"][0].reshape(128, 128, 8, 128), (0, 3, 2, 1)).reshape(128, 128, 1024),
                             inputs["peer_v"][0].reshape(128, 128, 1024)], axis=2)),
        **cst,
    )
    maps = []
    for b in range(n_cores):
        m = dict(shared)
        m["x"] = f(inputs["x"][b, :S_TOK])
        m["cT"] = f(inputs["c"][b].reshape(8, 128).T)
        maps.append(m)
    return maps


def kernel(**inputs):
    nc = build(8192)
    in_maps = make_in_maps(inputs, 8192, 8)
    res = run_bass_kernel_spmd(nc, in_maps, core_ids=list(range(8)))
    return np.stack([np.asarray(r["y"], dtype=np.float32) for r in res.results], axis=0)
```

```python
import numpy as np
from contextlib import ExitStack
import concourse.bass as bass
import concourse.mybir as mybir
from concourse.bass_utils import run_bass_kernel_spmd

F32 = mybir.dt.float32
F16 = mybir.dt.float16
F32R = mybir.dt.float32r
BF16 = mybir.dt.bfloat16
U32 = mybir.dt.uint32
AF = mybir.ActivationFunctionType
ALU = mybir.AluOpType
AX = mybir.AxisListType

D = 1024
NH = 8
HD = 64
NEG = -1.0e30


class Sched:
    ENG = ("pe", "act", "dve", "pool", "sp")
    ENGN = {"pe": "tensor", "act": "scalar", "dve": "vector", "pool": "gpsimd", "sp": "sync"}

    def __init__(self, nc, stack):
        self.nc = nc
        self.stack = stack
        self.prog = {e: [] for e in self.ENG}
        self.cnt = {e: 0 for e in self.ENG}
        self.sem = {e: stack.enter_context(nc.semaphore("sem_" + e)) for e in self.ENG}
        self.waited = {e: {} for e in self.ENG}
        self.res = {}
        self.dsem = {}
        self.ninst = 0

    def dma_sem(self, name):
        if name not in self.dsem:
            self.dsem[name] = [self.stack.enter_context(self.nc.semaphore("d_" + name)), 0]
        return self.dsem[name]

    def _need(self, eng, tok, waits):
        if tok is None:
            return
        kind, a, b = tok
        if kind == "e" and a == eng and eng == "pe":
            return
        key = (kind, a)
        if self.waited[eng].get(key, 0) >= b:
            return
        if waits.get(key, 0) < b:
            waits[key] = b

    def _deps(self, eng, reads, writes):
        waits = {}
        for k in reads:
            r = self.res.setdefault(k, {"w": None, "r": {}})
            self._need(eng, r["w"], waits)
        for k in writes:
            r = self.res.setdefault(k, {"w": None, "r": {}})
            self._need(eng, r["w"], waits)
            for (kind, a), b in r["r"].items():
                self._need(eng, (kind, a, b), waits)
        for key, v in waits.items():
            self.waited[eng][key] = v
            self.prog[eng].append(("wait", key, v))

    def _record(self, tok, reads, writes):
        for k in reads:
            rr = self.res[k]["r"]
            key = (tok[0], tok[1])
            if rr.get(key, 0) < tok[2]:
                rr[key] = tok[2]
        for k in writes:
            self.res[k] = {"w": tok, "r": {}}

    def op(self, eng, fn, reads=(), writes=()):
        self._deps(eng, reads, writes)
        self.cnt[eng] += 1
        tok = ("e", eng, self.cnt[eng])
        self.prog[eng].append(("op", fn))
        self._record(tok, reads, writes)
        self.ninst += 1

    def dma(self, q, fn, semname, reads=(), writes=()):
        self._deps(q, reads, writes)
        s = self.dma_sem(semname)
        s[1] += 16
        tok = ("d", semname, s[1])
        self.prog[q].append(("dma", fn, semname))
        self._record(tok, reads, writes)
        self.ninst += 1

    def barrier(self):
        for e in self.ENG:
            for o in self.ENG:
                if o != e and self.cnt[o] > self.waited[e].get(("e", o), 0):
                    self.waited[e][("e", o)] = self.cnt[o]
                    self.prog[e].append(("wait", ("e", o), self.cnt[o]))
            for name, s in self.dsem.items():
                if s[1] > self.waited[e].get(("d", name), 0):
                    self.waited[e][("d", name)] = s[1]
                    self.prog[e].append(("wait", ("d", name), s[1]))

    def emit(self):
        nc = self.nc
        with nc.Block() as block:
            for e in self.ENG:
                prog = self.prog[e]

                def body(engine, prog=prog, e=e):
                    for it in prog:
                        if it[0] == "wait":
                            key, v = it[1], it[2]
                            h = self.sem[key[1]] if key[0] == "e" else self.dsem[key[1]][0]
                            engine.wait_ge(h, v)
                        elif it[0] == "op":
                            it[1](engine).then_inc(self.sem[e], 1)
                        else:
                            it[1](engine).then_inc(self.dsem[it[2]][0], 16)
                getattr(block, self.ENGN[e])(body)
        self.prog = {e: [] for e in self.ENG}

    def mm(self, out, lhsT, rhs, start, stop, reads, writes):
        self.op("pe", lambda e: e.matmul(out, lhsT=lhsT, rhs=rhs, start=start, stop=stop), reads, writes)

    def tr(self, out, in_, ident, reads, writes):
        self.op("pe", lambda e: e.transpose(out, in_, ident), reads, writes)

    def act(self, out, in_, func, reads, writes, scale=None, bias=None, accum_out=None):
        kw = {}
        if scale is not None:
            kw["scale"] = scale
        if bias is not None:
            kw["bias"] = bias
        if accum_out is not None:
            kw["accum_out"] = accum_out
        self.op("act", lambda e: e.activation(out=out, in_=in_, func=func, **kw), reads, writes)

    def tt(self, eng, out, in0, in1, op, reads, writes):
        self.op(eng, lambda e: e.tensor_tensor(out=out, in0=in0, in1=in1, op=op), reads, writes)

    def ts(self, eng, out, in0, s1, s2, op0, op1, reads, writes):
        if op1 is None:
            self.op(eng, lambda e: e.tensor_scalar(out=out, in0=in0, scalar1=s1, scalar2=None, op0=op0), reads, writes)
        else:
            self.op(eng, lambda e: e.tensor_scalar(out=out, in0=in0, scalar1=s1, scalar2=s2, op0=op0, op1=op1), reads, writes)

    def copy(self, eng, out, in_, reads, writes):
        self.op(eng, lambda e: e.tensor_copy(out=out, in_=in_), reads, writes)

    def memset(self, eng, ap, val, writes):
        self.op(eng, lambda e: e.memset(ap, val), (), writes)

    def ld(self, out, in_, sem, writes, q="sp"):
        self.dma(q, lambda e: e.dma_start(out=out, in_=in_), sem, (), writes)

    def st(self, out, in_, sem, reads, q="pool"):
        self.dma(q, lambda e: e.dma_start(out=out, in_=in_), sem, reads, ())


def _consts():
    ident = np.eye(128, dtype=np.float32)
    k = np.arange(128)[:, None, None]
    rel = np.arange(17)[None, :, None]
    q = np.arange(128)[None, None, :]
    dist = rel * 128 + q - k
    cnt = ((dist >= 0) & (dist <= 128)).astype(np.float32)
    cnt += ((dist >= 0) & (dist <= 512) & (dist % 4 == 0)).astype(np.float32)
    cnt += ((dist >= 0) & (dist <= 2048) & (dist % 16 == 0)).astype(np.float32)
    CT = np.ascontiguousarray(cnt.reshape(128, 17 * 128), dtype=np.float32)
    slopes = np.array([2.0 ** (-8.0 * (h + 1) / NH) for h in range(NH)], dtype=np.float64)
    kl = np.arange(128, dtype=np.float64)[:, None, None]
    bk = slopes[None, :, None] * (kl - 64.0) - slopes[None, :, None] * 128.0 * np.arange(17)[None, None, :]
    biasK = np.ascontiguousarray(bk.reshape(128, NH * 17), dtype=np.float32)
    bm = slopes[None, :, None] * (kl - 64.0) - slopes[None, :, None] * 128.0 * (np.arange(-3, 17)[None, None, :] + 1.5)
    biasM = np.ascontiguousarray(bm.reshape(128, NH * 20), dtype=np.float32)
    band = np.zeros((128, 4, 3, 128), dtype=np.float64)
    s = np.arange(128)[:, None]
    t = np.arange(128)[None, :]
    for pg, w in enumerate((2, 4, 8, 16)):
        cur = ((s <= t) & (s > t - w)).astype(np.float64) / w - (s == t)
        prev = ((t + 128 - s) < w).astype(np.float64) / w
        cntf = np.minimum(t + 1, w).astype(np.float64)
        first = ((s <= t) & (s > t - w)).astype(np.float64) / cntf - (s == t)
        band[:, pg, 0, :] = cur
        band[:, pg, 1, :] = prev
        band[:, pg, 2, :] = first
    band = np.ascontiguousarray(band.reshape(128, 12 * 128), dtype=np.float32)
    iota4 = np.ascontiguousarray(np.tile(np.arange(128, dtype=np.float32), (128, 8)))
    return dict(ident=ident, CT=CT, biasK=biasK, biasM=biasM, band=band, iota4=iota4)


def build(S_TOK=8192, debug=False):
    NCHK = S_TOK // 128
    NG = S_TOK // 512
    NB = S_TOK // 256
    nc = bass.Bass("TRN2", target_bir_lowering=False)
    scr_kind = "ExternalOutput" if debug else "Internal"

    def din(name, shape, dt=F32):
        return nc.dram_tensor(name, list(shape), dt, kind="ExternalInput").ap()

    def dscr(name, shape, dt=F32):
        return nc.dram_tensor(name, list(shape), dt, kind=scr_kind).ap()

    x_d = din("x", [S_TOK, D])
    cT_d = din("cT", [128, 8])
    wada_d = din("w_ada", [D, 6 * D])
    badaT_d = din("b_adaT", [128, 48])
    badabc_d = din("b_ada_bc", [128, 2 * D])
    n1g_d = din("n1g", [128, 8])
    n2g_d = din("n2g", [128, 8])
    fgbc_d = din("fg_bc", [128, D])
    win_d = din("w_in", [D, 2048], F32R)
    poolw_d = din("pool_w", [4, 128, 128], F32R)
    poolsc_d = din("pool_scT", [128, 4])
    wout_d = din("w_out", [D, D], F32R)
    wq_d = din("wq", [D, 2048])
    skT_d = din("skT", [2, 128, 128], F32R)
    UV_d = din("UV", [128, 128, 2048])
    ident_d = din("ident", [128, 128])
    CT_d = din("CT", [128, 17 * 128])
    biasK_d = din("biasK", [128, NH * 17])
    biasM_d = din("biasM", [128, NH * 20])
    band_d = din("band", [128, 12 * 128], F32R)
    iota4_d = din("iota4", [128, 1024])

    qT_s = dscr("qT_scr", [4, 128, S_TOK], F32R)
    kT_s = dscr("kT_scr", [4, 128, S_TOK], F32R)
    v_s = dscr("v_scr", [S_TOK, NH * 66], BF16)
    u_s = dscr("u_scr", [S_TOK, 512], F32R)
    attn_s = dscr("attn_scr", [S_TOK, 512])
    x1_s = dscr("x1_scr", [S_TOK, D])
    h2T_s = dscr("h2T_scr", [8, 128, S_TOK], BF16)
    UVh_s = dscr("UVh_scr", [128, 128, 2048], BF16)
    q2T_s = dscr("q2T_scr", [8, 128, S_TOK], F32R)
    bT_s = dscr("bT_scr", [128, S_TOK])
    nT_s = dscr("nT_scr", [128, S_TOK])
    tT_s = dscr("tT_scr", [128, S_TOK])
    iT_s = dscr("iT_scr", [128, S_TOK])
    y_d = nc.dram_tensor("y", [S_TOK, D], F32, kind="ExternalOutput").ap()

    with ExitStack() as top:
        S = Sched(nc, top)

        def sb(stack, name, shape, dt=F32):
            return stack.enter_context(nc.sbuf_tensor("s_" + name, list(shape), dt))

        psall = top.enter_context(nc.psum_tensor("psall", [128, 8, 512], F32))
        bank = [psall[:, i, :] for i in range(8)]
        BK = ["bank%d" % i for i in range(8)]

        ident = sb(top, "ident", [128, 128])
        scale1 = sb(top, "scale1", [128, 8])
        shift1 = sb(top, "shift1", [128, 8])
        scale2 = sb(top, "scale2", [128, 8])
        shift2 = sb(top, "shift2", [128, 8])
        g1bc = sb(top, "g1bc", [128, D])
        g2bc = sb(top, "g2bc", [128, D])
        S.ld(ident[:], ident_d, "c_ident", ["ident"])

        with ExitStack() as ph:
            cT = sb(ph, "cT", [128, 8])
            cs = sb(ph, "cs", [128, 8])
            csrep = sb(ph, "csrep", [128, 8, 128])
            badaT = sb(ph, "badaT", [128, 48])
            badabc = sb(ph, "badabc", [128, 2 * D])
            n1g = sb(ph, "n1g", [128, 8])
            n2g = sb(ph, "n2g", [128, 8])
            modc = sb(ph, "modc", [128, 48])
            wa = [sb(ph, "wa%d" % i, [128, 8, 512]) for i in range(2)]
            S.ld(cT[:], cT_d, "p1a", ["cT"])
            S.ld(badaT[:], badaT_d, "p1b", ["badaT"])
            S.ld(badabc[:], badabc_d, "p1c", ["badabc"])
            S.ld(n1g[:], n1g_d, "p1d", ["n1g"])
            S.ld(n2g[:], n2g_d, "p1e", ["n2g"])
            S.act(cs[:], cT[:], AF.Silu, ["cT"], ["cs"])
            S.copy("dve", csrep[:], cs[:].unsqueeze(2).to_broadcast([128, 8, 128]), ["cs"], ["csrep"])
            S.memset("dve", modc[:], 0.0, ["modc"])
            for n in range(12):
                w = wa[n % 2]
                wk = "wa%d" % (n % 2)
                S.ld(w[:], wada_d[:, n * 512:(n + 1) * 512].rearrange("(kc p) n -> p kc n", p=128), wk, [wk])
                if n in (4, 5, 10, 11):
                    pb = bank[n % 2]
                    for kc in range(8):
                        S.mm(pb[:, :], csrep[:, kc, :], w[:, kc, :], kc == 0, kc == 7, ["csrep", wk], [BK[n % 2]])
                    dst = g1bc if n < 6 else g2bc
                    dk = "g1bc" if n < 6 else "g2bc"
                    off = (n % 2) * 512
                    boff = off if n < 6 else D + off
                    S.tt("dve", dst[:, off:off + 512], pb[:, :], badabc[:, boff:boff + 512], ALU.add,
                         [BK[n % 2], "badabc"], [dk])
                else:
                    pb = bank[2 + n % 2]
                    for jj in range(4):
                        for kc in range(8):
                            S.mm(pb[:, jj:jj + 1], w[:, kc, jj * 128:(jj + 1) * 128], cs[:, kc:kc + 1],
                                 kc == 0, kc == 7, ["cs", wk], [BK[2 + n % 2]])
                    S.tt("dve", modc[:, 4 * n:4 * n + 4], pb[:, 0:4], badaT[:, 4 * n:4 * n + 4], ALU.add,
                         [BK[2 + n % 2], "badaT"], ["modc"])
            S.op("dve", lambda e: e.scalar_tensor_tensor(out=scale1[:], in0=modc[:, 8:16], scalar=1.0, in1=n1g[:],
                                                         op0=ALU.add, op1=ALU.mult), ["modc", "n1g"], ["scale1"])
            S.op("dve", lambda e: e.scalar_tensor_tensor(out=scale2[:], in0=modc[:, 32:40], scalar=1.0, in1=n2g[:],
                                                         op0=ALU.add, op1=ALU.mult), ["modc", "n2g"], ["scale2"])
            S.copy("dve", shift1[:], modc[:, 0:8], ["modc"], ["shift1"])
            S.copy("dve", shift2[:], modc[:, 24:32], ["modc"], ["shift2"])
            S.barrier()
            S.emit()

        def norm_transpose(xt, xk, xn, xnk, hT, hTk, ss, rstd, nch, scol, scolk, bcol, bcolk, pbanks, hT2=None, hT2k=None):
            for c in range(nch):
                S.act(xn[:, c, :], xt[:, c, :], AF.Square, [xk], [xnk], accum_out=ss[:, c:c + 1])
            S.ts("dve", rstd[:, 0:nch], ss[:, 0:nch], 1.0 / D, 1e-6, ALU.mult, ALU.add, ["ss", xnk], ["rstd"])
            S.act(rstd[:, 0:nch], rstd[:, 0:nch], AF.Sqrt, ["rstd"], ["rstd"])
            S.op("dve", lambda e: e.reciprocal(out=rstd[:, 0:nch], in_=rstd[:, 0:nch]), ["rstd"], ["rstd"])
            for c in range(nch):
                S.act(xn[:, c, :], xt[:, c, :], AF.Identity, [xk, "rstd"], [xnk], scale=rstd[:, c:c + 1])
            for kc in range(8):
                pb = pbanks[kc % 2]
                for c in range(nch):
                    S.tr(bank[pb][:, c * 128:(c + 1) * 128], xn[:, c, kc * 128:(kc + 1) * 128], ident[:],
                         [xnk, "ident"], [BK[pb]])
                S.act(hT[:, kc, 0:nch * 128], bank[pb][:, 0:nch * 128], AF.Identity, [BK[pb], scolk, bcolk], [hTk],
                      scale=scol[:, kc:kc + 1], bias=bcol[:, kc:kc + 1])
                if hT2 is not None:
                    S.act(hT2[:, kc, 0:nch * 128], bank[pb][:, 0:nch * 128], AF.Identity, [BK[pb], scolk, bcolk], [hT2k],
                          scale=scol[:, kc:kc + 1], bias=bcol[:, kc:kc + 1])

        with ExitStack() as ph:
            S.barrier()
            win = sb(ph, "win", [128, 8, 2048], F32R)
            xt = [sb(ph, "p2x%d" % i, [128, 4, D]) for i in range(2)]
            xn = sb(ph, "p2xn", [128, 4, D])
            hT = sb(ph, "p2hT", [128, 8, 512], F32R)
            ss = sb(ph, "p2ss", [128, 4])
            rstd = sb(ph, "p2rstd", [128, 4])
            qst = sb(ph, "p2q", [128, 4, 512], F32R)
            kst = sb(ph, "p2k", [128, 4, 512], F32R)
            vst = sb(ph, "p2v", [128, 4, NH, 66], BF16)
            ones_t = sb(ph, "p2ones", [128, 4 * NH * 66])
            ust = sb(ph, "p2u", [128, 4, 512], F32R)
            for i in range(4):
                S.ld(win[:, :, i * 512:(i + 1) * 512],
                     win_d[:, i * 512:(i + 1) * 512].rearrange("(kc p) n -> p kc n", p=128), "win", ["win"], q="pool")
            S.memset("dve", ones_t[:], 1.0, ["ones_t"])
            S.copy("dve", vst[:].rearrange("p c h f -> p (c h f)"), ones_t[:], ["ones_t"], ["vst"])
            xv = x_d.rearrange("(g c p) d -> g p c d", p=128, c=4)
            S.ld(xt[0][:], xv[0], "p2x0", ["p2x0"])
            for g in range(NG):
                xk = "p2x%d" % (g % 2)
                if g + 1 < NG:
                    S.ld(xt[(g + 1) % 2][:], xv[g + 1], "p2x%d" % ((g + 1) % 2), ["p2x%d" % ((g + 1) % 2)])
                norm_transpose(xt[g % 2], xk, xn, "p2xn", hT, "p2hT", ss, rstd, 4,
                               scale1, "scale1", shift1, "shift1", (0, 1))
                t0 = g * 512
                for hp in range(4):
                    pb = 2 + hp % 2
                    for kc in range(8):
                        S.mm(bank[pb][:, :], win[:, kc, hp * 128:(hp + 1) * 128], hT[:, kc, :], kc == 0, kc == 7,
                             ["win", "p2hT"], [BK[pb]])
                    S.act(qst[:, hp, :], bank[pb][:, :], AF.Copy, [BK[pb]], ["qst"], scale=0.125)
                for hp in range(4):
                    pb = 2 + hp % 2
                    for kc in range(8):
                        S.mm(bank[pb][:, :], win[:, kc, 512 + hp * 128:512 + (hp + 1) * 128], hT[:, kc, :],
                             kc == 0, kc == 7, ["win", "p2hT"], [BK[pb]])
                    S.copy("dve", kst[:, hp, :], bank[pb][:, :], [BK[pb]], ["kst"])
                for c in range(4):
                    pb = 4 + c % 2
                    for kc in range(8):
                        S.mm(bank[pb][:, :], hT[:, kc, c * 128:(c + 1) * 128], win[:, kc, 1024:1536], kc == 0, kc == 7,
                             ["win", "p2hT"], [BK[pb]])
                    S.act(vst[:, c, :, 0:64], bank[pb][:, :].rearrange("p (h f) -> p h f", f=64), AF.Copy,
                          [BK[pb]], ["vst"])
                for c in range(4):
                    pb = 6 + c % 2
                    for kc in range(8):
                        S.mm(bank[pb][:, :], hT[:, kc, c * 128:(c + 1) * 128], win[:, kc, 1536:2048], kc == 0, kc == 7,
                             ["win", "p2hT"], [BK[pb]])
                    S.copy("dve", ust[:, c, :], bank[pb][:, :], [BK[pb]], ["ust"])
                S.st(qT_s[:, :, t0:t0 + 512].rearrange("h p t -> p h t"), qst[:], "p2sq", ["qst"])
                S.st(kT_s[:, :, t0:t0 + 512].rearrange("h p t -> p h t"), kst[:], "p2sk", ["kst"])
                S.st(v_s[t0:t0 + 512, :].rearrange("(c p) f -> p c f", p=128), vst[:].rearrange("p c h f -> p c (h f)"),
                     "p2sv", ["vst"])
                S.st(u_s[t0:t0 + 512, :].rearrange("(c p) f -> p c f", p=128), ust[:], "p2su", ["ust"])
            S.barrier()
            S.emit()

        with ExitStack() as ph:
            S.barrier()
            CT = sb(ph, "CT", [128, 17 * 128])
            biasK = sb(ph, "biasK", [128, NH * 17])
            biasM = sb(ph, "biasM", [128, NH * 20])
            KT = sb(ph, "KT", [128, S_TOK], F32R)
            Vp = sb(ph, "Vp", [128, NCHK, 2, 66], BF16)
            QTg = [sb(ph, "QTg%d" % i, [128, 512], F32R) for i in range(2)]
            PT = [sb(ph, "PT%d" % i, [128, 512]) for i in range(2)]
            PTm = [[sb(ph, "PTm%d_%d" % (p_, i), [128, 512], BF16) for i in range(20)] for p_ in range(2)]
            ao = [sb(ph, "ao%d" % i, [128, 4, 128]) for i in range(2)]
            rden = sb(ph, "rden", [128, 8])
            S.ld(CT[:], CT_d, "p3c", ["CT"])
            S.ld(biasK[:], biasK_d, "p3b", ["biasK"])
            S.ld(biasM[:], biasM_d, "p3b2", ["biasM"])
            cnt3 = dict(nrd=0, nsb=0, nob=0, npt=0)
            units = [(hp, g, hh) for hp in range(4) for g in range(NG) for hh in range(2)]

            def p3_S(n):
                hp, g, hh = units[n]
                par = n % 2
                it = hp * NG + g
                qk = "QTg%d" % (it % 2)
                qt = QTg[it % 2]
                if g == 0 and hh == 0:
                    npc = max(1, S_TOK // 2048)
                    for i in range(npc):
                        w = S_TOK // npc
                        S.ld(KT[:, i * w:(i + 1) * w], kT_s[hp, :, i * w:(i + 1) * w], "p3k", ["KT"], q="pool")
                if hh == 0:
                    S.ld(qt[:], qT_s[hp, :, g * 512:(g + 1) * 512], qk, [qk], q="pool")
                h = 2 * hp + hh
                r0 = 64 * hh
                kbs = list(range(max(0, 4 * g - 16), 4 * g + 4))
                steps = []
                for kb in kbs:
                    steps.append(lambda kb=kb: s_step(kb, hp, g, hh, h, r0, par, qk, qt))
                return steps

            def s_step(kb, hp, g, hh, h, r0, par, qk, qt):
                if True:
                    kbi = kb - (4 * g - 16)
                    sbk = cnt3["nsb"] % 2
                    cnt3["nsb"] += 1
                    S.mm(bank[sbk][:, :], KT[r0:r0 + 64, kb * 128:(kb + 1) * 128], qt[r0:r0 + 64, :], True, True,
                         ["KT", qk], [BK[sbk]])
                    ptk = "PT%d" % (cnt3["npt"] % 2)
                    ptt = PT[cnt3["npt"] % 2]
                    cnt3["npt"] += 1
                    js = [j for j in range(4) if 0 <= 4 * g + j - kb <= 16]
                    j0, j1 = js[0], js[-1]
                    if h >= 2:
                        r_ = 4 * g - kb
                        S.act(ptt[:, j0 * 128:(j1 + 1) * 128], bank[sbk][:, j0 * 128:(j1 + 1) * 128], AF.Exp,
                              [BK[sbk], "biasM"], [ptk], bias=biasM[:, h * 20 + r_ + 3:h * 20 + r_ + 4])
                    else:
                        for j in js:
                            rel = 4 * g + j - kb
                            S.act(ptt[:, j * 128:(j + 1) * 128], bank[sbk][:, j * 128:(j + 1) * 128], AF.Exp,
                                  [BK[sbk], "biasK"], [ptk], bias=biasK[:, h * 17 + rel:h * 17 + rel + 1])
                    rel0 = 4 * g + j0 - kb
                    S.tt("dve", PTm[par][kbi][:, j0 * 128:(j1 + 1) * 128], ptt[:, j0 * 128:(j1 + 1) * 128],
                         CT[:, rel0 * 128:(rel0 + j1 - j0 + 1) * 128], ALU.mult, [ptk, "CT"], ["PTm%d_%d" % (par, kbi)])

            def p3_V(n):
                hp, g, hh = units[n]
                par = n % 2
                it = hp * NG + g
                aok = "ao%d" % (it % 2)
                aot = ao[it % 2]
                if g == 0 and hh == 0:
                    nvp = max(1, NCHK // 8)
                    for i in range(nvp):
                        nb_ = NCHK // nvp
                        S.ld(Vp[:, i * nb_:(i + 1) * nb_, :, :].rearrange("p b h f -> p b (h f)"),
                             v_s[i * nb_ * 128:(i + 1) * nb_ * 128, hp * 132:(hp + 1) * 132].rearrange("(b p) f -> p b f", p=128),
                             "p3v", ["Vp"], q="sp")
                kbs = list(range(max(0, 4 * g - 16), 4 * g + 4))
                steps = []
                for j in range(4):
                    steps.append(lambda j=j: v_step(j, hp, g, hh, par, aok, aot, kbs))
                return steps

            def v_step(j, hp, g, hh, par, aok, aot, kbs):
                if True:
                    obk = 2 + cnt3["nob"] % 2
                    cnt3["nob"] += 1
                    kl = [kb for kb in kbs if 0 <= 4 * g + j - kb <= 16]
                    for n_, kb in enumerate(kl):
                        kbi = kb - (4 * g - 16)
                        S.mm(bank[obk][:, 0:66], PTm[par][kbi][:, j * 128:(j + 1) * 128], Vp[:, kb, hh, :],
                             n_ == 0, n_ == len(kl) - 1, ["PTm%d_%d" % (par, kbi), "Vp"], [BK[obk]])
                    rc = cnt3["nrd"] % 8
                    cnt3["nrd"] += 1
                    S.op("dve", lambda e, obk=obk, rc=rc: e.reciprocal(out=rden[:, rc:rc + 1], in_=bank[obk][:, 64:65]),
                         [BK[obk]], ["rden%d" % rc])
                    S.act(aot[:, j, hh * 64:(hh + 1) * 64], bank[obk][:, 0:64], AF.Identity,
                          [BK[obk], "rden%d" % rc], [aok], scale=rden[:, rc:rc + 1])
                if hh == 1 and j == 3:
                    S.st(attn_s[g * 512:(g + 1) * 512, hp * 128:(hp + 1) * 128].rearrange("(c p) f -> p c f", p=128),
                         aot[:], aok, [aok], q="act")

            for st_ in p3_S(0):
                st_()
            for n in range(len(units)):
                vs = p3_V(n)
                ss_ = p3_S(n + 1) if n + 1 < len(units) else []
                ns = len(ss_)
                vi = 0
                for i_, st_ in enumerate(ss_):
                    st_()
                    while vi < 4 and (i_ + 1) * 4 >= (vi + 1) * ns:
                        vs[vi]()
                        vi += 1
                while vi < 4:
                    vs[vi]()
                    vi += 1
            S.barrier()
            S.emit()

        with ExitStack() as ph:
            S.barrier()
            wout = sb(ph, "wout", [128, 8, D], F32R)
            band = sb(ph, "band", [128, 12 * 128], F32R)
            poolw = sb(ph, "poolw", [128, 4, 128], F32R)
            poolsc = sb(ph, "poolsc", [128, 4])
            xt = [sb(ph, "p4x%d" % i, [128, 4, D]) for i in range(2)]
            at = [sb(ph, "p4a%d" % i, [128, 4, 512]) for i in range(2)]
            ub = [sb(ph, "p4u%d" % i, [128, 5, 512], F32R) for i in range(2)]
            concatT = sb(ph, "concatT", [128, 8, 512], F32R)
            mixT = sb(ph, "mixT", [128, 4, 512], F32R)
            x1t = sb(ph, "x1t", [128, 4, D])
            for i in range(2):
                S.ld(wout[:, :, i * 512:(i + 1) * 512],
                     wout_d[:, i * 512:(i + 1) * 512].rearrange("(kc p) n -> p kc n", p=128), "wout", ["wout"], q="pool")
            S.ld(band[:], band_d, "p4c1", ["band"], q="pool")
            S.ld(poolw[:], poolw_d.rearrange("g c e -> c g e"), "p4c2", ["poolw"], q="pool")
            S.ld(poolsc[:], poolsc_d, "p4c3", ["poolsc"])
            xv = x_d.rearrange("(g c p) d -> g p c d", p=128, c=4)
            av = attn_s.rearrange("(g c p) d -> g p c d", p=128, c=4)
            uv = u_s.rearrange("(g c p) d -> g p c d", p=128, c=4)

            def p4_load(g):
                i = g % 2
                S.ld(xt[i][:], xv[g], "p4x%d" % i, ["p4x%d" % i])
                S.ld(at[i][:], av[g], "p4a%d" % i, ["p4a%d" % i])
                S.ld(ub[i][:, 1:5, :], uv[g], "p4u%d" % i, ["p4u%d" % i], q="pool")
                if g > 0:
                    S.ld(ub[i][:, 0, :], u_s[g * 512 - 128:g * 512, :], "p4u%d" % i, ["p4u%d" % i], q="pool")
            p4_load(0)
            for g in range(NG):
                i = g % 2
                xk, ak, uk = "p4x%d" % i, "p4a%d" % i, "p4u%d" % i
                if g + 1 < NG:
                    p4_load(g + 1)
                for kc in range(4):
                    pb = kc % 2
                    for c in range(4):
                        S.tr(bank[pb][:, c * 128:(c + 1) * 128], at[i][:, c, kc * 128:(kc + 1) * 128], ident[:],
                             [ak, "ident"], [BK[pb]])
                    S.act(concatT[:, kc, :], bank[pb][:, :], AF.Copy, [BK[pb]], ["concatT"])
                for pg in range(4):
                    pb = 2 + pg % 2
                    for c in range(4):
                        o = bank[pb][:, c * 128:(c + 1) * 128]
                        if 4 * g + c == 0:
                            S.mm(o, ub[i][:, 1 + c, pg * 128:(pg + 1) * 128], band[:, (pg * 3 + 2) * 128:(pg * 3 + 3) * 128],
                                 True, True, [uk, "band"], [BK[pb]])
                        else:
                            S.mm(o, ub[i][:, 1 + c, pg * 128:(pg + 1) * 128], band[:, (pg * 3 + 0) * 128:(pg * 3 + 1) * 128],
                                 True, False, [uk, "band"], [BK[pb]])
                            S.mm(o, ub[i][:, c, pg * 128:(pg + 1) * 128], band[:, (pg * 3 + 1) * 128:(pg * 3 + 2) * 128],
                                 False, True, [uk, "band"], [BK[pb]])
                    S.copy("dve", mixT[:, pg, :], bank[pb][:, :], [BK[pb]], ["mixT"])
                for pg in range(4):
                    pb = 4 + pg % 2
                    S.mm(bank[pb][:, :], poolw[:, pg, :], mixT[:, pg, :], True, True, ["poolw", "mixT"], [BK[pb]])
                    S.act(concatT[:, 4 + pg, :], bank[pb][:, :], AF.Identity, [BK[pb], "poolsc"], ["concatT"],
                          scale=poolsc[:, pg:pg + 1])
                for c in range(4):
                    for n in range(2):
                        pb = 6 + n
                        for kc in range(8):
                            S.mm(bank[pb][:, :], concatT[:, kc, c * 128:(c + 1) * 128], wout[:, kc, n * 512:(n + 1) * 512],
                                 kc == 0, kc == 7, ["concatT", "wout"], [BK[pb]])
                        S.tt("dve", x1t[:, c, n * 512:(n + 1) * 512], bank[pb][:, :], g1bc[:, n * 512:(n + 1) * 512], ALU.mult,
                             [BK[pb], "g1bc"], ["x1t"])
                    S.tt("pool", x1t[:, c, :], x1t[:, c, :], xt[i][:, c, :], ALU.add, ["x1t", xk], ["x1t"])
                S.st(x1_s[g * 512:(g + 1) * 512, :].rearrange("(c p) d -> p c d", p=128), x1t[:], "p4s", ["x1t"])
            S.barrier()
            S.emit()

        with ExitStack() as ph:
            S.barrier()
            wq = sb(ph, "wq", [128, 8, 2048])
            skT = sb(ph, "skT", [128, 2, 128], F32R)
            xt = [sb(ph, "p5x%d" % i, [128, 2, D]) for i in range(2)]
            xn = sb(ph, "p5xn", [128, 2, D])
            hT = sb(ph, "p5hT", [128, 8, 256])
            hTr = sb(ph, "p5hTr", [128, 8, 256], BF16)
            ss = sb(ph, "p5ss", [128, 4])
            rstd = sb(ph, "p5rstd", [128, 4])
            QT = sb(ph, "p5QT", [128, 16, 256], F32R)
            Ssb = [[sb(ph, "Ssb%d_%d" % (a_, c_), [128, 16, 128]) for c_ in range(2)] for a_ in range(2)]
            res4 = [sb(ph, "res4_%d" % c_, [128, 4, 128]) for c_ in range(2)]
            Sm = sb(ph, "Sm", [128, 16, 128])
            sv = sb(ph, "sv", [128, 16, 16])
            si = sb(ph, "si", [128, 8, 16], U32)
            sif = sb(ph, "sif", [128, 8, 16])
            cand = sb(ph, "cand", [128, 8, 256])
            candm = Sm[:].rearrange("p a n -> p (a n)").rearrange("p (h c) -> p h c", c=256)
            ee = sb(ph, "ee", [128, 8, 256])
            c8a = sb(ph, "c8a", [128, 8, 8])
            c8b = sb(ph, "c8b", [128, 8, 8])
            Zt = sb(ph, "Zt", [128, 8])
            mz = sb(ph, "mz", [128, 8])
            b1 = sb(ph, "b1", [128, 8, 16])
            nz = sb(ph, "nz", [128, 8, 16])
            th = sb(ph, "th", [128, 8, 16])
            stg = sb(ph, "stg", [128, 4, 256])
            for i in range(4):
                S.ld(wq[:, :, i * 512:(i + 1) * 512],
                     wq_d[:, i * 512:(i + 1) * 512].rearrange("(kc p) n -> p kc n", p=128), "wq", ["wq"])
            S.ld(skT[:], skT_d.rearrange("h k n -> k h n"), "p5c", ["skT"], q="pool")
            xv = x1_s.rearrange("(g c p) d -> g p c d", p=128, c=2)
            S.ld(xt[0][:], xv[0], "p5x0", ["p5x0"])
            sv4 = sv[:].rearrange("p (h two) a -> p h two a", two=2)
            sv1 = sv4[:, :, 0, :]
            sv2 = sv4[:, :, 1, :]
            def p5_A1(g):
                xk = "p5x%d" % (g % 2)
                if g + 1 < NB:
                    S.ld(xt[(g + 1) % 2][:], xv[g + 1], "p5x%d" % ((g + 1) % 2), ["p5x%d" % ((g + 1) % 2)])
                norm_transpose(xt[g % 2], xk, xn, "p5xn", hT, "p5hT", ss, rstd, 2,
                               scale2, "scale2", shift2, "shift2", (0, 1), hTr, "p5hTr")
                t0 = g * 256
                S.st(h2T_s[:, :, t0:t0 + 256].rearrange("k p t -> p k t"), hTr[:], "p5sh", ["p5hTr"])

            def p5_A2(g, half):
                for hpq in range(8 * half, 8 * half + 8):
                    pb = 2 + hpq % 2
                    for kc in range(8):
                        S.mm(bank[pb][:, 0:256], wq[:, kc, hpq * 128:(hpq + 1) * 128], hT[:, kc, :], kc == 0, kc == 7,
                             ["wq", "p5hT"], [BK[pb]])
                    S.act(QT[:, hpq, :], bank[pb][:, 0:256], AF.Copy, [BK[pb]], ["p5QT"])
                    if hpq % 2 == 1:
                        conv_some(1)

            def p5_A3(g):
                t0 = g * 256
                S.st(q2T_s[:, :, t0:t0 + 256].rearrange("h p t -> p h t"),
                     QT[:].rearrange("p (h two) t -> p h two t", two=2)[:, :, 1, :], "p5sq", ["p5QT"])
                for c in range(2):
                    sk_ = "Ssb%d_%d" % (g % 2, c)
                    for b4 in range(4):
                        pb = 4 + b4
                        for k4 in range(4):
                            hpq = b4 * 4 + k4
                            S.mm(bank[pb][:, k4 * 128:(k4 + 1) * 128], QT[:, hpq, c * 128:(c + 1) * 128], skT[:, hpq % 2, :],
                                 True, True, ["p5QT", "skT"], [BK[pb]])
                        S.act(Ssb[g % 2][c][:, b4 * 4:(b4 + 1) * 4, :], bank[pb][:, :].rearrange("p (a n) -> p a n", n=128), AF.Copy,
                              [BK[pb]], [sk_])

            def p5_B1(g, c):
                sk_ = "Ssb%d_%d" % (g % 2, c)
                Sc = Ssb[g % 2][c]
                for hpq in range(16):
                    S.op("dve", lambda e, hpq=hpq: e.max(out=sv[:, hpq, 0:8], in_=Sc[:, hpq, :]), [sk_], ["sv"])
                    S.op("dve", lambda e, hpq=hpq: e.match_replace(out=Sm[:, hpq, :], in_to_replace=sv[:, hpq, 0:8],
                                                                 in_values=Sc[:, hpq, :], imm_value=NEG),
                         [sk_, "sv"], ["Sm"])
                    S.op("dve", lambda e, hpq=hpq: e.max(out=sv[:, hpq, 8:16], in_=Sm[:, hpq, :]), ["Sm"], ["sv"])
                for h in range(8):
                    S.op("dve", lambda e, h=h: e.max_index(out=si[:, h, 0:8], in_max=sv[:, 2 * h, 0:8],
                                                          in_values=Sc[:, 2 * h, :]), [sk_, "sv"], ["si"])
                    S.op("dve", lambda e, h=h: e.max_index(out=si[:, h, 8:16], in_max=sv[:, 2 * h, 8:16],
                                                          in_values=Sm[:, 2 * h, :]), ["Sm", "sv"], ["si"])
                S.copy("dve", res4[c][:, 3, :].rearrange("p (h a) -> p h a", a=16), si[:], ["si"], ["res4_%d" % c])
                S.tt("dve", cand[:].rearrange("p h (a b) -> p h a b", b=16),
                     sv1.unsqueeze(3).to_broadcast([128, 8, 16, 16]),
                     sv2.unsqueeze(2).to_broadcast([128, 8, 16, 16]), ALU.add, ["sv"], ["cand"])
                for h in range(8):
                    S.op("dve", lambda e, h=h: e.max(out=c8a[:, h, :], in_=cand[:, h, :]), ["cand"], ["c8a"])
                    S.op("dve", lambda e, h=h: e.match_replace(out=candm[:, h, :], in_to_replace=c8a[:, h, :],
                                                             in_values=cand[:, h, :], imm_value=NEG),
                         ["cand", "c8a"], ["Sm"])
                    S.op("dve", lambda e, h=h: e.max(out=c8b[:, h, :], in_=candm[:, h, :]), ["Sm"], ["c8b"])
                S.tt("dve", ee[:], cand[:], c8a[:, :, 0:1].to_broadcast([128, 8, 256]), ALU.subtract,
                     ["cand", "c8a"], ["ee"])

            def p5_B2(g, c):
                S.act(ee[:], ee[:], AF.Exp, ["ee"], ["ee"])
                S.tt("dve", candm[:], cand[:], c8b[:, :, 7:8].to_broadcast([128, 8, 256]), ALU.is_ge,
                     ["cand", "c8b"], ["Sm"])
                S.tt("dve", ee[:], ee[:], candm[:], ALU.mult, ["ee", "Sm"], ["ee"])
                S.op("dve", lambda e: e.tensor_reduce(out=Zt[:], in_=ee[:], axis=AX.X, op=ALU.add), ["ee"], ["Zt"])
                S.act(Zt[:], Zt[:], AF.Ln, ["Zt"], ["Zt"])
                S.tt("dve", mz[:], Zt[:], c8a[:, :, 0], ALU.add, ["Zt", "c8a"], ["mz"])
                rk = "res4_%d" % c
                S.copy("dve", res4[c][:, 0, :].rearrange("p (h a) -> p h a", a=16), sv1, ["sv"], [rk])
                S.ts("dve", res4[c][:, 1, :].rearrange("p (h a) -> p h a", a=16),
                     mz[:].unsqueeze(2).to_broadcast([128, 8, 16]), -1.0, None, ALU.mult, None, ["mz"], [rk])
                S.copy("dve", res4[c][:, 2, :].rearrange("p (h a) -> p h a", a=16),
                       c8b[:, :, 7:8].to_broadcast([128, 8, 16]), ["c8b"], [rk])

            def p5_C(g):
                t0 = g * 256
                for c in range(2):
                    for n_ in range(4):
                        pb = n_ % 2
                        S.tr(bank[pb][:, 0:128], res4[c][:, n_, :], ident[:], ["res4_%d" % c, "ident"], [BK[pb]])
                        S.act(stg[:, n_, c * 128:(c + 1) * 128], bank[pb][:, 0:128], AF.Copy, [BK[pb]], ["stg"])
                S.st(bT_s[:, t0:t0 + 256], stg[:, 0, :], "p5s1", ["stg"])
                S.st(nT_s[:, t0:t0 + 256], stg[:, 1, :], "p5s4", ["stg"])
                S.st(tT_s[:, t0:t0 + 256], stg[:, 2, :], "p5s2", ["stg"])
                S.st(iT_s[:, t0:t0 + 256], stg[:, 3, :], "p5s3", ["stg"])

            cin = [sb(ph, "cin%d" % i, [128, 1024]) for i in range(2)]
            cout = [sb(ph, "cout%d" % i, [128, 1024], BF16) for i in range(2)]
            conv_state = [0]

            def conv_some(n_items):
                for _ in range(n_items):
                    i = conv_state[0]
                    if i >= 256:
                        return
                    conv_state[0] += 1
                    k2 = i % 2
                    hf = (i % 2) * 1024
                    S.ld(cin[k2][:], UV_d[i // 2][:, hf:hf + 1024], "cin%d" % k2, ["cin%d" % k2])
                    S.act(cout[k2][:], cin[k2][:], AF.Copy, ["cin%d" % k2], ["cout%d" % k2])
                    S.st(UVh_s[i // 2][:, hf:hf + 1024], cout[k2][:], "cout%d" % k2, ["cout%d" % k2], q="act")

            per_blk = (256 + NB - 1) // NB
            p5_A1(0)
            p5_A2(0, 0)
            p5_A2(0, 1)
            p5_A3(0)
            for g in range(NB):
                nx = g + 1 < NB
                if nx:
                    p5_A1(g + 1)
                p5_B1(g, 0)
                if nx:
                    p5_A2(g + 1, 0)
                p5_B2(g, 0)
                p5_B1(g, 1)
                if nx:
                    p5_A2(g + 1, 1)
                    p5_A3(g + 1)
                p5_B2(g, 1)
                p5_C(g)
            conv_some(256)
            S.barrier()
            S.emit()

        with ExitStack() as ph:
            S.barrier()
            skT2 = sb(ph, "skT2", [128, 128], F32R)
            iota8 = sb(ph, "iota8", [128, 8, 128])
            fgbc = sb(ph, "fgbc", [128, D])
            hT = [sb(ph, "bhT%d" % i, [128, 8, 256], BF16) for i in range(2)]
            q2 = [sb(ph, "bq2%d" % i, [128, 8, 256], F32R) for i in range(2)]
            bti = [sb(ph, "bbti%d" % i, [128, 4, 256]) for i in range(2)]
            x1b = [sb(ph, "bx1%d" % i, [128, 2, D]) for i in range(2)]
            Q2rep = [sb(ph, "Q2rep%d" % i, [128, 16, 128], F32R) for i in range(2)]
            Xt = [sb(ph, "Xt%d" % i, [128, 8, 128]) for i in range(2)]
            Et = [sb(ph, "Et%d" % i, [128, 8, 128]) for i in range(2)]
            Lp = [sb(ph, "Lp%d" % i, [128, 8, 128]) for i in range(2)]
            WT = sb(ph, "WT", [128, 256, 128], F16)
            NUV = 6
            UVb = [sb(ph, "UVb%d" % i, [128, 2048], BF16) for i in range(NUV)]
            Gt = [sb(ph, "Gt%d" % i, [128, 256]) for i in range(2)]
            At = [sb(ph, "At%d" % i, [128, 256], BF16) for i in range(2)]
            x2 = sb(ph, "x2", [128, 2, D])
            ss = sb(ph, "bss", [128, 2])
            rstd = sb(ph, "brstd", [128, 2])
            junk = sb(ph, "bjunk", [128, D], BF16)
            S.ld(skT2[:], skT_d[1], "b_c1", ["skT2"], q="pool")
            S.ld(iota8[:].rearrange("p a n -> p (a n)"), iota4_d, "b_c2", ["iota8"])
            S.ld(fgbc[:], fgbc_d, "b_c3", ["fgbc"])

            def b_load(bb):
                i = bb % 2
                t0 = bb * 256
                S.ld(hT[i][:], h2T_s[:, :, t0:t0 + 256].rearrange("k p t -> p k t"), "bhT%d" % i, ["bhT%d" % i])
                S.ld(q2[i][:], q2T_s[:, :, t0:t0 + 256].rearrange("h p t -> p h t"), "bq2%d" % i, ["bq2%d" % i], q="pool")
                S.ld(bti[i][:, 0, :], bT_s[:, t0:t0 + 256], "bbti%d" % i, ["bbti%d" % i])
                S.ld(bti[i][:, 1, :], nT_s[:, t0:t0 + 256], "bbti%d" % i, ["bbti%d" % i])
                S.ld(bti[i][:, 2, :], tT_s[:, t0:t0 + 256], "bbti%d" % i, ["bbti%d" % i])
                S.ld(bti[i][:, 3, :], iT_s[:, t0:t0 + 256], "bbti%d" % i, ["bbti%d" % i])
                S.ld(x1b[i][:], x1_s[t0:t0 + 256, :].rearrange("(c p) d -> p c d", p=128), "bx1%d" % i, ["bx1%d" % i])

            nuv_ctr = [0]

            def uv_load(i):
                s_ = nuv_ctr[0] % NUV
                nuv_ctr[0] += 1
                S.ld(UVb[s_][:], UVh_s[i], "UVb%d" % s_, ["UVb%d" % s_])
                return s_

            b_load(0)
            ngrp = 0
            for bb in range(NB):
                bi = bb % 2
                hk, qk, btk, x1k = "bhT%d" % bi, "bq2%d" % bi, "bbti%d" % bi, "bx1%d" % bi
                if bb + 1 < NB:
                    b_load(bb + 1)
                pend = [uv_load(i_) for i_ in range(NUV)]
                def emit_q2rep(pr):
                    tq = pr * 16
                    qr = Q2rep[pr % 2]
                    qrk = "Q2rep%d" % (pr % 2)
                    S.act(qr[:].rearrange("k t (h a) -> k t h a", a=16),
                          q2[bi][:, :, tq:tq + 16].rearrange("k h t -> k t h").unsqueeze(3).to_broadcast([128, 16, 8, 16]),
                          AF.Copy, [qk], [qrk])

                def emit_s2rep(grp):
                    qr = Q2rep[(grp // 2) % 2]
                    qrk = "Q2rep%d" % ((grp // 2) % 2)
                    rb = 2 * (grp % 2)
                    for tt_ in range(8):
                        S.mm(bank[rb + tt_ // 4][:, (tt_ % 4) * 128:(tt_ % 4 + 1) * 128], qr[:, (grp % 2) * 8 + tt_, :], skT2[:],
                             True, True, [qrk, "skT2"], [BK[rb + tt_ // 4]])

                def emit_gate(grp):
                    tq = grp * 8
                    u = grp % 2
                    rb = 2 * u
                    wb = 4 + 2 * u
                    xk, ek, lk = "Xt%d" % u, "Et%d" % u, "Lp%d" % u
                    r3 = psall[:, rb:rb + 2, :].rearrange("p b (a n) -> p (b a) n", n=128)
                    rks = [BK[rb], BK[rb + 1]]

                    def bc(row):
                        return bti[bi][:, row, tq:tq + 8].unsqueeze(2).to_broadcast([128, 8, 128])
                    S.tt("dve", Xt[u][:], r3, bc(0), ALU.add, rks + [btk], [xk])
                    S.tt("dve", Et[u][:], Xt[u][:], bc(2), ALU.is_lt, [xk, btk], [ek])
                    S.op("dve", lambda e: e.scalar_tensor_tensor(out=Et[u][:], in0=Et[u][:], scalar=-1.0e4, in1=Xt[u][:],
                                                                 op0=ALU.mult, op1=ALU.add), [ek, xk], [ek])
                    for tt_ in range(8):
                        S.act(Et[u][:, tt_, :], Et[u][:, tt_, :], AF.Exp, [ek, btk], [ek],
                              bias=bti[bi][:, 1, tq + tt_:tq + tt_ + 1])
                    S.tt("dve", Lp[u][:], iota8[:], bc(3), ALU.is_equal, ["iota8", btk], [lk])
                    for tt_ in range(8):
                        S.mm(bank[wb + tt_ // 4][:, (tt_ % 4) * 128:(tt_ % 4 + 1) * 128], Et[u][:, tt_, :], Lp[u][:, tt_, :],
                             True, True, [ek, lk], [BK[wb + tt_ // 4]])

                def emit_gate_back(grp):
                    tq = grp * 8
                    wb = 4 + 2 * (grp % 2)
                    S.act(WT[:, tq:tq + 8, :], psall[:, wb:wb + 2, :].rearrange("p b (a n) -> p (b a) n", n=128), AF.Copy,
                          [BK[wb], BK[wb + 1]], ["WT"])

                emit_q2rep(0)
                emit_s2rep(0)
                emit_q2rep(1)
                for grp in range(32):
                    if grp + 1 < 32:
                        emit_s2rep(grp + 1)
                    if grp % 2 == 1 and grp // 2 + 2 < 16:
                        emit_q2rep(grp // 2 + 2)
                    emit_gate(grp)
                    if grp >= 1:
                        emit_gate_back(grp - 1)
                emit_gate_back(31)
                slot = {}

                def emit_A(i):
                    slot[i] = pend.pop(0)
                    s_ = slot[i]
                    ab = i % 2
                    for kc in range(8):
                        S.mm(bank[ab][:, 0:256], UVb[s_][:, kc * 128:(kc + 1) * 128], hT[bi][:, kc, :], kc == 0, kc == 7,
                             ["UVb%d" % s_, hk], [BK[ab]])

                def emit_V(i):
                    s_ = slot[i]
                    ab = i % 2
                    S.act(Gt[ab][:], bank[ab][:, 0:256], AF.Gelu, [BK[ab]], ["Gt%d" % ab])
                    S.tt("dve", At[ab][:], Gt[ab][:], WT[:, :, i], ALU.mult, ["Gt%d" % ab, "WT"], ["At%d" % ab])
                    for c in range(2):
                        for n in range(2):
                            yb = 4 + c * 2 + n
                            S.mm(bank[yb][:, :], At[ab][:, c * 128:(c + 1) * 128], UVb[s_][:, D + n * 512:D + (n + 1) * 512],
                                 i == 0, i == 127, ["At%d" % ab, "UVb%d" % s_], [BK[yb]])
                    if i + NUV < 128:
                        pend.append(uv_load(i + NUV))

                emit_A(0)
                for i in range(128):
                    if i + 1 < 128:
                        emit_A(i + 1)
                    emit_V(i)
                for c in range(2):
                    for n in range(2):
                        yb = 4 + c * 2 + n
                        S.tt("dve", x2[:, c, n * 512:(n + 1) * 512], bank[yb][:, :], g2bc[:, n * 512:(n + 1) * 512], ALU.mult,
                             [BK[yb], "g2bc"], ["x2"])
                    S.tt("pool", x2[:, c, :], x2[:, c, :], x1b[bi][:, c, :], ALU.add, ["x2", x1k], ["x2"])
                    S.act(junk[:], x2[:, c, :], AF.Square, ["x2"], ["bjunk"], accum_out=ss[:, c:c + 1])
                S.ts("dve", rstd[:], ss[:], 1.0 / D, 1e-6, ALU.mult, ALU.add, ["bss", "bjunk"], ["brstd"])
                S.act(rstd[:], rstd[:], AF.Sqrt, ["brstd"], ["brstd"])
                S.op("dve", lambda e: e.reciprocal(out=rstd[:], in_=rstd[:]), ["brstd"], ["brstd"])
                for c in range(2):
                    S.act(x2[:, c, :], x2[:, c, :], AF.Identity, ["x2", "brstd"], ["x2"], scale=rstd[:, c:c + 1])
                    S.tt("pool", x2[:, c, :], x2[:, c, :], fgbc[:], ALU.mult, ["x2", "fgbc"], ["x2"])
                S.st(y_d[bb * 256:(bb + 1) * 256, :].rearrange("(c p) d -> p c d", p=128), x2[:], "yout", ["x2"])
            S.barrier()
            S.emit()
        print("bass instructions:", S.ninst)
    return nc


def make_in_maps(inputs, S_TOK=8192, n_cores=8):
    f = lambda a: np.ascontiguousarray(a, dtype=np.float32)
    cst = _consts()
    w_ada = f(inputs["w_ada"][0])
    b_ada = inputs["b_ada"][0]
    shared = dict(
        w_ada=w_ada,
        b_adaT=f(b_ada.reshape(48, 128).T),
        b_ada_bc=f(np.broadcast_to(np.concatenate([b_ada[2048:3072], b_ada[5120:6144]])[None, :], (128, 2048))),
        n1g=f(inputs["norm1_g"][0].reshape(8, 128).T),
        n2g=f(inputs["norm2_g"][0].reshape(8, 128).T),
        fg_bc=f(np.broadcast_to(inputs["final_g"][None, :], (128, D))),
        w_in=f(inputs["w_in"][0]),
        pool_w=f(inputs["pool_w"][0]),
        pool_scT=f(inputs["pool_scale"][0].reshape(4, 128).T),
        w_out=f(inputs["w_out"][0]),
        wq=f(inputs["peer_wq"][0]),
        skT=f(np.transpose(inputs["peer_subkeys"][0], (0, 2, 1))),
        UV=f(np.concatenate([np.transpose(inputs["peer_u"][0].reshape(128, 128, 8, 128), (0, 3, 2, 1)).reshape(128, 128, 1024),
                             inputs["peer_v"][0].reshape(128, 128, 1024)], axis=2)),
        **cst,
    )
    maps = []
    for b in range(n_cores):
        m = dict(shared)
        m["x"] = f(inputs["x"][b, :S_TOK])
        m["cT"] = f(inputs["c"][b].reshape(8, 128).T)
        maps.append(m)
    return maps


def kernel(**inputs):
    nc = build(8192)
    in_maps = make_in_maps(inputs, 8192, 8)
    res = run_bass_kernel_spmd(nc, in_maps, core_ids=list(range(8)))
    return np.stack([np.asarray(r["y"], dtype=np.float32) for r in res.results], axis=0)
```

```python
import numpy as np
from contextlib import ExitStack
import concourse.bass as bass
import concourse.mybir as mybir
from concourse.bass_utils import run_bass_kernel_spmd

F32 = mybir.dt.float32
F16 = mybir.dt.float16
F32R = mybir.dt.float32r
BF16 = mybir.dt.bfloat16
U32 = mybir.dt.uint32
AF = mybir.ActivationFunctionType
ALU = mybir.AluOpType
AX = mybir.AxisListType

D = 1024
NH = 8
HD = 64
NEG = -1.0e30


class Sched:
    ENG = ("pe", "act", "dve", "pool", "sp")
    ENGN = {"pe": "tensor", "act": "scalar", "dve": "vector", "pool": "gpsimd", "sp": "sync"}

    def __init__(self, nc, stack):
        self.nc = nc
        self.stack = stack
        self.prog = {e: [] for e in self.ENG}
        self.cnt = {e: 0 for e in self.ENG}
        self.sem = {e: stack.enter_context(nc.semaphore("sem_" + e)) for e in self.ENG}
        self.waited = {e: {} for e in self.ENG}
        self.res = {}
        self.dsem = {}
        self.ninst = 0

    def dma_sem(self, name):
        if name not in self.dsem:
            self.dsem[name] = [self.stack.enter_context(self.nc.semaphore("d_" + name)), 0]
        return self.dsem[name]

    def _need(self, eng, tok, waits):
        if tok is None:
            return
        kind, a, b = tok
        if kind == "e" and a == eng and eng == "pe":
            return
        key = (kind, a)
        if self.waited[eng].get(key, 0) >= b:
            return
        if waits.get(key, 0) < b:
            waits[key] = b

    def _deps(self, eng, reads, writes):
        waits = {}
        for k in reads:
            r = self.res.setdefault(k, {"w": None, "r": {}})
            self._need(eng, r["w"], waits)
        for k in writes:
            r = self.res.setdefault(k, {"w": None, "r": {}})
            self._need(eng, r["w"], waits)
            for (kind, a), b in r["r"].items():
                self._need(eng, (kind, a, b), waits)
        for key, v in waits.items():
            self.waited[eng][key] = v
            self.prog[eng].append(("wait", key, v))

    def _record(self, tok, reads, writes):
        for k in reads:
            rr = self.res[k]["r"]
            key = (tok[0], tok[1])
            if rr.get(key, 0) < tok[2]:
                rr[key] = tok[2]
        for k in writes:
            self.res[k] = {"w": tok, "r": {}}

    def op(self, eng, fn, reads=(), writes=()):
        self._deps(eng, reads, writes)
        self.cnt[eng] += 1
        tok = ("e", eng, self.cnt[eng])
        self.prog[eng].append(("op", fn))
        self._record(tok, reads, writes)
        self.ninst += 1

    def dma(self, q, fn, semname, reads=(), writes=()):
        self._deps(q, reads, writes)
        s = self.dma_sem(semname)
        s[1] += 16
        tok = ("d", semname, s[1])
        self.prog[q].append(("dma", fn, semname))
        self._record(tok, reads, writes)
        self.ninst += 1

    def barrier(self):
        for e in self.ENG:
            for o in self.ENG:
                if o != e and self.cnt[o] > self.waited[e].get(("e", o), 0):
                    self.waited[e][("e", o)] = self.cnt[o]
                    self.prog[e].append(("wait", ("e", o), self.cnt[o]))
            for name, s in self.dsem.items():
                if s[1] > self.waited[e].get(("d", name), 0):
                    self.waited[e][("d", name)] = s[1]
                    self.prog[e].append(("wait", ("d", name), s[1]))

    def emit(self):
        nc = self.nc
        with nc.Block() as block:
            for e in self.ENG:
                prog = self.prog[e]

                def body(engine, prog=prog, e=e):
                    for it in prog:
                        if it[0] == "wait":
                            key, v = it[1], it[2]
                            h = self.sem[key[1]] if key[0] == "e" else self.dsem[key[1]][0]
                            engine.wait_ge(h, v)
                        elif it[0] == "op":
                            it[1](engine).then_inc(self.sem[e], 1)
                        else:
                            it[1](engine).then_inc(self.dsem[it[2]][0], 16)
                getattr(block, self.ENGN[e])(body)
        self.prog = {e: [] for e in self.ENG}

    def mm(self, out, lhsT, rhs, start, stop, reads, writes):
        self.op("pe", lambda e: e.matmul(out, lhsT=lhsT, rhs=rhs, start=start, stop=stop), reads, writes)

    def tr(self, out, in_, ident, reads, writes):
        self.op("pe", lambda e: e.transpose(out, in_, ident), reads, writes)

    def act(self, out, in_, func, reads, writes, scale=None, bias=None, accum_out=None):
        kw = {}
        if scale is not None:
            kw["scale"] = scale
        if bias is not None:
            kw["bias"] = bias
        if accum_out is not None:
            kw["accum_out"] = accum_out
        self.op("act", lambda e: e.activation(out=out, in_=in_, func=func, **kw), reads, writes)

    def tt(self, eng, out, in0, in1, op, reads, writes):
        self.op(eng, lambda e: e.tensor_tensor(out=out, in0=in0, in1=in1, op=op), reads, writes)

    def ts(self, eng, out, in0, s1, s2, op0, op1, reads, writes):
        if op1 is None:
            self.op(eng, lambda e: e.tensor_scalar(out=out, in0=in0, scalar1=s1, scalar2=None, op0=op0), reads, writes)
        else:
            self.op(eng, lambda e: e.tensor_scalar(out=out, in0=in0, scalar1=s1, scalar2=s2, op0=op0, op1=op1), reads, writes)

    def copy(self, eng, out, in_, reads, writes):
        self.op(eng, lambda e: e.tensor_copy(out=out, in_=in_), reads, writes)

    def memset(self, eng, ap, val, writes):
        self.op(eng, lambda e: e.memset(ap, val), (), writes)

    def ld(self, out, in_, sem, writes, q="sp"):
        self.dma(q, lambda e: e.dma_start(out=out, in_=in_), sem, (), writes)

    def st(self, out, in_, sem, reads, q="pool"):
        self.dma(q, lambda e: e.dma_start(out=out, in_=in_), sem, reads, ())


def _consts():
    ident = np.eye(128, dtype=np.float32)
    k = np.arange(128)[:, None, None]
    rel = np.arange(17)[None, :, None]
    q = np.arange(128)[None, None, :]
    dist = rel * 128 + q - k
    cnt = ((dist >= 0) & (dist <= 128)).astype(np.float32)
    cnt += ((dist >= 0) & (dist <= 512) & (dist % 4 == 0)).astype(np.float32)
    cnt += ((dist >= 0) & (dist <= 2048) & (dist % 16 == 0)).astype(np.float32)
    CT = np.ascontiguousarray(cnt.reshape(128, 17 * 128), dtype=np.float32)
    slopes = np.array([2.0 ** (-8.0 * (h + 1) / NH) for h in range(NH)], dtype=np.float64)
    kl = np.arange(128, dtype=np.float64)[:, None, None]
    bk = slopes[None, :, None] * (kl - 64.0) - slopes[None, :, None] * 128.0 * np.arange(17)[None, None, :]
    biasK = np.ascontiguousarray(bk.reshape(128, NH * 17), dtype=np.float32)
    bm = slopes[None, :, None] * (kl - 64.0) - slopes[None, :, None] * 128.0 * (np.arange(-3, 17)[None, None, :] + 1.5)
    biasM = np.ascontiguousarray(bm.reshape(128, NH * 20), dtype=np.float32)
    band = np.zeros((128, 4, 3, 128), dtype=np.float64)
    s = np.arange(128)[:, None]
    t = np.arange(128)[None, :]
    for pg, w in enumerate((2, 4, 8, 16)):
        cur = ((s <= t) & (s > t - w)).astype(np.float64) / w - (s == t)
        prev = ((t + 128 - s) < w).astype(np.float64) / w
        cntf = np.minimum(t + 1, w).astype(np.float64)
        first = ((s <= t) & (s > t - w)).astype(np.float64) / cntf - (s == t)
        band[:, pg, 0, :] = cur
        band[:, pg, 1, :] = prev
        band[:, pg, 2, :] = first
    band = np.ascontiguousarray(band.reshape(128, 12 * 128), dtype=np.float32)
    iota4 = np.ascontiguousarray(np.tile(np.arange(128, dtype=np.float32), (128, 8)))
    return dict(ident=ident, CT=CT, biasK=biasK, biasM=biasM, band=band, iota4=iota4)


def build(S_TOK=8192, debug=False):
    NCHK = S_TOK // 128
    NG = S_TOK // 512
    NB = S_TOK // 256
    nc = bass.Bass("TRN2", target_bir_lowering=False)
    scr_kind = "ExternalOutput" if debug else "Internal"

    def din(name, shape, dt=F32):
        return nc.dram_tensor(name, list(shape), dt, kind="ExternalInput").ap()

    def dscr(name, shape, dt=F32):
        return nc.dram_tensor(name, list(shape), dt, kind=scr_kind).ap()

    x_d = din("x", [S_TOK, D])
    cT_d = din("cT", [128, 8])
    wada_d = din("w_ada", [D, 6 * D])
    badaT_d = din("b_adaT", [128, 48])
    badabc_d = din("b_ada_bc", [128, 2 * D])
    n1g_d = din("n1g", [128, 8])
    n2g_d = din("n2g", [128, 8])
    fgbc_d = din("fg_bc", [128, D])
    win_d = din("w_in", [D, 2048], F32R)
    poolw_d = din("pool_w", [4, 128, 128], F32R)
    poolsc_d = din("pool_scT", [128, 4])
    wout_d = din("w_out", [D, D], F32R)
    wq_d = din("wq", [D, 2048])
    skT_d = din("skT", [2, 128, 128])
    UV_d = din("UV", [128, 128, 2048])
    ident_d = din("ident", [128, 128])
    CT_d = din("CT", [128, 17 * 128])
    biasK_d = din("biasK", [128, NH * 17])
    biasM_d = din("biasM", [128, NH * 20])
    band_d = din("band", [128, 12 * 128], F32R)
    iota4_d = din("iota4", [128, 1024])

    qT_s = dscr("qT_scr", [4, 128, S_TOK], F32R)
    kT_s = dscr("kT_scr", [4, 128, S_TOK], F32R)
    v_s = dscr("v_scr", [S_TOK, NH * 66], BF16)
    u_s = dscr("u_scr", [S_TOK, 512], F32R)
    attn_s = dscr("attn_scr", [S_TOK, 512])
    x1_s = dscr("x1_scr", [S_TOK, D])
    h2T_s = dscr("h2T_scr", [8, 128, S_TOK], BF16)
    UVh_s = dscr("UVh_scr", [128, 128, 2048], BF16)
    q2T_s = dscr("q2T_scr", [8, 128, S_TOK])
    bT_s = dscr("bT_scr", [128, S_TOK])
    nT_s = dscr("nT_scr", [128, S_TOK])
    tT_s = dscr("tT_scr", [128, S_TOK])
    iT_s = dscr("iT_scr", [128, S_TOK])
    y_d = nc.dram_tensor("y", [S_TOK, D], F32, kind="ExternalOutput").ap()

    with ExitStack() as top:
        S = Sched(nc, top)

        def sb(stack, name, shape, dt=F32):
            return stack.enter_context(nc.sbuf_tensor("s_" + name, list(shape), dt))

        psall = top.enter_context(nc.psum_tensor("psall", [128, 8, 512], F32))
        bank = [psall[:, i, :] for i in range(8)]
        BK = ["bank%d" % i for i in range(8)]

        ident = sb(top, "ident", [128, 128])
        scale1 = sb(top, "scale1", [128, 8])
        shift1 = sb(top, "shift1", [128, 8])
        scale2 = sb(top, "scale2", [128, 8])
        shift2 = sb(top, "shift2", [128, 8])
        g1bc = sb(top, "g1bc", [128, D])
        g2bc = sb(top, "g2bc", [128, D])
        S.ld(ident[:], ident_d, "c_ident", ["ident"])

        with ExitStack() as ph:
            cT = sb(ph, "cT", [128, 8])
            cs = sb(ph, "cs", [128, 8])
            csrep = sb(ph, "csrep", [128, 8, 128])
            badaT = sb(ph, "badaT", [128, 48])
            badabc = sb(ph, "badabc", [128, 2 * D])
            n1g = sb(ph, "n1g", [128, 8])
            n2g = sb(ph, "n2g", [128, 8])
            modc = sb(ph, "modc", [128, 48])
            wa = [sb(ph, "wa%d" % i, [128, 8, 512]) for i in range(2)]
            S.ld(cT[:], cT_d, "p1a", ["cT"])
            S.ld(badaT[:], badaT_d, "p1b", ["badaT"])
            S.ld(badabc[:], badabc_d, "p1c", ["badabc"])
            S.ld(n1g[:], n1g_d, "p1d", ["n1g"])
            S.ld(n2g[:], n2g_d, "p1e", ["n2g"])
            S.act(cs[:], cT[:], AF.Silu, ["cT"], ["cs"])
            S.copy("dve", csrep[:], cs[:].unsqueeze(2).to_broadcast([128, 8, 128]), ["cs"], ["csrep"])
            S.memset("dve", modc[:], 0.0, ["modc"])
            for n in range(12):
                w = wa[n % 2]
                wk = "wa%d" % (n % 2)
                S.ld(w[:], wada_d[:, n * 512:(n + 1) * 512].rearrange("(kc p) n -> p kc n", p=128), wk, [wk])
                if n in (4, 5, 10, 11):
                    pb = bank[n % 2]
                    for kc in range(8):
                        S.mm(pb[:, :], csrep[:, kc, :], w[:, kc, :], kc == 0, kc == 7, ["csrep", wk], [BK[n % 2]])
                    dst = g1bc if n < 6 else g2bc
                    dk = "g1bc" if n < 6 else "g2bc"
                    off = (n % 2) * 512
                    boff = off if n < 6 else D + off
                    S.tt("dve", dst[:, off:off + 512], pb[:, :], badabc[:, boff:boff + 512], ALU.add,
                         [BK[n % 2], "badabc"], [dk])
                else:
                    pb = bank[2 + n % 2]
                    for jj in range(4):
                        for kc in range(8):
                            S.mm(pb[:, jj:jj + 1], w[:, kc, jj * 128:(jj + 1) * 128], cs[:, kc:kc + 1],
                                 kc == 0, kc == 7, ["cs", wk], [BK[2 + n % 2]])
                    S.tt("dve", modc[:, 4 * n:4 * n + 4], pb[:, 0:4], badaT[:, 4 * n:4 * n + 4], ALU.add,
                         [BK[2 + n % 2], "badaT"], ["modc"])
            S.op("dve", lambda e: e.scalar_tensor_tensor(out=scale1[:], in0=modc[:, 8:16], scalar=1.0, in1=n1g[:],
                                                         op0=ALU.add, op1=ALU.mult), ["modc", "n1g"], ["scale1"])
            S.op("dve", lambda e: e.scalar_tensor_tensor(out=scale2[:], in0=modc[:, 32:40], scalar=1.0, in1=n2g[:],
                                                         op0=ALU.add, op1=ALU.mult), ["modc", "n2g"], ["scale2"])
            S.copy("dve", shift1[:], modc[:, 0:8], ["modc"], ["shift1"])
            S.copy("dve", shift2[:], modc[:, 24:32], ["modc"], ["shift2"])
            S.barrier()
            S.emit()

        def norm_transpose(xt, xk, xn, xnk, hT, hTk, ss, rstd, nch, scol, scolk, bcol, bcolk, pbanks, hT2=None, hT2k=None):
            for c in range(nch):
                S.act(xn[:, c, :], xt[:, c, :], AF.Square, [xk], [xnk], accum_out=ss[:, c:c + 1])
            S.ts("dve", rstd[:, 0:nch], ss[:, 0:nch], 1.0 / D, 1e-6, ALU.mult, ALU.add, ["ss", xnk], ["rstd"])
            S.act(rstd[:, 0:nch], rstd[:, 0:nch], AF.Sqrt, ["rstd"], ["rstd"])
            S.op("dve", lambda e: e.reciprocal(out=rstd[:, 0:nch], in_=rstd[:, 0:nch]), ["rstd"], ["rstd"])
            for c in range(nch):
                S.act(xn[:, c, :], xt[:, c, :], AF.Identity, [xk, "rstd"], [xnk], scale=rstd[:, c:c + 1])
            for kc in range(8):
                pb = pbanks[kc % 2]
                for c in range(nch):
                    S.tr(bank[pb][:, c * 128:(c + 1) * 128], xn[:, c, kc * 128:(kc + 1) * 128], ident[:],
                         [xnk, "ident"], [BK[pb]])
                S.act(hT[:, kc, 0:nch * 128], bank[pb][:, 0:nch * 128], AF.Identity, [BK[pb], scolk, bcolk], [hTk],
                      scale=scol[:, kc:kc + 1], bias=bcol[:, kc:kc + 1])
                if hT2 is not None:
                    S.act(hT2[:, kc, 0:nch * 128], bank[pb][:, 0:nch * 128], AF.Identity, [BK[pb], scolk, bcolk], [hT2k],
                          scale=scol[:, kc:kc + 1], bias=bcol[:, kc:kc + 1])

        with ExitStack() as ph:
            S.barrier()
            win = sb(ph, "win", [128, 8, 2048], F32R)
            xt = [sb(ph, "p2x%d" % i, [128, 4, D]) for i in range(2)]
            xn = sb(ph, "p2xn", [128, 4, D])
            hT = sb(ph, "p2hT", [128, 8, 512], F32R)
            ss = sb(ph, "p2ss", [128, 4])
            rstd = sb(ph, "p2rstd", [128, 4])
            qst = sb(ph, "p2q", [128, 4, 512], F32R)
            kst = sb(ph, "p2k", [128, 4, 512], F32R)
            vst = sb(ph, "p2v", [128, 4, NH, 66], BF16)
            ones_t = sb(ph, "p2ones", [128, 4 * NH * 66])
            ust = sb(ph, "p2u", [128, 4, 512], F32R)
            for i in range(4):
                S.ld(win[:, :, i * 512:(i + 1) * 512],
                     win_d[:, i * 512:(i + 1) * 512].rearrange("(kc p) n -> p kc n", p=128), "win", ["win"], q="pool")
            S.memset("dve", ones_t[:], 1.0, ["ones_t"])
            S.copy("dve", vst[:].rearrange("p c h f -> p (c h f)"), ones_t[:], ["ones_t"], ["vst"])
            xv = x_d.rearrange("(g c p) d -> g p c d", p=128, c=4)
            S.ld(xt[0][:], xv[0], "p2x0", ["p2x0"])
            for g in range(NG):
                xk = "p2x%d" % (g % 2)
                if g + 1 < NG:
                    S.ld(xt[(g + 1) % 2][:], xv[g + 1], "p2x%d" % ((g + 1) % 2), ["p2x%d" % ((g + 1) % 2)])
                norm_transpose(xt[g % 2], xk, xn, "p2xn", hT, "p2hT", ss, rstd, 4,
                               scale1, "scale1", shift1, "shift1", (0, 1))
                t0 = g * 512
                for hp in range(4):
                    pb = 2 + hp % 2
                    for kc in range(8):
                        S.mm(bank[pb][:, :], win[:, kc, hp * 128:(hp + 1) * 128], hT[:, kc, :], kc == 0, kc == 7,
                             ["win", "p2hT"], [BK[pb]])
                    S.act(qst[:, hp, :], bank[pb][:, :], AF.Copy, [BK[pb]], ["qst"], scale=0.125)
                for hp in range(4):
                    pb = 2 + hp % 2
                    for kc in range(8):
                        S.mm(bank[pb][:, :], win[:, kc, 512 + hp * 128:512 + (hp + 1) * 128], hT[:, kc, :],
                             kc == 0, kc == 7, ["win", "p2hT"], [BK[pb]])
                    S.copy("dve", kst[:, hp, :], bank[pb][:, :], [BK[pb]], ["kst"])
                for c in range(4):
                    pb = 4 + c % 2
                    for kc in range(8):
                        S.mm(bank[pb][:, :], hT[:, kc, c * 128:(c + 1) * 128], win[:, kc, 1024:1536], kc == 0, kc == 7,
                             ["win", "p2hT"], [BK[pb]])
                    S.act(vst[:, c, :, 0:64], bank[pb][:, :].rearrange("p (h f) -> p h f", f=64), AF.Copy,
                          [BK[pb]], ["vst"])
                for c in range(4):
                    pb = 6 + c % 2
                    for kc in range(8):
                        S.mm(bank[pb][:, :], hT[:, kc, c * 128:(c + 1) * 128], win[:, kc, 1536:2048], kc == 0, kc == 7,
                             ["win", "p2hT"], [BK[pb]])
                    S.copy("dve", ust[:, c, :], bank[pb][:, :], [BK[pb]], ["ust"])
                S.st(qT_s[:, :, t0:t0 + 512].rearrange("h p t -> p h t"), qst[:], "p2sq", ["qst"])
                S.st(kT_s[:, :, t0:t0 + 512].rearrange("h p t -> p h t"), kst[:], "p2sk", ["kst"])
                S.st(v_s[t0:t0 + 512, :].rearrange("(c p) f -> p c f", p=128), vst[:].rearrange("p c h f -> p c (h f)"),
                     "p2sv", ["vst"])
                S.st(u_s[t0:t0 + 512, :].rearrange("(c p) f -> p c f", p=128), ust[:], "p2su", ["ust"])
            S.barrier()
            S.emit()

        with ExitStack() as ph:
            S.barrier()
            CT = sb(ph, "CT", [128, 17 * 128])
            biasK = sb(ph, "biasK", [128, NH * 17])
            biasM = sb(ph, "biasM", [128, NH * 20])
            KT = sb(ph, "KT", [128, S_TOK], F32R)
            Vp = sb(ph, "Vp", [128, NCHK, 2, 66], BF16)
            QTg = [sb(ph, "QTg%d" % i, [128, 512], F32R) for i in range(2)]
            PT = [sb(ph, "PT%d" % i, [128, 512]) for i in range(2)]
            PTm = [[sb(ph, "PTm%d_%d" % (p_, i), [128, 512], BF16) for i in range(20)] for p_ in range(2)]
            ao = [sb(ph, "ao%d" % i, [128, 4, 128]) for i in range(2)]
            rden = sb(ph, "rden", [128, 8])
            S.ld(CT[:], CT_d, "p3c", ["CT"])
            S.ld(biasK[:], biasK_d, "p3b", ["biasK"])
            S.ld(biasM[:], biasM_d, "p3b2", ["biasM"])
            cnt3 = dict(nrd=0, nsb=0, nob=0, npt=0)
            units = [(hp, g, hh) for hp in range(4) for g in range(NG) for hh in range(2)]

            def p3_S(n):
                hp, g, hh = units[n]
                par = n % 2
                it = hp * NG + g
                qk = "QTg%d" % (it % 2)
                qt = QTg[it % 2]
                if g == 0 and hh == 0:
                    npc = max(1, S_TOK // 2048)
                    for i in range(npc):
                        w = S_TOK // npc
                        S.ld(KT[:, i * w:(i + 1) * w], kT_s[hp, :, i * w:(i + 1) * w], "p3k", ["KT"], q="pool")
                if hh == 0:
                    S.ld(qt[:], qT_s[hp, :, g * 512:(g + 1) * 512], qk, [qk], q="pool")
                h = 2 * hp + hh
                r0 = 64 * hh
                kbs = list(range(max(0, 4 * g - 16), 4 * g + 4))
                steps = []
                for kb in kbs:
                    steps.append(lambda kb=kb: s_step(kb, hp, g, hh, h, r0, par, qk, qt))
                return steps

            def s_step(kb, hp, g, hh, h, r0, par, qk, qt):
                if True:
                    kbi = kb - (4 * g - 16)
                    sbk = cnt3["nsb"] % 2
                    cnt3["nsb"] += 1
                    S.mm(bank[sbk][:, :], KT[r0:r0 + 64, kb * 128:(kb + 1) * 128], qt[r0:r0 + 64, :], True, True,
                         ["KT", qk], [BK[sbk]])
                    ptk = "PT%d" % (cnt3["npt"] % 2)
                    ptt = PT[cnt3["npt"] % 2]
                    cnt3["npt"] += 1
                    js = [j for j in range(4) if 0 <= 4 * g + j - kb <= 16]
                    j0, j1 = js[0], js[-1]
                    if h >= 2:
                        r_ = 4 * g - kb
                        S.act(ptt[:, j0 * 128:(j1 + 1) * 128], bank[sbk][:, j0 * 128:(j1 + 1) * 128], AF.Exp,
                              [BK[sbk], "biasM"], [ptk], bias=biasM[:, h * 20 + r_ + 3:h * 20 + r_ + 4])
                    else:
                        for j in js:
                            rel = 4 * g + j - kb
                            S.act(ptt[:, j * 128:(j + 1) * 128], bank[sbk][:, j * 128:(j + 1) * 128], AF.Exp,
                                  [BK[sbk], "biasK"], [ptk], bias=biasK[:, h * 17 + rel:h * 17 + rel + 1])
                    rel0 = 4 * g + j0 - kb
                    S.tt("dve", PTm[par][kbi][:, j0 * 128:(j1 + 1) * 128], ptt[:, j0 * 128:(j1 + 1) * 128],
                         CT[:, rel0 * 128:(rel0 + j1 - j0 + 1) * 128], ALU.mult, [ptk, "CT"], ["PTm%d_%d" % (par, kbi)])

            def p3_V(n):
                hp, g, hh = units[n]
                par = n % 2
                it = hp * NG + g
                aok = "ao%d" % (it % 2)
                aot = ao[it % 2]
                if g == 0 and hh == 0:
                    nvp = max(1, NCHK // 8)
                    for i in range(nvp):
                        nb_ = NCHK // nvp
                        S.ld(Vp[:, i * nb_:(i + 1) * nb_, :, :].rearrange("p b h f -> p b (h f)"),
                             v_s[i * nb_ * 128:(i + 1) * nb_ * 128, hp * 132:(hp + 1) * 132].rearrange("(b p) f -> p b f", p=128),
                             "p3v", ["Vp"], q="sp")
                kbs = list(range(max(0, 4 * g - 16), 4 * g + 4))
                steps = []
                for j in range(4):
                    steps.append(lambda j=j: v_step(j, hp, g, hh, par, aok, aot, kbs))
                return steps

            def v_step(j, hp, g, hh, par, aok, aot, kbs):
                if True:
                    obk = 2 + cnt3["nob"] % 2
                    cnt3["nob"] += 1
                    kl = [kb for kb in kbs if 0 <= 4 * g + j - kb <= 16]
                    for n_, kb in enumerate(kl):
                        kbi = kb - (4 * g - 16)
                        S.mm(bank[obk][:, 0:66], PTm[par][kbi][:, j * 128:(j + 1) * 128], Vp[:, kb, hh, :],
                             n_ == 0, n_ == len(kl) - 1, ["PTm%d_%d" % (par, kbi), "Vp"], [BK[obk]])
                    rc = cnt3["nrd"] % 8
                    cnt3["nrd"] += 1
                    S.op("dve", lambda e, obk=obk, rc=rc: e.reciprocal(out=rden[:, rc:rc + 1], in_=bank[obk][:, 64:65]),
                         [BK[obk]], ["rden%d" % rc])
                    S.act(aot[:, j, hh * 64:(hh + 1) * 64], bank[obk][:, 0:64], AF.Identity,
                          [BK[obk], "rden%d" % rc], [aok], scale=rden[:, rc:rc + 1])
                if hh == 1 and j == 3:
                    S.st(attn_s[g * 512:(g + 1) * 512, hp * 128:(hp + 1) * 128].rearrange("(c p) f -> p c f", p=128),
                         aot[:], aok, [aok], q="act")

            for st_ in p3_S(0):
                st_()
            for n in range(len(units)):
                vs = p3_V(n)
                ss_ = p3_S(n + 1) if n + 1 < len(units) else []
                ns = len(ss_)
                vi = 0
                for i_, st_ in enumerate(ss_):
                    st_()
                    while vi < 4 and (i_ + 1) * 4 >= (vi + 1) * ns:
                        vs[vi]()
                        vi += 1
                while vi < 4:
                    vs[vi]()
                    vi += 1
            S.barrier()
            S.emit()

        with ExitStack() as ph:
            S.barrier()
            wout = sb(ph, "wout", [128, 8, D], F32R)
            band = sb(ph, "band", [128, 12 * 128], F32R)
            poolw = sb(ph, "poolw", [128, 4, 128], F32R)
            poolsc = sb(ph, "poolsc", [128, 4])
            xt = [sb(ph, "p4x%d" % i, [128, 4, D]) for i in range(2)]
            at = [sb(ph, "p4a%d" % i, [128, 4, 512]) for i in range(2)]
            ub = [sb(ph, "p4u%d" % i, [128, 5, 512], F32R) for i in range(2)]
            concatT = sb(ph, "concatT", [128, 8, 512], F32R)
            mixT = sb(ph, "mixT", [128, 4, 512], F32R)
            x1t = sb(ph, "x1t", [128, 4, D])
            for i in range(2):
                S.ld(wout[:, :, i * 512:(i + 1) * 512],
                     wout_d[:, i * 512:(i + 1) * 512].rearrange("(kc p) n -> p kc n", p=128), "wout", ["wout"], q="pool")
            S.ld(band[:], band_d, "p4c1", ["band"], q="pool")
            S.ld(poolw[:], poolw_d.rearrange("g c e -> c g e"), "p4c2", ["poolw"], q="pool")
            S.ld(poolsc[:], poolsc_d, "p4c3", ["poolsc"])
            xv = x_d.rearrange("(g c p) d -> g p c d", p=128, c=4)
            av = attn_s.rearrange("(g c p) d -> g p c d", p=128, c=4)
            uv = u_s.rearrange("(g c p) d -> g p c d", p=128, c=4)

            def p4_load(g):
                i = g % 2
                S.ld(xt[i][:], xv[g], "p4x%d" % i, ["p4x%d" % i])
                S.ld(at[i][:], av[g], "p4a%d" % i, ["p4a%d" % i])
                S.ld(ub[i][:, 1:5, :], uv[g], "p4u%d" % i, ["p4u%d" % i], q="pool")
                if g > 0:
                    S.ld(ub[i][:, 0, :], u_s[g * 512 - 128:g * 512, :], "p4u%d" % i, ["p4u%d" % i], q="pool")
            p4_load(0)
            for g in range(NG):
                i = g % 2
                xk, ak, uk = "p4x%d" % i, "p4a%d" % i, "p4u%d" % i
                if g + 1 < NG:
                    p4_load(g + 1)
                for kc in range(4):
                    pb = kc % 2
                    for c in range(4):
                        S.tr(bank[pb][:, c * 128:(c + 1) * 128], at[i][:, c, kc * 128:(kc + 1) * 128], ident[:],
                             [ak, "ident"], [BK[pb]])
                    S.act(concatT[:, kc, :], bank[pb][:, :], AF.Copy, [BK[pb]], ["concatT"])
                for pg in range(4):
                    pb = 2 + pg % 2
                    for c in range(4):
                        o = bank[pb][:, c * 128:(c + 1) * 128]
                        if 4 * g + c == 0:
                            S.mm(o, ub[i][:, 1 + c, pg * 128:(pg + 1) * 128], band[:, (pg * 3 + 2) * 128:(pg * 3 + 3) * 128],
                                 True, True, [uk, "band"], [BK[pb]])
                        else:
                            S.mm(o, ub[i][:, 1 + c, pg * 128:(pg + 1) * 128], band[:, (pg * 3 + 0) * 128:(pg * 3 + 1) * 128],
                                 True, False, [uk, "band"], [BK[pb]])
                            S.mm(o, ub[i][:, c, pg * 128:(pg + 1) * 128], band[:, (pg * 3 + 1) * 128:(pg * 3 + 2) * 128],
                                 False, True, [uk, "band"], [BK[pb]])
                    S.copy("dve", mixT[:, pg, :], bank[pb][:, :], [BK[pb]], ["mixT"])
                for pg in range(4):
                    pb = 4 + pg % 2
                    S.mm(bank[pb][:, :], poolw[:, pg, :], mixT[:, pg, :], True, True, ["poolw", "mixT"], [BK[pb]])
                    S.act(concatT[:, 4 + pg, :], bank[pb][:, :], AF.Identity, [BK[pb], "poolsc"], ["concatT"],
                          scale=poolsc[:, pg:pg + 1])
                for c in range(4):
                    for n in range(2):
                        pb = 6 + n
                        for kc in range(8):
                            S.mm(bank[pb][:, :], concatT[:, kc, c * 128:(c + 1) * 128], wout[:, kc, n * 512:(n + 1) * 512],
                                 kc == 0, kc == 7, ["concatT", "wout"], [BK[pb]])
                        S.tt("dve", x1t[:, c, n * 512:(n + 1) * 512], bank[pb][:, :], g1bc[:, n * 512:(n + 1) * 512], ALU.mult,
                             [BK[pb], "g1bc"], ["x1t"])
                    S.tt("pool", x1t[:, c, :], x1t[:, c, :], xt[i][:, c, :], ALU.add, ["x1t", xk], ["x1t"])
                S.st(x1_s[g * 512:(g + 1) * 512, :].rearrange("(c p) d -> p c d", p=128), x1t[:], "p4s", ["x1t"])
            S.barrier()
            S.emit()

        with ExitStack() as ph:
            S.barrier()
            wq = sb(ph, "wq", [128, 8, 2048])
            skT = sb(ph, "skT", [128, 2, 128])
            xt = [sb(ph, "p5x%d" % i, [128, 2, D]) for i in range(2)]
            xn = sb(ph, "p5xn", [128, 2, D])
            hT = sb(ph, "p5hT", [128, 8, 256])
            hTr = sb(ph, "p5hTr", [128, 8, 256], BF16)
            ss = sb(ph, "p5ss", [128, 4])
            rstd = sb(ph, "p5rstd", [128, 4])
            QT = sb(ph, "p5QT", [128, 16, 256])
            Ssb = [[sb(ph, "Ssb%d_%d" % (a_, c_), [128, 16, 128]) for c_ in range(2)] for a_ in range(2)]
            res4 = [sb(ph, "res4_%d" % c_, [128, 4, 128]) for c_ in range(2)]
            Sm = sb(ph, "Sm", [128, 16, 128])
            sv = sb(ph, "sv", [128, 16, 16])
            si = sb(ph, "si", [128, 8, 16], U32)
            sif = sb(ph, "sif", [128, 8, 16])
            cand = sb(ph, "cand", [128, 8, 256])
            candm = Sm[:].rearrange("p a n -> p (a n)").rearrange("p (h c) -> p h c", c=256)
            ee = sb(ph, "ee", [128, 8, 256])
            c8a = sb(ph, "c8a", [128, 8, 8])
            c8b = sb(ph, "c8b", [128, 8, 8])
            Zt = sb(ph, "Zt", [128, 8])
            mz = sb(ph, "mz", [128, 8])
            b1 = sb(ph, "b1", [128, 8, 16])
            nz = sb(ph, "nz", [128, 8, 16])
            th = sb(ph, "th", [128, 8, 16])
            stg = sb(ph, "stg", [128, 4, 256])
            for i in range(4):
                S.ld(wq[:, :, i * 512:(i + 1) * 512],
                     wq_d[:, i * 512:(i + 1) * 512].rearrange("(kc p) n -> p kc n", p=128), "wq", ["wq"])
            S.ld(skT[:], skT_d.rearrange("h k n -> k h n"), "p5c", ["skT"])
            xv = x1_s.rearrange("(g c p) d -> g p c d", p=128, c=2)
            S.ld(xt[0][:], xv[0], "p5x0", ["p5x0"])
            sv4 = sv[:].rearrange("p (h two) a -> p h two a", two=2)
            sv1 = sv4[:, :, 0, :]
            sv2 = sv4[:, :, 1, :]
            def p5_A1(g):
                xk = "p5x%d" % (g % 2)
                if g + 1 < NB:
                    S.ld(xt[(g + 1) % 2][:], xv[g + 1], "p5x%d" % ((g + 1) % 2), ["p5x%d" % ((g + 1) % 2)])
                norm_transpose(xt[g % 2], xk, xn, "p5xn", hT, "p5hT", ss, rstd, 2,
                               scale2, "scale2", shift2, "shift2", (0, 1), hTr, "p5hTr")
                t0 = g * 256
                S.st(h2T_s[:, :, t0:t0 + 256].rearrange("k p t -> p k t"), hTr[:], "p5sh", ["p5hTr"])

            def p5_A2(g, half):
                for hpq in range(8 * half, 8 * half + 8):
                    pb = 2 + hpq % 2
                    for kc in range(8):
                        S.mm(bank[pb][:, 0:256], wq[:, kc, hpq * 128:(hpq + 1) * 128], hT[:, kc, :], kc == 0, kc == 7,
                             ["wq", "p5hT"], [BK[pb]])
                    S.act(QT[:, hpq, :], bank[pb][:, 0:256], AF.Copy, [BK[pb]], ["p5QT"])
                    if hpq % 2 == 1:
                        conv_some(1)

            def p5_A3(g):
                t0 = g * 256
                S.st(q2T_s[:, :, t0:t0 + 256].rearrange("h p t -> p h t"),
                     QT[:].rearrange("p (h two) t -> p h two t", two=2)[:, :, 1, :], "p5sq", ["p5QT"])
                for c in range(2):
                    sk_ = "Ssb%d_%d" % (g % 2, c)
                    for b4 in range(4):
                        pb = 4 + b4
                        for k4 in range(4):
                            hpq = b4 * 4 + k4
                            S.mm(bank[pb][:, k4 * 128:(k4 + 1) * 128], QT[:, hpq, c * 128:(c + 1) * 128], skT[:, hpq % 2, :],
                                 True, True, ["p5QT", "skT"], [BK[pb]])
                        S.act(Ssb[g % 2][c][:, b4 * 4:(b4 + 1) * 4, :], bank[pb][:, :].rearrange("p (a n) -> p a n", n=128), AF.Copy,
                              [BK[pb]], [sk_])

            def p5_B1(g, c):
                sk_ = "Ssb%d_%d" % (g % 2, c)
                Sc = Ssb[g % 2][c]
                for hpq in range(16):
                    S.op("dve", lambda e, hpq=hpq: e.max(out=sv[:, hpq, 0:8], in_=Sc[:, hpq, :]), [sk_], ["sv"])
                    S.op("dve", lambda e, hpq=hpq: e.match_replace(out=Sm[:, hpq, :], in_to_replace=sv[:, hpq, 0:8],
                                                                 in_values=Sc[:, hpq, :], imm_value=NEG),
                         [sk_, "sv"], ["Sm"])
                    S.op("dve", lambda e, hpq=hpq: e.max(out=sv[:, hpq, 8:16], in_=Sm[:, hpq, :]), ["Sm"], ["sv"])
                for h in range(8):
                    S.op("dve", lambda e, h=h: e.max_index(out=si[:, h, 0:8], in_max=sv[:, 2 * h, 0:8],
                                                          in_values=Sc[:, 2 * h, :]), [sk_, "sv"], ["si"])
                    S.op("dve", lambda e, h=h: e.max_index(out=si[:, h, 8:16], in_max=sv[:, 2 * h, 8:16],
                                                          in_values=Sm[:, 2 * h, :]), ["Sm", "sv"], ["si"])
                S.copy("dve", res4[c][:, 3, :].rearrange("p (h a) -> p h a", a=16), si[:], ["si"], ["res4_%d" % c])
                S.tt("dve", cand[:].rearrange("p h (a b) -> p h a b", b=16),
                     sv1.unsqueeze(3).to_broadcast([128, 8, 16, 16]),
                     sv2.unsqueeze(2).to_broadcast([128, 8, 16, 16]), ALU.add, ["sv"], ["cand"])
                for h in range(8):
                    S.op("dve", lambda e, h=h: e.max(out=c8a[:, h, :], in_=cand[:, h, :]), ["cand"], ["c8a"])
                    S.op("dve", lambda e, h=h: e.match_replace(out=candm[:, h, :], in_to_replace=c8a[:, h, :],
                                                             in_values=cand[:, h, :], imm_value=NEG),
                         ["cand", "c8a"], ["Sm"])
                    S.op("dve", lambda e, h=h: e.max(out=c8b[:, h, :], in_=candm[:, h, :]), ["Sm"], ["c8b"])
                S.tt("dve", ee[:], cand[:], c8a[:, :, 0:1].to_broadcast([128, 8, 256]), ALU.subtract,
                     ["cand", "c8a"], ["ee"])

            def p5_B2(g, c):
                S.act(ee[:], ee[:], AF.Exp, ["ee"], ["ee"])
                S.tt("dve", candm[:], cand[:], c8b[:, :, 7:8].to_broadcast([128, 8, 256]), ALU.is_ge,
                     ["cand", "c8b"], ["Sm"])
                S.tt("dve", ee[:], ee[:], candm[:], ALU.mult, ["ee", "Sm"], ["ee"])
                S.op("dve", lambda e: e.tensor_reduce(out=Zt[:], in_=ee[:], axis=AX.X, op=ALU.add), ["ee"], ["Zt"])
                S.act(Zt[:], Zt[:], AF.Ln, ["Zt"], ["Zt"])
                S.tt("dve", mz[:], Zt[:], c8a[:, :, 0], ALU.add, ["Zt", "c8a"], ["mz"])
                rk = "res4_%d" % c
                S.tt("dve", res4[c][:, 0, :].rearrange("p (h a) -> p h a", a=16), sv1,
                     mz[:].unsqueeze(2).to_broadcast([128, 8, 16]), ALU.subtract, ["sv", "mz"], [rk])
                S.op("dve", lambda e: e.scalar_tensor_tensor(out=Zt[:], in0=c8b[:, :, 7], scalar=-1.0e-5, in1=mz[:],
                                                             op0=ALU.add, op1=ALU.subtract), ["c8b", "mz"], ["Zt"])
                S.copy("dve", res4[c][:, 2, :].rearrange("p (h a) -> p h a", a=16),
                       Zt[:].unsqueeze(2).to_broadcast([128, 8, 16]), ["Zt"], [rk])

            def p5_C(g):
                t0 = g * 256
                for c in range(2):
                    for n_ in (0, 2, 3):
                        pb = n_ % 2
                        S.tr(bank[pb][:, 0:128], res4[c][:, n_, :], ident[:], ["res4_%d" % c, "ident"], [BK[pb]])
                        S.act(stg[:, n_, c * 128:(c + 1) * 128], bank[pb][:, 0:128], AF.Copy, [BK[pb]], ["stg"])
                S.st(bT_s[:, t0:t0 + 256], stg[:, 0, :], "p5s1", ["stg"])
                S.st(tT_s[:, t0:t0 + 256], stg[:, 2, :], "p5s2", ["stg"])
                S.st(iT_s[:, t0:t0 + 256], stg[:, 3, :], "p5s3", ["stg"])

            cin = [sb(ph, "cin%d" % i, [128, 1024]) for i in range(2)]
            cout = [sb(ph, "cout%d" % i, [128, 1024], BF16) for i in range(2)]
            conv_state = [0]

            def conv_some(n_items):
                for _ in range(n_items):
                    i = conv_state[0]
                    if i >= 256:
                        return
                    conv_state[0] += 1
                    k2 = i % 2
                    hf = (i % 2) * 1024
                    S.ld(cin[k2][:], UV_d[i // 2][:, hf:hf + 1024], "cin%d" % k2, ["cin%d" % k2])
                    S.act(cout[k2][:], cin[k2][:], AF.Copy, ["cin%d" % k2], ["cout%d" % k2])
                    S.st(UVh_s[i // 2][:, hf:hf + 1024], cout[k2][:], "cout%d" % k2, ["cout%d" % k2], q="act")

            per_blk = (256 + NB - 1) // NB
            p5_A1(0)
            p5_A2(0, 0)
            p5_A2(0, 1)
            p5_A3(0)
            for g in range(NB):
                nx = g + 1 < NB
                if nx:
                    p5_A1(g + 1)
                p5_B1(g, 0)
                if nx:
                    p5_A2(g + 1, 0)
                p5_B2(g, 0)
                p5_B1(g, 1)
                if nx:
                    p5_A2(g + 1, 1)
                    p5_A3(g + 1)
                p5_B2(g, 1)
                p5_C(g)
            conv_some(256)
            S.barrier()
            S.emit()

        with ExitStack() as ph:
            S.barrier()
            skT2 = sb(ph, "skT2", [128, 128])
            iota8 = sb(ph, "iota8", [128, 8, 128])
            fgbc = sb(ph, "fgbc", [128, D])
            hT = [sb(ph, "bhT%d" % i, [128, 8, 256], BF16) for i in range(2)]
            q2 = [sb(ph, "bq2%d" % i, [128, 8, 256]) for i in range(2)]
            bti = [sb(ph, "bbti%d" % i, [128, 4, 256]) for i in range(2)]
            x1b = [sb(ph, "bx1%d" % i, [128, 2, D]) for i in range(2)]
            Q2rep = [sb(ph, "Q2rep%d" % i, [128, 16, 128]) for i in range(2)]
            Xt = [sb(ph, "Xt%d" % i, [128, 8, 128]) for i in range(2)]
            Et = [sb(ph, "Et%d" % i, [128, 8, 128]) for i in range(2)]
            Lp = [sb(ph, "Lp%d" % i, [128, 8, 128]) for i in range(2)]
            WT = sb(ph, "WT", [128, 256, 128], F16)
            NUV = 6
            UVb = [sb(ph, "UVb%d" % i, [128, 2048], BF16) for i in range(NUV)]
            Gt = [sb(ph, "Gt%d" % i, [128, 256]) for i in range(2)]
            At = [sb(ph, "At%d" % i, [128, 256], BF16) for i in range(2)]
            x2 = sb(ph, "x2", [128, 2, D])
            ss = sb(ph, "bss", [128, 2])
            rstd = sb(ph, "brstd", [128, 2])
            junk = sb(ph, "bjunk", [128, D], BF16)
            S.ld(skT2[:], skT_d[1], "b_c1", ["skT2"])
            S.ld(iota8[:].rearrange("p a n -> p (a n)"), iota4_d, "b_c2", ["iota8"])
            S.ld(fgbc[:], fgbc_d, "b_c3", ["fgbc"])

            def b_load(bb):
                i = bb % 2
                t0 = bb * 256
                S.ld(hT[i][:], h2T_s[:, :, t0:t0 + 256].rearrange("k p t -> p k t"), "bhT%d" % i, ["bhT%d" % i])
                S.ld(q2[i][:], q2T_s[:, :, t0:t0 + 256].rearrange("h p t -> p h t"), "bq2%d" % i, ["bq2%d" % i])
                S.ld(bti[i][:, 0, :], bT_s[:, t0:t0 + 256], "bbti%d" % i, ["bbti%d" % i])
                S.ld(bti[i][:, 2, :], tT_s[:, t0:t0 + 256], "bbti%d" % i, ["bbti%d" % i])
                S.ld(bti[i][:, 3, :], iT_s[:, t0:t0 + 256], "bbti%d" % i, ["bbti%d" % i])
                S.ld(x1b[i][:], x1_s[t0:t0 + 256, :].rearrange("(c p) d -> p c d", p=128), "bx1%d" % i, ["bx1%d" % i])

            nuv_ctr = [0]

            def uv_load(i):
                s_ = nuv_ctr[0] % NUV
                nuv_ctr[0] += 1
                S.ld(UVb[s_][:], UVh_s[i], "UVb%d" % s_, ["UVb%d" % s_])
                return s_

            b_load(0)
            ngrp = 0
            for bb in range(NB):
                bi = bb % 2
                hk, qk, btk, x1k = "bhT%d" % bi, "bq2%d" % bi, "bbti%d" % bi, "bx1%d" % bi
                if bb + 1 < NB:
                    b_load(bb + 1)
                pend = [uv_load(i_) for i_ in range(NUV)]
                def emit_q2rep(pr):
                    tq = pr * 16
                    qr = Q2rep[pr % 2]
                    qrk = "Q2rep%d" % (pr % 2)
                    S.act(qr[:].rearrange("k t (h a) -> k t h a", a=16),
                          q2[bi][:, :, tq:tq + 16].rearrange("k h t -> k t h").unsqueeze(3).to_broadcast([128, 16, 8, 16]),
                          AF.Copy, [qk], [qrk])

                def emit_s2rep(grp):
                    qr = Q2rep[(grp // 2) % 2]
                    qrk = "Q2rep%d" % ((grp // 2) % 2)
                    rb = 2 * (grp % 2)
                    for tt_ in range(8):
                        S.mm(bank[rb + tt_ // 4][:, (tt_ % 4) * 128:(tt_ % 4 + 1) * 128], qr[:, (grp % 2) * 8 + tt_, :], skT2[:],
                             True, True, [qrk, "skT2"], [BK[rb + tt_ // 4]])

                def emit_gate(grp):
                    tq = grp * 8
                    u = grp % 2
                    rb = 2 * u
                    wb = 4 + 2 * u
                    xk, ek, lk = "Xt%d" % u, "Et%d" % u, "Lp%d" % u
                    r3 = psall[:, rb:rb + 2, :].rearrange("p b (a n) -> p (b a) n", n=128)
                    rks = [BK[rb], BK[rb + 1]]

                    def bc(row):
                        return bti[bi][:, row, tq:tq + 8].unsqueeze(2).to_broadcast([128, 8, 128])
                    S.tt("dve", Xt[u][:], r3, bc(0), ALU.add, rks + [btk], [xk])
                    S.tt("dve", Et[u][:], Xt[u][:], bc(2), ALU.is_lt, [xk, btk], [ek])
                    S.op("dve", lambda e: e.scalar_tensor_tensor(out=Et[u][:], in0=Et[u][:], scalar=-1.0e4, in1=Xt[u][:],
                                                                 op0=ALU.mult, op1=ALU.add), [ek, xk], [ek])
                    S.act(Et[u][:], Et[u][:], AF.Exp, [ek], [ek])
                    S.tt("dve", Lp[u][:], iota8[:], bc(3), ALU.is_equal, ["iota8", btk], [lk])
                    for tt_ in range(8):
                        S.mm(bank[wb + tt_ // 4][:, (tt_ % 4) * 128:(tt_ % 4 + 1) * 128], Et[u][:, tt_, :], Lp[u][:, tt_, :],
                             True, True, [ek, lk], [BK[wb + tt_ // 4]])

                def emit_gate_back(grp):
                    tq = grp * 8
                    wb = 4 + 2 * (grp % 2)
                    S.act(WT[:, tq:tq + 8, :], psall[:, wb:wb + 2, :].rearrange("p b (a n) -> p (b a) n", n=128), AF.Copy,
                          [BK[wb], BK[wb + 1]], ["WT"])

                emit_q2rep(0)
                emit_s2rep(0)
                emit_q2rep(1)
                for grp in range(32):
                    if grp + 1 < 32:
                        emit_s2rep(grp + 1)
                    if grp % 2 == 1 and grp // 2 + 2 < 16:
                        emit_q2rep(grp // 2 + 2)
                    emit_gate(grp)
                    if grp >= 1:
                        emit_gate_back(grp - 1)
                emit_gate_back(31)
                slot = {}

                def emit_A(i):
                    slot[i] = pend.pop(0)
                    s_ = slot[i]
                    ab = i % 2
                    for kc in range(8):
                        S.mm(bank[ab][:, 0:256], UVb[s_][:, kc * 128:(kc + 1) * 128], hT[bi][:, kc, :], kc == 0, kc == 7,
                             ["UVb%d" % s_, hk], [BK[ab]])

                def emit_V(i):
                    s_ = slot[i]
                    ab = i % 2
                    S.act(Gt[ab][:], bank[ab][:, 0:256], AF.Gelu, [BK[ab]], ["Gt%d" % ab])
                    S.tt("dve", At[ab][:], Gt[ab][:], WT[:, :, i], ALU.mult, ["Gt%d" % ab, "WT"], ["At%d" % ab])
                    for c in range(2):
                        for n in range(2):
                            yb = 4 + c * 2 + n
                            S.mm(bank[yb][:, :], At[ab][:, c * 128:(c + 1) * 128], UVb[s_][:, D + n * 512:D + (n + 1) * 512],
                                 i == 0, i == 127, ["At%d" % ab, "UVb%d" % s_], [BK[yb]])
                    if i + NUV < 128:
                        pend.append(uv_load(i + NUV))

                emit_A(0)
                for i in range(128):
                    if i + 1 < 128:
                        emit_A(i + 1)
                    emit_V(i)
                for c in range(2):
                    for n in range(2):
                        yb = 4 + c * 2 + n
                        S.tt("dve", x2[:, c, n * 512:(n + 1) * 512], bank[yb][:, :], g2bc[:, n * 512:(n + 1) * 512], ALU.mult,
                             [BK[yb], "g2bc"], ["x2"])
                    S.tt("pool", x2[:, c, :], x2[:, c, :], x1b[bi][:, c, :], ALU.add, ["x2", x1k], ["x2"])
                    S.act(junk[:], x2[:, c, :], AF.Square, ["x2"], ["bjunk"], accum_out=ss[:, c:c + 1])
                S.ts("dve", rstd[:], ss[:], 1.0 / D, 1e-6, ALU.mult, ALU.add, ["bss", "bjunk"], ["brstd"])
                S.act(rstd[:], rstd[:], AF.Sqrt, ["brstd"], ["brstd"])
                S.op("dve", lambda e: e.reciprocal(out=rstd[:], in_=rstd[:]), ["brstd"], ["brstd"])
                for c in range(2):
                    S.act(x2[:, c, :], x2[:, c, :], AF.Identity, ["x2", "brstd"], ["x2"], scale=rstd[:, c:c + 1])
                    S.tt("pool", x2[:, c, :], x2[:, c, :], fgbc[:], ALU.mult, ["x2", "fgbc"], ["x2"])
                S.st(y_d[bb * 256:(bb + 1) * 256, :].rearrange("(c p) d -> p c d", p=128), x2[:], "yout", ["x2"])
            S.barrier()
            S.emit()
        print("bass instructions:", S.ninst)
    return nc


def make_in_maps(inputs, S_TOK=8192, n_cores=8):
    f = lambda a: np.ascontiguousarray(a, dtype=np.float32)
    cst = _consts()
    w_ada = f(inputs["w_ada"][0])
    b_ada = inputs["b_ada"][0]
    shared = dict(
        w_ada=w_ada,
        b_adaT=f(b_ada.reshape(48, 128).T),
        b_ada_bc=f(np.broadcast_to(np.concatenate([b_ada[2048:3072], b_ada[5120:6144]])[None, :], (128, 2048))),
        n1g=f(inputs["norm1_g"][0].reshape(8, 128).T),
        n2g=f(inputs["norm2_g"][0].reshape(8, 128).T),
        fg_bc=f(np.broadcast_to(inputs["final_g"][None, :], (128, D))),
        w_in=f(inputs["w_in"][0]),
        pool_w=f(inputs["pool_w"][0]),
        pool_scT=f(inputs["pool_scale"][0].reshape(4, 128).T),
        w_out=f(inputs["w_out"][0]),
        wq=f(inputs["peer_wq"][0]),
        skT=f(np.transpose(inputs["peer_subkeys"][0], (0, 2, 1))),
        UV=f(np.concatenate([np.transpose(inputs["peer_u"][0].reshape(128, 128, 8, 128), (0, 3, 2, 1)).reshape(128, 128, 1024),
                             inputs["peer_v"][0].reshape(128, 128, 1024)], axis=2)),
        **cst,
    )
    maps = []
    for b in range(n_cores):
        m = dict(shared)
        m["x"] = f(inputs["x"][b, :S_TOK])
        m["cT"] = f(inputs["c"][b].reshape(8, 128).T)
        maps.append(m)
    return maps


def kernel(**inputs):
    nc = build(8192)
    in_maps = make_in_maps(inputs, 8192, 8)
    res = run_bass_kernel_spmd(nc, in_maps, core_ids=list(range(8)))
    return np.stack([np.asarray(r["y"], dtype=np.float32) for r in res.results], axis=0)
```

```python
import numpy as np
from contextlib import ExitStack
import concourse.bass as bass
import concourse.mybir as mybir
from concourse.bass_utils import run_bass_kernel_spmd

F32 = mybir.dt.float32
F16 = mybir.dt.float16
F32R = mybir.dt.float32r
BF16 = mybir.dt.bfloat16
U32 = mybir.dt.uint32
AF = mybir.ActivationFunctionType
ALU = mybir.AluOpType
AX = mybir.AxisListType

D = 1024
NH = 8
HD = 64
NEG = -1.0e30


class Sched:
    ENG = ("pe", "act", "dve", "pool", "sp")
    ENGN = {"pe": "tensor", "act": "scalar", "dve": "vector", "pool": "gpsimd", "sp": "sync"}

    def __init__(self, nc, stack):
        self.nc = nc
        self.stack = stack
        self.prog = {e: [] for e in self.ENG}
        self.cnt = {e: 0 for e in self.ENG}
        self.sem = {e: stack.enter_context(nc.semaphore("sem_" + e)) for e in self.ENG}
        self.waited = {e: {} for e in self.ENG}
        self.res = {}
        self.dsem = {}
        self.ninst = 0

    def dma_sem(self, name):
        if name not in self.dsem:
            self.dsem[name] = [self.stack.enter_context(self.nc.semaphore("d_" + name)), 0]
        return self.dsem[name]

    def _need(self, eng, tok, waits):
        if tok is None:
            return
        kind, a, b = tok
        if kind == "e" and a == eng and eng == "pe":
            return
        key = (kind, a)
        if self.waited[eng].get(key, 0) >= b:
            return
        if waits.get(key, 0) < b:
            waits[key] = b

    def _deps(self, eng, reads, writes):
        waits = {}
        for k in reads:
            r = self.res.setdefault(k, {"w": None, "r": {}})
            self._need(eng, r["w"], waits)
        for k in writes:
            r = self.res.setdefault(k, {"w": None, "r": {}})
            self._need(eng, r["w"], waits)
            for (kind, a), b in r["r"].items():
                self._need(eng, (kind, a, b), waits)
        for key, v in waits.items():
            self.waited[eng][key] = v
            self.prog[eng].append(("wait", key, v))

    def _record(self, tok, reads, writes):
        for k in reads:
            rr = self.res[k]["r"]
            key = (tok[0], tok[1])
            if rr.get(key, 0) < tok[2]:
                rr[key] = tok[2]
        for k in writes:
            self.res[k] = {"w": tok, "r": {}}

    def op(self, eng, fn, reads=(), writes=()):
        self._deps(eng, reads, writes)
        self.cnt[eng] += 1
        tok = ("e", eng, self.cnt[eng])
        self.prog[eng].append(("op", fn))
        self._record(tok, reads, writes)
        self.ninst += 1

    def dma(self, q, fn, semname, reads=(), writes=()):
        self._deps(q, reads, writes)
        s = self.dma_sem(semname)
        s[1] += 16
        tok = ("d", semname, s[1])
        self.prog[q].append(("dma", fn, semname))
        self._record(tok, reads, writes)
        self.ninst += 1

    def barrier(self):
        for e in self.ENG:
            for o in self.ENG:
                if o != e and self.cnt[o] > self.waited[e].get(("e", o), 0):
                    self.waited[e][("e", o)] = self.cnt[o]
                    self.prog[e].append(("wait", ("e", o), self.cnt[o]))
            for name, s in self.dsem.items():
                if s[1] > self.waited[e].get(("d", name), 0):
                    self.waited[e][("d", name)] = s[1]
                    self.prog[e].append(("wait", ("d", name), s[1]))

    def emit(self):
        nc = self.nc
        with nc.Block() as block:
            for e in self.ENG:
                prog = self.prog[e]

                def body(engine, prog=prog, e=e):
                    for it in prog:
                        if it[0] == "wait":
                            key, v = it[1], it[2]
                            h = self.sem[key[1]] if key[0] == "e" else self.dsem[key[1]][0]
                            engine.wait_ge(h, v)
                        elif it[0] == "op":
                            it[1](engine).then_inc(self.sem[e], 1)
                        else:
                            it[1](engine).then_inc(self.dsem[it[2]][0], 16)
                getattr(block, self.ENGN[e])(body)
        self.prog = {e: [] for e in self.ENG}

    def mm(self, out, lhsT, rhs, start, stop, reads, writes):
        self.op("pe", lambda e: e.matmul(out, lhsT=lhsT, rhs=rhs, start=start, stop=stop), reads, writes)

    def tr(self, out, in_, ident, reads, writes):
        self.op("pe", lambda e: e.transpose(out, in_, ident), reads, writes)

    def act(self, out, in_, func, reads, writes, scale=None, bias=None, accum_out=None):
        kw = {}
        if scale is not None:
            kw["scale"] = scale
        if bias is not None:
            kw["bias"] = bias
        if accum_out is not None:
            kw["accum_out"] = accum_out
        self.op("act", lambda e: e.activation(out=out, in_=in_, func=func, **kw), reads, writes)

    def tt(self, eng, out, in0, in1, op, reads, writes):
        self.op(eng, lambda e: e.tensor_tensor(out=out, in0=in0, in1=in1, op=op), reads, writes)

    def ts(self, eng, out, in0, s1, s2, op0, op1, reads, writes):
        if op1 is None:
            self.op(eng, lambda e: e.tensor_scalar(out=out, in0=in0, scalar1=s1, scalar2=None, op0=op0), reads, writes)
        else:
            self.op(eng, lambda e: e.tensor_scalar(out=out, in0=in0, scalar1=s1, scalar2=s2, op0=op0, op1=op1), reads, writes)

    def copy(self, eng, out, in_, reads, writes):
        self.op(eng, lambda e: e.tensor_copy(out=out, in_=in_), reads, writes)

    def memset(self, eng, ap, val, writes):
        self.op(eng, lambda e: e.memset(ap, val), (), writes)

    def ld(self, out, in_, sem, writes, q="sp"):
        self.dma(q, lambda e: e.dma_start(out=out, in_=in_), sem, (), writes)

    def st(self, out, in_, sem, reads, q="pool"):
        self.dma(q, lambda e: e.dma_start(out=out, in_=in_), sem, reads, ())


def _consts():
    ident = np.eye(128, dtype=np.float32)
    k = np.arange(128)[:, None, None]
    rel = np.arange(17)[None, :, None]
    q = np.arange(128)[None, None, :]
    dist = rel * 128 + q - k
    cnt = ((dist >= 0) & (dist <= 128)).astype(np.float32)
    cnt += ((dist >= 0) & (dist <= 512) & (dist % 4 == 0)).astype(np.float32)
    cnt += ((dist >= 0) & (dist <= 2048) & (dist % 16 == 0)).astype(np.float32)
    CT = np.ascontiguousarray(cnt.reshape(128, 17 * 128), dtype=np.float32)
    slopes = np.array([2.0 ** (-8.0 * (h + 1) / NH) for h in range(NH)], dtype=np.float64)
    kl = np.arange(128, dtype=np.float64)[:, None, None]
    bk = slopes[None, :, None] * (kl - 64.0) - slopes[None, :, None] * 128.0 * np.arange(17)[None, None, :]
    biasK = np.ascontiguousarray(bk.reshape(128, NH * 17), dtype=np.float32)
    bm = slopes[None, :, None] * (kl - 64.0) - slopes[None, :, None] * 128.0 * (np.arange(-3, 17)[None, None, :] + 1.5)
    biasM = np.ascontiguousarray(bm.reshape(128, NH * 20), dtype=np.float32)
    band = np.zeros((128, 4, 3, 128), dtype=np.float64)
    s = np.arange(128)[:, None]
    t = np.arange(128)[None, :]
    for pg, w in enumerate((2, 4, 8, 16)):
        cur = ((s <= t) & (s > t - w)).astype(np.float64) / w - (s == t)
        prev = ((t + 128 - s) < w).astype(np.float64) / w
        cntf = np.minimum(t + 1, w).astype(np.float64)
        first = ((s <= t) & (s > t - w)).astype(np.float64) / cntf - (s == t)
        band[:, pg, 0, :] = cur
        band[:, pg, 1, :] = prev
        band[:, pg, 2, :] = first
    band = np.ascontiguousarray(band.reshape(128, 12 * 128), dtype=np.float32)
    iota4 = np.ascontiguousarray(np.tile(np.arange(128, dtype=np.float32), (128, 8)))
    return dict(ident=ident, CT=CT, biasK=biasK, biasM=biasM, band=band, iota4=iota4)


def build(S_TOK=8192, debug=False):
    NCHK = S_TOK // 128
    NG = S_TOK // 512
    NB = S_TOK // 256
    nc = bass.Bass("TRN2", target_bir_lowering=False)
    scr_kind = "ExternalOutput" if debug else "Internal"

    def din(name, shape, dt=F32):
        return nc.dram_tensor(name, list(shape), dt, kind="ExternalInput").ap()

    def dscr(name, shape, dt=F32):
        return nc.dram_tensor(name, list(shape), dt, kind=scr_kind).ap()

    x_d = din("x", [S_TOK, D])
    cT_d = din("cT", [128, 8])
    wada_d = din("w_ada", [D, 6 * D])
    badaT_d = din("b_adaT", [128, 48])
    badabc_d = din("b_ada_bc", [128, 2 * D])
    n1g_d = din("n1g", [128, 8])
    n2g_d = din("n2g", [128, 8])
    fgbc_d = din("fg_bc", [128, D])
    win_d = din("w_in", [D, 2048], F32R)
    poolw_d = din("pool_w", [4, 128, 128], F32R)
    poolsc_d = din("pool_scT", [128, 4])
    wout_d = din("w_out", [D, D], F32R)
    wq_d = din("wq", [D, 2048])
    skT_d = din("skT", [2, 128, 128], F32R)
    UV_d = din("UV", [128, 128, 2048])
    ident_d = din("ident", [128, 128])
    CT_d = din("CT", [128, 17 * 128])
    biasK_d = din("biasK", [128, NH * 17])
    biasM_d = din("biasM", [128, NH * 20])
    band_d = din("band", [128, 12 * 128], F32R)
    iota4_d = din("iota4", [128, 1024])

    qT_s = dscr("qT_scr", [4, 128, S_TOK], F32R)
    kT_s = dscr("kT_scr", [4, 128, S_TOK], F32R)
    v_s = dscr("v_scr", [S_TOK, NH * 66], BF16)
    u_s = dscr("u_scr", [S_TOK, 512], F32R)
    attn_s = dscr("attn_scr", [S_TOK, 512])
    x1_s = dscr("x1_scr", [S_TOK, D])
    h2T_s = dscr("h2T_scr", [8, 128, S_TOK], BF16)
    UVh_s = dscr("UVh_scr", [128, 128, 2048], BF16)
    q2T_s = dscr("q2T_scr", [8, 128, S_TOK], F32R)
    bT_s = dscr("bT_scr", [128, S_TOK])
    nT_s = dscr("nT_scr", [128, S_TOK])
    tT_s = dscr("tT_scr", [128, S_TOK])
    iT_s = dscr("iT_scr", [128, S_TOK])
    y_d = nc.dram_tensor("y", [S_TOK, D], F32, kind="ExternalOutput").ap()

    with ExitStack() as top:
        S = Sched(nc, top)

        def sb(stack, name, shape, dt=F32):
            return stack.enter_context(nc.sbuf_tensor("s_" + name, list(shape), dt))

        psall = top.enter_context(nc.psum_tensor("psall", [128, 8, 512], F32))
        bank = [psall[:, i, :] for i in range(8)]
        BK = ["bank%d" % i for i in range(8)]

        ident = sb(top, "ident", [128, 128])
        scale1 = sb(top, "scale1", [128, 8])
        shift1 = sb(top, "shift1", [128, 8])
        scale2 = sb(top, "scale2", [128, 8])
        shift2 = sb(top, "shift2", [128, 8])
        g1bc = sb(top, "g1bc", [128, D])
        g2bc = sb(top, "g2bc", [128, D])
        S.ld(ident[:], ident_d, "c_ident", ["ident"])

        with ExitStack() as ph:
            cT = sb(ph, "cT", [128, 8])
            cs = sb(ph, "cs", [128, 8])
            csrep = sb(ph, "csrep", [128, 8, 128])
            badaT = sb(ph, "badaT", [128, 48])
            badabc = sb(ph, "badabc", [128, 2 * D])
            n1g = sb(ph, "n1g", [128, 8])
            n2g = sb(ph, "n2g", [128, 8])
            modc = sb(ph, "modc", [128, 48])
            wa = [sb(ph, "wa%d" % i, [128, 8, 512]) for i in range(2)]
            S.ld(cT[:], cT_d, "p1a", ["cT"])
            S.ld(badaT[:], badaT_d, "p1b", ["badaT"])
            S.ld(badabc[:], badabc_d, "p1c", ["badabc"])
            S.ld(n1g[:], n1g_d, "p1d", ["n1g"])
            S.ld(n2g[:], n2g_d, "p1e", ["n2g"])
            S.act(cs[:], cT[:], AF.Silu, ["cT"], ["cs"])
            S.copy("dve", csrep[:], cs[:].unsqueeze(2).to_broadcast([128, 8, 128]), ["cs"], ["csrep"])
            S.memset("dve", modc[:], 0.0, ["modc"])
            for n in range(12):
                w = wa[n % 2]
                wk = "wa%d" % (n % 2)
                S.ld(w[:], wada_d[:, n * 512:(n + 1) * 512].rearrange("(kc p) n -> p kc n", p=128), wk, [wk])
                if n in (4, 5, 10, 11):
                    pb = bank[n % 2]
                    for kc in range(8):
                        S.mm(pb[:, :], csrep[:, kc, :], w[:, kc, :], kc == 0, kc == 7, ["csrep", wk], [BK[n % 2]])
                    dst = g1bc if n < 6 else g2bc
                    dk = "g1bc" if n < 6 else "g2bc"
                    off = (n % 2) * 512
                    boff = off if n < 6 else D + off
                    S.tt("dve", dst[:, off:off + 512], pb[:, :], badabc[:, boff:boff + 512], ALU.add,
                         [BK[n % 2], "badabc"], [dk])
                else:
                    pb = bank[2 + n % 2]
                    for jj in range(4):
                        for kc in range(8):
                            S.mm(pb[:, jj:jj + 1], w[:, kc, jj * 128:(jj + 1) * 128], cs[:, kc:kc + 1],
                                 kc == 0, kc == 7, ["cs", wk], [BK[2 + n % 2]])
                    S.tt("dve", modc[:, 4 * n:4 * n + 4], pb[:, 0:4], badaT[:, 4 * n:4 * n + 4], ALU.add,
                         [BK[2 + n % 2], "badaT"], ["modc"])
            S.op("dve", lambda e: e.scalar_tensor_tensor(out=scale1[:], in0=modc[:, 8:16], scalar=1.0, in1=n1g[:],
                                                         op0=ALU.add, op1=ALU.mult), ["modc", "n1g"], ["scale1"])
            S.op("dve", lambda e: e.scalar_tensor_tensor(out=scale2[:], in0=modc[:, 32:40], scalar=1.0, in1=n2g[:],
                                                         op0=ALU.add, op1=ALU.mult), ["modc", "n2g"], ["scale2"])
            S.copy("dve", shift1[:], modc[:, 0:8], ["modc"], ["shift1"])
            S.copy("dve", shift2[:], modc[:, 24:32], ["modc"], ["shift2"])
            S.barrier()
            S.emit()

        def norm_transpose(xt, xk, xn, xnk, hT, hTk, ss, rstd, nch, scol, scolk, bcol, bcolk, pbanks, hT2=None, hT2k=None):
            for c in range(nch):
                S.act(xn[:, c, :], xt[:, c, :], AF.Square, [xk], [xnk], accum_out=ss[:, c:c + 1])
            S.ts("dve", rstd[:, 0:nch], ss[:, 0:nch], 1.0 / D, 1e-6, ALU.mult, ALU.add, ["ss", xnk], ["rstd"])
            S.act(rstd[:, 0:nch], rstd[:, 0:nch], AF.Sqrt, ["rstd"], ["rstd"])
            S.op("dve", lambda e: e.reciprocal(out=rstd[:, 0:nch], in_=rstd[:, 0:nch]), ["rstd"], ["rstd"])
            for c in range(nch):
                S.act(xn[:, c, :], xt[:, c, :], AF.Identity, [xk, "rstd"], [xnk], scale=rstd[:, c:c + 1])
            for kc in range(8):
                pb = pbanks[kc % 2]
                for c in range(nch):
                    S.tr(bank[pb][:, c * 128:(c + 1) * 128], xn[:, c, kc * 128:(kc + 1) * 128], ident[:],
                         [xnk, "ident"], [BK[pb]])
                S.act(hT[:, kc, 0:nch * 128], bank[pb][:, 0:nch * 128], AF.Identity, [BK[pb], scolk, bcolk], [hTk],
                      scale=scol[:, kc:kc + 1], bias=bcol[:, kc:kc + 1])
                if hT2 is not None:
                    S.act(hT2[:, kc, 0:nch * 128], bank[pb][:, 0:nch * 128], AF.Identity, [BK[pb], scolk, bcolk], [hT2k],
                          scale=scol[:, kc:kc + 1], bias=bcol[:, kc:kc + 1])

        with ExitStack() as ph:
            S.barrier()
            win = sb(ph, "win", [128, 8, 2048], F32R)
            xt = [sb(ph, "p2x%d" % i, [128, 4, D]) for i in range(2)]
            xn = sb(ph, "p2xn", [128, 4, D])
            hT = sb(ph, "p2hT", [128, 8, 512], F32R)
            ss = sb(ph, "p2ss", [128, 4])
            rstd = sb(ph, "p2rstd", [128, 4])
            qst = sb(ph, "p2q", [128, 4, 512], F32R)
            kst = sb(ph, "p2k", [128, 4, 512], F32R)
            vst = sb(ph, "p2v", [128, 4, NH, 66], BF16)
            ones_t = sb(ph, "p2ones", [128, 4 * NH * 66])
            ust = sb(ph, "p2u", [128, 4, 512], F32R)
            for i in range(4):
                S.ld(win[:, :, i * 512:(i + 1) * 512],
                     win_d[:, i * 512:(i + 1) * 512].rearrange("(kc p) n -> p kc n", p=128), "win", ["win"], q="pool")
            S.memset("dve", ones_t[:], 1.0, ["ones_t"])
            S.copy("dve", vst[:].rearrange("p c h f -> p (c h f)"), ones_t[:], ["ones_t"], ["vst"])
            xv = x_d.rearrange("(g c p) d -> g p c d", p=128, c=4)
            S.ld(xt[0][:], xv[0], "p2x0", ["p2x0"])
            for g in range(NG):
                xk = "p2x%d" % (g % 2)
                if g + 1 < NG:
                    S.ld(xt[(g + 1) % 2][:], xv[g + 1], "p2x%d" % ((g + 1) % 2), ["p2x%d" % ((g + 1) % 2)])
                norm_transpose(xt[g % 2], xk, xn, "p2xn", hT, "p2hT", ss, rstd, 4,
                               scale1, "scale1", shift1, "shift1", (0, 1))
                t0 = g * 512
                for hp in range(4):
                    pb = 2 + hp % 2
                    for kc in range(8):
                        S.mm(bank[pb][:, :], win[:, kc, hp * 128:(hp + 1) * 128], hT[:, kc, :], kc == 0, kc == 7,
                             ["win", "p2hT"], [BK[pb]])
                    S.act(qst[:, hp, :], bank[pb][:, :], AF.Copy, [BK[pb]], ["qst"], scale=0.125)
                for hp in range(4):
                    pb = 2 + hp % 2
                    for kc in range(8):
                        S.mm(bank[pb][:, :], win[:, kc, 512 + hp * 128:512 + (hp + 1) * 128], hT[:, kc, :],
                             kc == 0, kc == 7, ["win", "p2hT"], [BK[pb]])
                    S.copy("dve", kst[:, hp, :], bank[pb][:, :], [BK[pb]], ["kst"])
                for c in range(4):
                    pb = 4 + c % 2
                    for kc in range(8):
                        S.mm(bank[pb][:, :], hT[:, kc, c * 128:(c + 1) * 128], win[:, kc, 1024:1536], kc == 0, kc == 7,
                             ["win", "p2hT"], [BK[pb]])
                    S.act(vst[:, c, :, 0:64], bank[pb][:, :].rearrange("p (h f) -> p h f", f=64), AF.Copy,
                          [BK[pb]], ["vst"])
                for c in range(4):
                    pb = 6 + c % 2
                    for kc in range(8):
                        S.mm(bank[pb][:, :], hT[:, kc, c * 128:(c + 1) * 128], win[:, kc, 1536:2048], kc == 0, kc == 7,
                             ["win", "p2hT"], [BK[pb]])
                    S.copy("dve", ust[:, c, :], bank[pb][:, :], [BK[pb]], ["ust"])
                S.st(qT_s[:, :, t0:t0 + 512].rearrange("h p t -> p h t"), qst[:], "p2sq", ["qst"])
                S.st(kT_s[:, :, t0:t0 + 512].rearrange("h p t -> p h t"), kst[:], "p2sk", ["kst"])
                S.st(v_s[t0:t0 + 512, :].rearrange("(c p) f -> p c f", p=128), vst[:].rearrange("p c h f -> p c (h f)"),
                     "p2sv", ["vst"])
                S.st(u_s[t0:t0 + 512, :].rearrange("(c p) f -> p c f", p=128), ust[:], "p2su", ["ust"])
            S.barrier()
            S.emit()

        with ExitStack() as ph:
            S.barrier()
            CT = sb(ph, "CT", [128, 17 * 128])
            biasK = sb(ph, "biasK", [128, NH * 17])
            biasM = sb(ph, "biasM", [128, NH * 20])
            KT = sb(ph, "KT", [128, S_TOK], F32R)
            Vp = sb(ph, "Vp", [128, NCHK, 2, 66], BF16)
            QTg = [sb(ph, "QTg%d" % i, [128, 512], F32R) for i in range(2)]
            PT = [sb(ph, "PT%d" % i, [128, 512]) for i in range(2)]
            PTm = [[sb(ph, "PTm%d_%d" % (p_, i), [128, 512], BF16) for i in range(20)] for p_ in range(2)]
            ao = [sb(ph, "ao%d" % i, [128, 4, 128]) for i in range(2)]
            rden = sb(ph, "rden", [128, 8])
            S.ld(CT[:], CT_d, "p3c", ["CT"])
            S.ld(biasK[:], biasK_d, "p3b", ["biasK"])
            S.ld(biasM[:], biasM_d, "p3b2", ["biasM"])
            cnt3 = dict(nrd=0, nsb=0, nob=0, npt=0)
            units = [(hp, g, hh) for hp in range(4) for g in range(NG) for hh in range(2)]

            def p3_S(n):
                hp, g, hh = units[n]
                par = n % 2
                it = hp * NG + g
                qk = "QTg%d" % (it % 2)
                qt = QTg[it % 2]
                if g == 0 and hh == 0:
                    npc = max(1, S_TOK // 2048)
                    for i in range(npc):
                        w = S_TOK // npc
                        S.ld(KT[:, i * w:(i + 1) * w], kT_s[hp, :, i * w:(i + 1) * w], "p3k", ["KT"], q="pool")
                if hh == 0:
                    S.ld(qt[:], qT_s[hp, :, g * 512:(g + 1) * 512], qk, [qk], q="pool")
                h = 2 * hp + hh
                r0 = 64 * hh
                kbs = list(range(max(0, 4 * g - 16), 4 * g + 4))
                steps = []
                for kb in kbs:
                    steps.append(lambda kb=kb: s_step(kb, hp, g, hh, h, r0, par, qk, qt))
                return steps

            def s_step(kb, hp, g, hh, h, r0, par, qk, qt):
                if True:
                    kbi = kb - (4 * g - 16)
                    sbk = cnt3["nsb"] % 2
                    cnt3["nsb"] += 1
                    S.mm(bank[sbk][:, :], KT[r0:r0 + 64, kb * 128:(kb + 1) * 128], qt[r0:r0 + 64, :], True, True,
                         ["KT", qk], [BK[sbk]])
                    ptk = "PT%d" % (cnt3["npt"] % 2)
                    ptt = PT[cnt3["npt"] % 2]
                    cnt3["npt"] += 1
                    js = [j for j in range(4) if 0 <= 4 * g + j - kb <= 16]
                    j0, j1 = js[0], js[-1]
                    if h >= 2:
                        r_ = 4 * g - kb
                        S.act(ptt[:, j0 * 128:(j1 + 1) * 128], bank[sbk][:, j0 * 128:(j1 + 1) * 128], AF.Exp,
                              [BK[sbk], "biasM"], [ptk], bias=biasM[:, h * 20 + r_ + 3:h * 20 + r_ + 4])
                    else:
                        for j in js:
                            rel = 4 * g + j - kb
                            S.act(ptt[:, j * 128:(j + 1) * 128], bank[sbk][:, j * 128:(j + 1) * 128], AF.Exp,
                                  [BK[sbk], "biasK"], [ptk], bias=biasK[:, h * 17 + rel:h * 17 + rel + 1])
                    rel0 = 4 * g + j0 - kb
                    S.tt("dve", PTm[par][kbi][:, j0 * 128:(j1 + 1) * 128], ptt[:, j0 * 128:(j1 + 1) * 128],
                         CT[:, rel0 * 128:(rel0 + j1 - j0 + 1) * 128], ALU.mult, [ptk, "CT"], ["PTm%d_%d" % (par, kbi)])

            def p3_V(n):
                hp, g, hh = units[n]
                par = n % 2
                it = hp * NG + g
                aok = "ao%d" % (it % 2)
                aot = ao[it % 2]
                if g == 0 and hh == 0:
                    nvp = max(1, NCHK // 8)
                    for i in range(nvp):
                        nb_ = NCHK // nvp
                        S.ld(Vp[:, i * nb_:(i + 1) * nb_, :, :].rearrange("p b h f -> p b (h f)"),
                             v_s[i * nb_ * 128:(i + 1) * nb_ * 128, hp * 132:(hp + 1) * 132].rearrange("(b p) f -> p b f", p=128),
                             "p3v", ["Vp"], q="sp")
                kbs = list(range(max(0, 4 * g - 16), 4 * g + 4))
                steps = []
                for j in range(4):
                    steps.append(lambda j=j: v_step(j, hp, g, hh, par, aok, aot, kbs))
                return steps

            def v_step(j, hp, g, hh, par, aok, aot, kbs):
                if True:
                    obk = 2 + cnt3["nob"] % 2
                    cnt3["nob"] += 1
                    kl = [kb for kb in kbs if 0 <= 4 * g + j - kb <= 16]
                    for n_, kb in enumerate(kl):
                        kbi = kb - (4 * g - 16)
                        S.mm(bank[obk][:, 0:66], PTm[par][kbi][:, j * 128:(j + 1) * 128], Vp[:, kb, hh, :],
                             n_ == 0, n_ == len(kl) - 1, ["PTm%d_%d" % (par, kbi), "Vp"], [BK[obk]])
                    rc = cnt3["nrd"] % 8
                    cnt3["nrd"] += 1
                    S.op("dve", lambda e, obk=obk, rc=rc: e.reciprocal(out=rden[:, rc:rc + 1], in_=bank[obk][:, 64:65]),
                         [BK[obk]], ["rden%d" % rc])
                    S.act(aot[:, j, hh * 64:(hh + 1) * 64], bank[obk][:, 0:64], AF.Identity,
                          [BK[obk], "rden%d" % rc], [aok], scale=rden[:, rc:rc + 1])
                if hh == 1 and j == 3:
                    S.st(attn_s[g * 512:(g + 1) * 512, hp * 128:(hp + 1) * 128].rearrange("(c p) f -> p c f", p=128),
                         aot[:], aok, [aok], q="act")

            for st_ in p3_S(0):
                st_()
            for n in range(len(units)):
                vs = p3_V(n)
                ss_ = p3_S(n + 1) if n + 1 < len(units) else []
                ns = len(ss_)
                vi = 0
                for i_, st_ in enumerate(ss_):
                    st_()
                    while vi < 4 and (i_ + 1) * 4 >= (vi + 1) * ns:
                        vs[vi]()
                        vi += 1
                while vi < 4:
                    vs[vi]()
                    vi += 1
            S.barrier()
            S.emit()

        with ExitStack() as ph:
            S.barrier()
            wout = sb(ph, "wout", [128, 8, D], F32R)
            band = sb(ph, "band", [128, 12 * 128], F32R)
            poolw = sb(ph, "poolw", [128, 4, 128], F32R)
            poolsc = sb(ph, "poolsc", [128, 4])
            xt = [sb(ph, "p4x%d" % i, [128, 4, D]) for i in range(2)]
            at = [sb(ph, "p4a%d" % i, [128, 4, 512]) for i in range(2)]
            ub = [sb(ph, "p4u%d" % i, [128, 5, 512], F32R) for i in range(2)]
            concatT = sb(ph, "concatT", [128, 8, 512], F32R)
            mixT = sb(ph, "mixT", [128, 4, 512], F32R)
            x1t = sb(ph, "x1t", [128, 4, D])
            for i in range(2):
                S.ld(wout[:, :, i * 512:(i + 1) * 512],
                     wout_d[:, i * 512:(i + 1) * 512].rearrange("(kc p) n -> p kc n", p=128), "wout", ["wout"], q="pool")
            S.ld(band[:], band_d, "p4c1", ["band"], q="pool")
            S.ld(poolw[:], poolw_d.rearrange("g c e -> c g e"), "p4c2", ["poolw"], q="pool")
            S.ld(poolsc[:], poolsc_d, "p4c3", ["poolsc"])
            xv = x_d.rearrange("(g c p) d -> g p c d", p=128, c=4)
            av = attn_s.rearrange("(g c p) d -> g p c d", p=128, c=4)
            uv = u_s.rearrange("(g c p) d -> g p c d", p=128, c=4)

            def p4_load(g):
                i = g % 2
                S.ld(xt[i][:], xv[g], "p4x%d" % i, ["p4x%d" % i])
                S.ld(at[i][:], av[g], "p4a%d" % i, ["p4a%d" % i])
                S.ld(ub[i][:, 1:5, :], uv[g], "p4u%d" % i, ["p4u%d" % i], q="pool")
                if g > 0:
                    S.ld(ub[i][:, 0, :], u_s[g * 512 - 128:g * 512, :], "p4u%d" % i, ["p4u%d" % i], q="pool")
            p4_load(0)
            for g in range(NG):
                i = g % 2
                xk, ak, uk = "p4x%d" % i, "p4a%d" % i, "p4u%d" % i
                if g + 1 < NG:
                    p4_load(g + 1)
                for kc in range(4):
                    pb = kc % 2
                    for c in range(4):
                        S.tr(bank[pb][:, c * 128:(c + 1) * 128], at[i][:, c, kc * 128:(kc + 1) * 128], ident[:],
                             [ak, "ident"], [BK[pb]])
                    S.act(concatT[:, kc, :], bank[pb][:, :], AF.Copy, [BK[pb]], ["concatT"])
                for pg in range(4):
                    pb = 2 + pg % 2
                    for c in range(4):
                        o = bank[pb][:, c * 128:(c + 1) * 128]
                        if 4 * g + c == 0:
                            S.mm(o, ub[i][:, 1 + c, pg * 128:(pg + 1) * 128], band[:, (pg * 3 + 2) * 128:(pg * 3 + 3) * 128],
                                 True, True, [uk, "band"], [BK[pb]])
                        else:
                            S.mm(o, ub[i][:, 1 + c, pg * 128:(pg + 1) * 128], band[:, (pg * 3 + 0) * 128:(pg * 3 + 1) * 128],
                                 True, False, [uk, "band"], [BK[pb]])
                            S.mm(o, ub[i][:, c, pg * 128:(pg + 1) * 128], band[:, (pg * 3 + 1) * 128:(pg * 3 + 2) * 128],
                                 False, True, [uk, "band"], [BK[pb]])
                    S.copy("dve", mixT[:, pg, :], bank[pb][:, :], [BK[pb]], ["mixT"])
                for pg in range(4):
                    pb = 4 + pg % 2
                    S.mm(bank[pb][:, :], poolw[:, pg, :], mixT[:, pg, :], True, True, ["poolw", "mixT"], [BK[pb]])
                    S.act(concatT[:, 4 + pg, :], bank[pb][:, :], AF.Identity, [BK[pb], "poolsc"], ["concatT"],
                          scale=poolsc[:, pg:pg + 1])
                for c in range(4):
                    for n in range(2):
                        pb = 6 + n
                        for kc in range(8):
                            S.mm(bank[pb][:, :], concatT[:, kc, c * 128:(c + 1) * 128], wout[:, kc, n * 512:(n + 1) * 512],
                                 kc == 0, kc == 7, ["concatT", "wout"], [BK[pb]])
                        S.tt("dve", x1t[:, c, n * 512:(n + 1) * 512], bank[pb][:, :], g1bc[:, n * 512:(n + 1) * 512], ALU.mult,
                             [BK[pb], "g1bc"], ["x1t"])
                    S.tt("pool", x1t[:, c, :], x1t[:, c, :], xt[i][:, c, :], ALU.add, ["x1t", xk], ["x1t"])
                S.st(x1_s[g * 512:(g + 1) * 512, :].rearrange("(c p) d -> p c d", p=128), x1t[:], "p4s", ["x1t"])
            S.barrier()
            S.emit()

        with ExitStack() as ph:
            S.barrier()
            wq = sb(ph, "wq", [128, 8, 2048])
            skT = sb(ph, "skT", [128, 2, 128], F32R)
            xt = [sb(ph, "p5x%d" % i, [128, 2, D]) for i in range(2)]
            xn = sb(ph, "p5xn", [128, 2, D])
            hT = sb(ph, "p5hT", [128, 8, 256])
            hTr = sb(ph, "p5hTr", [128, 8, 256], BF16)
            ss = sb(ph, "p5ss", [128, 4])
            rstd = sb(ph, "p5rstd", [128, 4])
            QT = sb(ph, "p5QT", [128, 16, 256], F32R)
            Ssb = [[sb(ph, "Ssb%d_%d" % (a_, c_), [128, 16, 128]) for c_ in range(2)] for a_ in range(2)]
            res4 = [sb(ph, "res4_%d" % c_, [128, 4, 128]) for c_ in range(2)]
            Sm = sb(ph, "Sm", [128, 16, 128])
            sv = sb(ph, "sv", [128, 16, 16])
            si = sb(ph, "si", [128, 8, 16], U32)
            sif = sb(ph, "sif", [128, 8, 16])
            cand = sb(ph, "cand", [128, 8, 256])
            candm = Sm[:].rearrange("p a n -> p (a n)").rearrange("p (h c) -> p h c", c=256)
            ee = sb(ph, "ee", [128, 8, 256])
            c8a = sb(ph, "c8a", [128, 8, 8])
            c8b = sb(ph, "c8b", [128, 8, 8])
            Zt = sb(ph, "Zt", [128, 8])
            mz = sb(ph, "mz", [128, 8])
            b1 = sb(ph, "b1", [128, 8, 16])
            nz = sb(ph, "nz", [128, 8, 16])
            th = sb(ph, "th", [128, 8, 16])
            stg = sb(ph, "stg", [128, 4, 256])
            for i in range(4):
                S.ld(wq[:, :, i * 512:(i + 1) * 512],
                     wq_d[:, i * 512:(i + 1) * 512].rearrange("(kc p) n -> p kc n", p=128), "wq", ["wq"])
            S.ld(skT[:], skT_d.rearrange("h k n -> k h n"), "p5c", ["skT"], q="pool")
            xv = x1_s.rearrange("(g c p) d -> g p c d", p=128, c=2)
            S.ld(xt[0][:], xv[0], "p5x0", ["p5x0"])
            sv4 = sv[:].rearrange("p (h two) a -> p h two a", two=2)
            sv1 = sv4[:, :, 0, :]
            sv2 = sv4[:, :, 1, :]
            def p5_A1(g):
                xk = "p5x%d" % (g % 2)
                if g + 1 < NB:
                    S.ld(xt[(g + 1) % 2][:], xv[g + 1], "p5x%d" % ((g + 1) % 2), ["p5x%d" % ((g + 1) % 2)])
                norm_transpose(xt[g % 2], xk, xn, "p5xn", hT, "p5hT", ss, rstd, 2,
                               scale2, "scale2", shift2, "shift2", (0, 1), hTr, "p5hTr")
                t0 = g * 256
                S.st(h2T_s[:, :, t0:t0 + 256].rearrange("k p t -> p k t"), hTr[:], "p5sh", ["p5hTr"])

            def p5_A2(g, half):
                for hpq in range(8 * half, 8 * half + 8):
                    pb = 2 + hpq % 2
                    for kc in range(8):
                        S.mm(bank[pb][:, 0:256], wq[:, kc, hpq * 128:(hpq + 1) * 128], hT[:, kc, :], kc == 0, kc == 7,
                             ["wq", "p5hT"], [BK[pb]])
                    S.act(QT[:, hpq, :], bank[pb][:, 0:256], AF.Copy, [BK[pb]], ["p5QT"])
                    if hpq % 2 == 1:
                        conv_some(1)

            def p5_A3(g):
                t0 = g * 256
                S.st(q2T_s[:, :, t0:t0 + 256].rearrange("h p t -> p h t"),
                     QT[:].rearrange("p (h two) t -> p h two t", two=2)[:, :, 1, :], "p5sq", ["p5QT"])
                for c in range(2):
                    sk_ = "Ssb%d_%d" % (g % 2, c)
                    for b4 in range(4):
                        pb = 4 + b4
                        for k4 in range(4):
                            hpq = b4 * 4 + k4
                            S.mm(bank[pb][:, k4 * 128:(k4 + 1) * 128], QT[:, hpq, c * 128:(c + 1) * 128], skT[:, hpq % 2, :],
                                 True, True, ["p5QT", "skT"], [BK[pb]])
                        S.act(Ssb[g % 2][c][:, b4 * 4:(b4 + 1) * 4, :], bank[pb][:, :].rearrange("p (a n) -> p a n", n=128), AF.Copy,
                              [BK[pb]], [sk_])

            def p5_B1(g, c):
                sk_ = "Ssb%d_%d" % (g % 2, c)
                Sc = Ssb[g % 2][c]
                for hpq in range(16):
                    S.op("dve", lambda e, hpq=hpq: e.max(out=sv[:, hpq, 0:8], in_=Sc[:, hpq, :]), [sk_], ["sv"])
                    S.op("dve", lambda e, hpq=hpq: e.match_replace(out=Sm[:, hpq, :], in_to_replace=sv[:, hpq, 0:8],
                                                                 in_values=Sc[:, hpq, :], imm_value=NEG),
                         [sk_, "sv"], ["Sm"])
                    S.op("dve", lambda e, hpq=hpq: e.max(out=sv[:, hpq, 8:16], in_=Sm[:, hpq, :]), ["Sm"], ["sv"])
                for h in range(8):
                    S.op("dve", lambda e, h=h: e.max_index(out=si[:, h, 0:8], in_max=sv[:, 2 * h, 0:8],
                                                          in_values=Sc[:, 2 * h, :]), [sk_, "sv"], ["si"])
                    S.op("dve", lambda e, h=h: e.max_index(out=si[:, h, 8:16], in_max=sv[:, 2 * h, 8:16],
                                                          in_values=Sm[:, 2 * h, :]), ["Sm", "sv"], ["si"])
                S.copy("dve", res4[c][:, 3, :].rearrange("p (h a) -> p h a", a=16), si[:], ["si"], ["res4_%d" % c])
                S.tt("dve", cand[:].rearrange("p h (a b) -> p h a b", b=16),
                     sv1.unsqueeze(3).to_broadcast([128, 8, 16, 16]),
                     sv2.unsqueeze(2).to_broadcast([128, 8, 16, 16]), ALU.add, ["sv"], ["cand"])
                for h in range(8):
                    S.op("dve", lambda e, h=h: e.max(out=c8a[:, h, :], in_=cand[:, h, :]), ["cand"], ["c8a"])
                    S.op("dve", lambda e, h=h: e.match_replace(out=candm[:, h, :], in_to_replace=c8a[:, h, :],
                                                             in_values=cand[:, h, :], imm_value=NEG),
                         ["cand", "c8a"], ["Sm"])
                    S.op("dve", lambda e, h=h: e.max(out=c8b[:, h, :], in_=candm[:, h, :]), ["Sm"], ["c8b"])
                S.tt("dve", ee[:], cand[:], c8a[:, :, 0:1].to_broadcast([128, 8, 256]), ALU.subtract,
                     ["cand", "c8a"], ["ee"])

            def p5_B2(g, c):
                S.act(ee[:], ee[:], AF.Exp, ["ee"], ["ee"])
                S.tt("dve", candm[:], cand[:], c8b[:, :, 7:8].to_broadcast([128, 8, 256]), ALU.is_ge,
                     ["cand", "c8b"], ["Sm"])
                S.tt("dve", ee[:], ee[:], candm[:], ALU.mult, ["ee", "Sm"], ["ee"])
                S.op("dve", lambda e: e.tensor_reduce(out=Zt[:], in_=ee[:], axis=AX.X, op=ALU.add), ["ee"], ["Zt"])
                S.act(Zt[:], Zt[:], AF.Ln, ["Zt"], ["Zt"])
                S.tt("dve", mz[:], Zt[:], c8a[:, :, 0], ALU.add, ["Zt", "c8a"], ["mz"])
                rk = "res4_%d" % c
                S.tt("dve", res4[c][:, 0, :].rearrange("p (h a) -> p h a", a=16), sv1,
                     mz[:].unsqueeze(2).to_broadcast([128, 8, 16]), ALU.subtract, ["sv", "mz"], [rk])
                S.op("dve", lambda e: e.scalar_tensor_tensor(out=Zt[:], in0=c8b[:, :, 7], scalar=-1.0e-5, in1=mz[:],
                                                             op0=ALU.add, op1=ALU.subtract), ["c8b", "mz"], ["Zt"])
                S.copy("dve", res4[c][:, 2, :].rearrange("p (h a) -> p h a", a=16),
                       Zt[:].unsqueeze(2).to_broadcast([128, 8, 16]), ["Zt"], [rk])

            def p5_C(g):
                t0 = g * 256
                for c in range(2):
                    for n_ in (0, 2, 3):
                        pb = n_ % 2
                        S.tr(bank[pb][:, 0:128], res4[c][:, n_, :], ident[:], ["res4_%d" % c, "ident"], [BK[pb]])
                        S.act(stg[:, n_, c * 128:(c + 1) * 128], bank[pb][:, 0:128], AF.Copy, [BK[pb]], ["stg"])
                S.st(bT_s[:, t0:t0 + 256], stg[:, 0, :], "p5s1", ["stg"])
                S.st(tT_s[:, t0:t0 + 256], stg[:, 2, :], "p5s2", ["stg"])
                S.st(iT_s[:, t0:t0 + 256], stg[:, 3, :], "p5s3", ["stg"])

            cin = [sb(ph, "cin%d" % i, [128, 1024]) for i in range(2)]
            cout = [sb(ph, "cout%d" % i, [128, 1024], BF16) for i in range(2)]
            conv_state = [0]

            def conv_some(n_items):
                for _ in range(n_items):
                    i = conv_state[0]
                    if i >= 256:
                        return
                    conv_state[0] += 1
                    k2 = i % 2
                    hf = (i % 2) * 1024
                    S.ld(cin[k2][:], UV_d[i // 2][:, hf:hf + 1024], "cin%d" % k2, ["cin%d" % k2])
                    S.act(cout[k2][:], cin[k2][:], AF.Copy, ["cin%d" % k2], ["cout%d" % k2])
                    S.st(UVh_s[i // 2][:, hf:hf + 1024], cout[k2][:], "cout%d" % k2, ["cout%d" % k2], q="act")

            per_blk = (256 + NB - 1) // NB
            p5_A1(0)
            p5_A2(0, 0)
            p5_A2(0, 1)
            p5_A3(0)
            for g in range(NB):
                nx = g + 1 < NB
                if nx:
                    p5_A1(g + 1)
                p5_B1(g, 0)
                if nx:
                    p5_A2(g + 1, 0)
                p5_B2(g, 0)
                p5_B1(g, 1)
                if nx:
                    p5_A2(g + 1, 1)
                    p5_A3(g + 1)
                p5_B2(g, 1)
                p5_C(g)
            conv_some(256)
            S.barrier()
            S.emit()

        with ExitStack() as ph:
            S.barrier()
            skT2 = sb(ph, "skT2", [128, 128], F32R)
            iota8 = sb(ph, "iota8", [128, 8, 128])
            fgbc = sb(ph, "fgbc", [128, D])
            hT = [sb(ph, "bhT%d" % i, [128, 8, 256], BF16) for i in range(2)]
            q2 = [sb(ph, "bq2%d" % i, [128, 8, 256], F32R) for i in range(2)]
            bti = [sb(ph, "bbti%d" % i, [128, 4, 256]) for i in range(2)]
            x1b = [sb(ph, "bx1%d" % i, [128, 2, D]) for i in range(2)]
            Q2rep = [sb(ph, "Q2rep%d" % i, [128, 16, 128], F32R) for i in range(2)]
            Xt = [sb(ph, "Xt%d" % i, [128, 8, 128]) for i in range(2)]
            Et = [sb(ph, "Et%d" % i, [128, 8, 128]) for i in range(2)]
            Lp = [sb(ph, "Lp%d" % i, [128, 8, 128]) for i in range(2)]
            WT = sb(ph, "WT", [128, 256, 128], F16)
            NUV = 6
            UVb = [sb(ph, "UVb%d" % i, [128, 2048], BF16) for i in range(NUV)]
            Gt = [sb(ph, "Gt%d" % i, [128, 256]) for i in range(2)]
            At = [sb(ph, "At%d" % i, [128, 256], BF16) for i in range(2)]
            x2 = sb(ph, "x2", [128, 2, D])
            ss = sb(ph, "bss", [128, 2])
            rstd = sb(ph, "brstd", [128, 2])
            junk = sb(ph, "bjunk", [128, D], BF16)
            S.ld(skT2[:], skT_d[1], "b_c1", ["skT2"], q="pool")
            S.ld(iota8[:].rearrange("p a n -> p (a n)"), iota4_d, "b_c2", ["iota8"])
            S.ld(fgbc[:], fgbc_d, "b_c3", ["fgbc"])

            def b_load(bb):
                i = bb % 2
                t0 = bb * 256
                S.ld(hT[i][:], h2T_s[:, :, t0:t0 + 256].rearrange("k p t -> p k t"), "bhT%d" % i, ["bhT%d" % i])
                S.ld(q2[i][:], q2T_s[:, :, t0:t0 + 256].rearrange("h p t -> p h t"), "bq2%d" % i, ["bq2%d" % i], q="pool")
                S.ld(bti[i][:, 0, :], bT_s[:, t0:t0 + 256], "bbti%d" % i, ["bbti%d" % i])
                S.ld(bti[i][:, 2, :], tT_s[:, t0:t0 + 256], "bbti%d" % i, ["bbti%d" % i])
                S.ld(bti[i][:, 3, :], iT_s[:, t0:t0 + 256], "bbti%d" % i, ["bbti%d" % i])
                S.ld(x1b[i][:], x1_s[t0:t0 + 256, :].rearrange("(c p) d -> p c d", p=128), "bx1%d" % i, ["bx1%d" % i])

            nuv_ctr = [0]

            def uv_load(i):
                s_ = nuv_ctr[0] % NUV
                nuv_ctr[0] += 1
                S.ld(UVb[s_][:], UVh_s[i], "UVb%d" % s_, ["UVb%d" % s_])
                return s_

            b_load(0)
            ngrp = 0
            for bb in range(NB):
                bi = bb % 2
                hk, qk, btk, x1k = "bhT%d" % bi, "bq2%d" % bi, "bbti%d" % bi, "bx1%d" % bi
                if bb + 1 < NB:
                    b_load(bb + 1)
                pend = [uv_load(i_) for i_ in range(NUV)]
                def emit_q2rep(pr):
                    tq = pr * 16
                    qr = Q2rep[pr % 2]
                    qrk = "Q2rep%d" % (pr % 2)
                    S.act(qr[:].rearrange("k t (h a) -> k t h a", a=16),
                          q2[bi][:, :, tq:tq + 16].rearrange("k h t -> k t h").unsqueeze(3).to_broadcast([128, 16, 8, 16]),
                          AF.Copy, [qk], [qrk])

                def emit_s2rep(grp):
                    qr = Q2rep[(grp // 2) % 2]
                    qrk = "Q2rep%d" % ((grp // 2) % 2)
                    rb = 2 * (grp % 2)
                    for tt_ in range(8):
                        S.mm(bank[rb + tt_ // 4][:, (tt_ % 4) * 128:(tt_ % 4 + 1) * 128], qr[:, (grp % 2) * 8 + tt_, :], skT2[:],
                             True, True, [qrk, "skT2"], [BK[rb + tt_ // 4]])

                def emit_gate(grp):
                    tq = grp * 8
                    u = grp % 2
                    rb = 2 * u
                    wb = 4 + 2 * u
                    xk, ek, lk = "Xt%d" % u, "Et%d" % u, "Lp%d" % u
                    r3 = psall[:, rb:rb + 2, :].rearrange("p b (a n) -> p (b a) n", n=128)
                    rks = [BK[rb], BK[rb + 1]]

                    def bc(row):
                        return bti[bi][:, row, tq:tq + 8].unsqueeze(2).to_broadcast([128, 8, 128])
                    S.tt("dve", Xt[u][:], r3, bc(0), ALU.add, rks + [btk], [xk])
                    S.tt("dve", Et[u][:], Xt[u][:], bc(2), ALU.is_lt, [xk, btk], [ek])
                    S.op("dve", lambda e: e.scalar_tensor_tensor(out=Et[u][:], in0=Et[u][:], scalar=-1.0e4, in1=Xt[u][:],
                                                                 op0=ALU.mult, op1=ALU.add), [ek, xk], [ek])
                    S.act(Et[u][:], Et[u][:], AF.Exp, [ek], [ek])
                    S.tt("dve", Lp[u][:], iota8[:], bc(3), ALU.is_equal, ["iota8", btk], [lk])
                    for tt_ in range(8):
                        S.mm(bank[wb + tt_ // 4][:, (tt_ % 4) * 128:(tt_ % 4 + 1) * 128], Et[u][:, tt_, :], Lp[u][:, tt_, :],
                             True, True, [ek, lk], [BK[wb + tt_ // 4]])

                def emit_gate_back(grp):
                    tq = grp * 8
                    wb = 4 + 2 * (grp % 2)
                    S.act(WT[:, tq:tq + 8, :], psall[:, wb:wb + 2, :].rearrange("p b (a n) -> p (b a) n", n=128), AF.Copy,
                          [BK[wb], BK[wb + 1]], ["WT"])

                emit_q2rep(0)
                emit_s2rep(0)
                emit_q2rep(1)
                for grp in range(32):
                    if grp + 1 < 32:
                        emit_s2rep(grp + 1)
                    if grp % 2 == 1 and grp // 2 + 2 < 16:
                        emit_q2rep(grp // 2 + 2)
                    emit_gate(grp)
                    if grp >= 1:
                        emit_gate_back(grp - 1)
                emit_gate_back(31)
                slot = {}

                def emit_A(i):
                    slot[i] = pend.pop(0)
                    s_ = slot[i]
                    ab = i % 2
                    for kc in range(8):
                        S.mm(bank[ab][:, 0:256], UVb[s_][:, kc * 128:(kc + 1) * 128], hT[bi][:, kc, :], kc == 0, kc == 7,
                             ["UVb%d" % s_, hk], [BK[ab]])

                def emit_V(i):
                    s_ = slot[i]
                    ab = i % 2
                    S.act(Gt[ab][:], bank[ab][:, 0:256], AF.Gelu, [BK[ab]], ["Gt%d" % ab])
                    S.tt("dve", At[ab][:], Gt[ab][:], WT[:, :, i], ALU.mult, ["Gt%d" % ab, "WT"], ["At%d" % ab])
                    for c in range(2):
                        for n in range(2):
                            yb = 4 + c * 2 + n
                            S.mm(bank[yb][:, :], At[ab][:, c * 128:(c + 1) * 128], UVb[s_][:, D + n * 512:D + (n + 1) * 512],
                                 i == 0, i == 127, ["At%d" % ab, "UVb%d" % s_], [BK[yb]])
                    if i + NUV < 128:
                        pend.append(uv_load(i + NUV))

                emit_A(0)
                for i in range(128):
                    if i + 1 < 128:
                        emit_A(i + 1)
                    emit_V(i)
                for c in range(2):
                    for n in range(2):
                        yb = 4 + c * 2 + n
                        S.tt("dve", x2[:, c, n * 512:(n + 1) * 512], bank[yb][:, :], g2bc[:, n * 512:(n + 1) * 512], ALU.mult,
                             [BK[yb], "g2bc"], ["x2"])
                    S.tt("pool", x2[:, c, :], x2[:, c, :], x1b[bi][:, c, :], ALU.add, ["x2", x1k], ["x2"])
                    S.act(junk[:], x2[:, c, :], AF.Square, ["x2"], ["bjunk"], accum_out=ss[:, c:c + 1])
                S.ts("dve", rstd[:], ss[:], 1.0 / D, 1e-6, ALU.mult, ALU.add, ["bss", "bjunk"], ["brstd"])
                S.act(rstd[:], rstd[:], AF.Sqrt, ["brstd"], ["brstd"])
                S.op("dve", lambda e: e.reciprocal(out=rstd[:], in_=rstd[:]), ["brstd"], ["brstd"])
                for c in range(2):
                    S.act(x2[:, c, :], x2[:, c, :], AF.Identity, ["x2", "brstd"], ["x2"], scale=rstd[:, c:c + 1])
                    S.tt("pool", x2[:, c, :], x2[:, c, :], fgbc[:], ALU.mult, ["x2", "fgbc"], ["x2"])
                S.st(y_d[bb * 256:(bb + 1) * 256, :].rearrange("(c p) d -> p c d", p=128), x2[:], "yout", ["x2"])
            S.barrier()
            S.emit()
        print("bass instructions:", S.ninst)
    return nc


def make_in_maps(inputs, S_TOK=8192, n_cores=8):
    f = lambda a: np.ascontiguousarray(a, dtype=np.float32)
    cst = _consts()
    w_ada = f(inputs["w_ada"][0])
    b_ada = inputs["b_ada"][0]
    shared = dict(
        w_ada=w_ada,
        b_adaT=f(b_ada.reshape(48, 128).T),
        b_ada_bc=f(np.broadcast_to(np.concatenate([b_ada[2048:3072], b_ada[5120:6144]])[None, :], (128, 2048))),
        n1g=f(inputs["norm1_g"][0].reshape(8, 128).T),
        n2g=f(inputs["norm2_g"][0].reshape(8, 128).T),
        fg_bc=f(np.broadcast_to(inputs["final_g"][None, :], (128, D))),
        w_in=f(inputs["w_in"][0]),
        pool_w=f(inputs["pool_w"][0]),
        pool_scT=f(inputs["pool_scale"][0].reshape(4, 128).T),
        w_out=f(inputs["w_out"][0]),
        wq=f(inputs["peer_wq"][0]),
        skT=f(np.transpose(inputs["peer_subkeys"][0], (0, 2, 1))),
        UV=f(np.concatenate([np.transpose(inputs["peer_u"][0].reshape(128, 128, 8, 128), (0, 3, 2, 1)).reshape(128, 128, 1024),
                             inputs["peer_v"][0].reshape(128, 128, 1024)], axis=2)),
        **cst,
    )
    maps = []
    for b in range(n_cores):
        m = dict(shared)
        m["x"] = f(inputs["x"][b, :S_TOK])
        m["cT"] = f(inputs["c"][b].reshape(8, 128).T)
        maps.append(m)
    return maps


def kernel(**inputs):
    nc = build(8192)
    in_maps = make_in_maps(inputs, 8192, 8)
    res = run_bass_kernel_spmd(nc, in_maps, core_ids=list(range(8)))
    return np.stack([np.asarray(r["y"], dtype=np.float32) for r in res.results], axis=0)
```
